# Optimizing a Trainium2 kernel written in Bass

```python
import math
import jax, jax.numpy as jnp
from jax import lax
import numpy as np


D_MODEL = 1024
BATCH = 4
SEQ = 4096
DEPTH = 2

HEAD_DIM = 64
N_HEADS_MIX = 8
BRANCH_WIDTH = N_HEADS_MIX * HEAD_DIM
N_BRANCHES = 3
IDX_HEADS = 16
IDX_DIM = 64
TOPK_MAX = 256
Q_BLOCK = 128
D_FF = 4 * D_MODEL
LN_EPS = 1e-5
DEEPNORM_ALPHA = (2.0 * DEPTH) ** 0.25
DEEPNORM_BETA = (8.0 * DEPTH) ** -0.25
FORGET_BIAS_MEAN = 3.0

IN_SIZES = (
    BRANCH_WIDTH, BRANCH_WIDTH, BRANCH_WIDTH,
    IDX_HEADS * IDX_DIM, IDX_DIM, IDX_HEADS,
    BRANCH_WIDTH, BRANCH_WIDTH, BRANCH_WIDTH,
    N_HEADS_MIX,
    BRANCH_WIDTH, BRANCH_WIDTH, BRANCH_WIDTH,
    N_BRANCHES * D_MODEL,
)
V_SECTIONS = (2, 8, 12)
IN_COLS = sum(IN_SIZES)

kernel_name = 'hybrid_dsa_fox_stickbreak_gated_deepnorm'


def _layer_norm(x, g, b):
    xf = x.astype(jnp.float32)
    mu = jnp.mean(xf, axis=-1, keepdims=True)
    xc = xf - mu
    var = jnp.mean(xc * xc, axis=-1, keepdims=True)
    y = xc * lax.rsqrt(var + LN_EPS) * g.astype(jnp.float32) + b.astype(jnp.float32)
    return y.astype(x.dtype)


def _alibi_slopes(n):
    return 2.0 ** (-8.0 * jnp.arange(1, n + 1, dtype=jnp.float32) / n)


def _to_blocks(a):
    b, s = a.shape[:2]
    a = a.reshape((b, s // Q_BLOCK, Q_BLOCK) + a.shape[2:])
    return jnp.moveaxis(a, 1, 0)


def _from_blocks(a):
    a = jnp.moveaxis(a, 0, 1)
    b, nb, qb = a.shape[:3]
    return a.reshape(b, nb * qb, -1)


def _dsa_attention(q, k, v, q_idx, k_idx, w_idx, slopes):
    s_len = q.shape[1]
    topk = min(TOPK_MAX, s_len // 4)
    pos = jnp.arange(s_len)
    scale = HEAD_DIM ** -0.5

    def block(args):
        qb, qib, wb, tq = args
        dots = jnp.einsum('bqhd,bkd->bqhk', qib, k_idx, preferred_element_type=jnp.float32)
        score = jnp.einsum('bqhk,bqh->bqk', jax.nn.relu(dots), wb.astype(jnp.float32))
        causal = pos[None, :] <= tq[:, None]
        score = jnp.where(causal[None], score, -jnp.inf)
        _, idx = lax.top_k(score, topk)
        valid = idx <= tq[None, :, None]
        k_sel = jax.vmap(lambda kb, ib: kb[ib])(k, idx)
        v_sel = jax.vmap(lambda vb, ib: vb[ib])(v, idx)
        logits = jnp.einsum('bqhd,bqkhd->bhqk', qb, k_sel, preferred_element_type=jnp.float32) * scale
        dist = (tq[None, :, None] - idx).astype(jnp.float32)
        logits = logits - slopes[None, :, None, None] * dist[:, None]
        logits = jnp.where(valid[:, None], logits, -jnp.inf)
        p = jax.nn.softmax(logits, axis=-1).astype(v.dtype)
        return jnp.einsum('bhqk,bqkhd->bqhd', p, v_sel)

    out = lax.map(block, (_to_blocks(q), _to_blocks(q_idx), _to_blocks(w_idx), pos.reshape(-1, Q_BLOCK)))
    return _from_blocks(out)


def _forgetting_attention(q, k, v, log_f):
    s_len = q.shape[1]
    pos = jnp.arange(s_len)
    scale = HEAD_DIM ** -0.5
    c = jnp.cumsum(log_f, axis=1)
    c_k = jnp.transpose(c, (0, 2, 1))[:, :, None, :]

    def block(args):
        qb, cq, tq = args
        logits = jnp.einsum('bqhd,bkhd->bhqk', qb, k, preferred_element_type=jnp.float32) * scale
        logits = logits + jnp.transpose(cq, (0, 2, 1))[..., None] - c_k
        causal = pos[None, :] <= tq[:, None]
        logits = jnp.where(causal[None, None], logits, -jnp.inf)
        p = jax.nn.softmax(logits, axis=-1).astype(v.dtype)
        return jnp.einsum('bhqk,bkhd->bqhd', p, v)

    out = lax.map(block, (_to_blocks(q), _to_blocks(c), pos.reshape(-1, Q_BLOCK)))
    return _from_blocks(out)


def _stick_breaking_attention(q, k, v):
    s_len = q.shape[1]
    pos = jnp.arange(s_len)
    scale = HEAD_DIM ** -0.5

    def block(args):
        qb, tq = args
        z = jnp.einsum('bqhd,bkhd->bhqk', qb, k, preferred_element_type=jnp.float32) * scale
        strict = (pos[None, :] < tq[:, None])[None, None]
        log_beta = jax.nn.log_sigmoid(z)
        log_one_minus = jnp.where(strict, jax.nn.log_sigmoid(-z), 0.0)
        later = lax.cumsum(log_one_minus, axis=3, reverse=True) - log_one_minus
        a = jnp.where(strict, jnp.exp(log_beta + later), 0.0).astype(v.dtype)
        return jnp.einsum('bhqk,bkhd->bqhd', a, v)

    out = lax.map(block, (_to_blocks(q), pos.reshape(-1, Q_BLOCK)))
    return _from_blocks(out)


def setup_inputs(seed: int = 0) -> dict:
    key = jax.random.key(seed)
    ks = jax.random.split(key, 12)
    x = jax.random.normal(ks[0], (BATCH, SEQ, D_MODEL), jnp.float32)
    sec_keys = jax.random.split(ks[1], len(IN_SIZES))
    secs = []
    for i, size in enumerate(IN_SIZES):
        sc = D_MODEL ** -0.5 * (DEEPNORM_BETA if i in V_SECTIONS else 1.0)
        secs.append(jax.random.normal(sec_keys[i], (DEPTH, D_MODEL, size), jnp.float32) * sc)
    w_in = jnp.concatenate(secs, axis=-1)
    b_forget = FORGET_BIAS_MEAN + 0.5 * jax.random.normal(ks[2], (DEPTH, N_HEADS_MIX), jnp.float32)
    w_branch = jax.random.normal(ks[3], (DEPTH, N_BRANCHES, BRANCH_WIDTH, D_MODEL), jnp.float32) * (BRANCH_WIDTH ** -0.5 * DEEPNORM_BETA)
    w_out = jax.random.normal(ks[4], (DEPTH, D_MODEL, D_MODEL), jnp.float32) * (D_MODEL ** -0.5 * DEEPNORM_BETA)
    ln1_g = 1.0 + 0.05 * jax.random.normal(ks[5], (DEPTH, D_MODEL), jnp.float32)
    ln1_b = 0.02 * jax.random.normal(ks[6], (DEPTH, D_MODEL), jnp.float32)
    w_ff1 = jax.random.normal(ks[7], (DEPTH, D_MODEL, D_FF), jnp.float32) * (D_MODEL ** -0.5 * DEEPNORM_BETA)
    w_ff2 = jax.random.normal(ks[8], (DEPTH, D_FF, D_MODEL), jnp.float32) * (D_FF ** -0.5 * DEEPNORM_BETA)
    ln2_g = 1.0 + 0.05 * jax.random.normal(ks[9], (DEPTH, D_MODEL), jnp.float32)
    ln2_b = 0.02 * jax.random.normal(ks[10], (DEPTH, D_MODEL), jnp.float32)
    return {'x': x, 'w_in': w_in, 'b_forget': b_forget, 'w_branch': w_branch, 'w_out': w_out,
            'ln1_g': ln1_g, 'ln1_b': ln1_b, 'w_ff1': w_ff1, 'w_ff2': w_ff2,
            'ln2_g': ln2_g, 'ln2_b': ln2_b}


def reference(x, w_in, b_forget, w_branch, w_out, ln1_g, ln1_b, w_ff1, w_ff2, ln2_g, ln2_b):
    b, s, _ = x.shape
    slopes = _alibi_slopes(N_HEADS_MIX)
    split_at = [int(v) for v in np.cumsum(IN_SIZES)[:-1]]

    def heads(a):
        return a.reshape(b, s, N_HEADS_MIX, HEAD_DIM)

    for layer in range(DEPTH):
        proj = jnp.einsum('bsd,dc->bsc', x, w_in[layer])
        (qa, ka, va, qi, ki, wi, qf, kf, vf, fg, qs, ks_, vs, gates) = jnp.split(proj, split_at, axis=-1)
        o_a = _dsa_attention(heads(qa), heads(ka), heads(va),
                             qi.reshape(b, s, IDX_HEADS, IDX_DIM), ki, wi, slopes)
        log_f = jax.nn.log_sigmoid((fg + b_forget[layer]).astype(jnp.float32))
        o_b = _forgetting_attention(heads(qf), heads(kf), heads(vf), log_f)
        o_c = _stick_breaking_attention(heads(qs), heads(ks_), heads(vs))
        branches = jnp.stack([o_a, o_b, o_c], axis=0)
        up = jnp.einsum('ibsw,iwd->bsid', branches, w_branch[layer])
        g = jax.nn.sigmoid(gates.reshape(b, s, N_BRANCHES, D_MODEL))
        merged = jnp.sum(up * g, axis=2)
        y = jnp.einsum('bsd,de->bse', merged, w_out[layer])
        x = _layer_norm(DEEPNORM_ALPHA * x + y, ln1_g[layer], ln1_b[layer])
        h = jnp.square(jax.nn.relu(jnp.einsum('bsd,df->bsf', x, w_ff1[layer])))
        y = jnp.einsum('bsf,fd->bsd', h, w_ff2[layer])
        x = _layer_norm(DEEPNORM_ALPHA * x + y, ln2_g[layer], ln2_b[layer])
    return x
```

```python
from contextlib import ExitStack

import numpy as np
import concourse.bass as bass
import concourse.mybir as mybir
from concourse.bass_utils import run_bass_kernel_spmd

F32 = mybir.dt.float32
BF16 = mybir.dt.bfloat16
AF = mybir.ActivationFunctionType
ALU = mybir.AluOpType

D = 1024
S = 4096
NB = 4
DEPTH = 2
DFF = 4096
NCOLS = 8792
ALPHA = (2.0 * DEPTH) ** 0.25
EPS = 1e-5
BIG = 30000.0
NIT = 24
OFF = dict(qa=0, ka=512, va=1024, qi=1536, ki=2560, wi=2624, qf=2640, kf=3152, vf=3664,
           fg=4176, qs=4184, ks=4696, vs=5208, g=5720)
ENGS = ("pe", "act", "dve", "pool", "sp")
ND = 40


class Op:
    __slots__ = ("eng", "fn", "deps", "signal", "sigval", "dsem", "dval", "dma")


class Prog:
    def __init__(self, nc, stack):
        self.nc = nc
        self.ops = {e: [] for e in ENGS}
        self.writers = {}
        self.readers = {}
        self.dma_ops = []
        self.sem = {e: stack.enter_context(nc.semaphore("s_" + e)) for e in ENGS}
        self.dsem = [stack.enter_context(nc.semaphore("d%d" % i)) for i in range(ND)]
        self.ccsem = stack.enter_context(nc.semaphore("ccsem"))
        self.cc_ops = []
        self.count = {e: 0 for e in ENGS}
        self.known = {e: {} for e in ENGS}
        self.last = {e: None for e in ENGS}
        self.nblk = 0

    def add(self, eng, fn, reads=(), writes=(), extra=(), dma=False, force_signal=False, cc=False):
        op = Op()
        op.eng, op.fn, op.dma = eng, fn, dma or cc
        op.signal = force_signal
        op.sigval = op.dsem = op.dval = None
        deps = set(extra)
        for k in reads:
            deps.update(self.writers.get(k, {}).values())
        for k in writes:
            deps.update(self.writers.get(k, {}).values())
            deps.update(self.readers.get(k, {}).values())
        if cc:
            self.cc_ops.append(op)
            op.dsem = "cc"
            op.dval = len(self.cc_ops)
        if dma:
            n = len(self.dma_ops)
            op.dsem = n % ND
            op.dval = 16 * (n // ND + 1)
            if n >= ND:
                deps.add(self.dma_ops[n - ND])
            self.dma_ops.append(op)
        deps.discard(op)
        if eng == "pe":
            deps = {d for d in deps if d.eng != "pe"}
        op.deps = deps
        for d in deps:
            d.signal = True
        for k in writes:
            self.writers[k] = {eng: op}
            self.readers[k] = {}
        for k in reads:
            self.readers.setdefault(k, {})[eng] = op
        self.ops[eng].append(op)
        self.last[eng] = op
        return op

    def barrier(self):
        lasts = [self.last[e] for e in ENGS if e != "sp" and self.last[e] is not None]
        lasts += self.dma_ops[-ND:]
        lasts += self.cc_ops[-4:]
        for e in ENGS:
            self.add(e, lambda g: g.nop(), extra=lasts, force_signal=True)
        self.writers = {}
        self.readers = {}

    def flush(self):
        self.barrier()
        nc = self.nc
        engobj = {"pe": "tensor", "act": "scalar", "dve": "vector", "pool": "gpsimd", "sp": "sync"}

        def emit(e, g):
            known = self.known[e]
            for op in self.ops[e]:
                need = {}
                for d in op.deps:
                    if d.dma:
                        key, val = ("d", d.dsem), d.dval
                    else:
                        key, val = d.eng, d.sigval
                    if need.get(key, 0) < val:
                        need[key] = val
                for key, val in need.items():
                    if known.get(key, 0) < val:
                        if isinstance(key, tuple):
                            s = self.ccsem if key[1] == "cc" else self.dsem[key[1]]
                        else:
                            s = self.sem[key]
                        g.wait_ge(s, val)
                        known[key] = val
                ins = op.fn(g)
                if op.dsem == "cc":
                    ins.then_inc(self.ccsem, 1)
                elif op.dma:
                    ins.then_inc(self.dsem[op.dsem], 16)
                elif op.signal:
                    ins.then_inc(self.sem[e], 1)
            self.ops[e] = []

        for e in ENGS:
            for op in self.ops[e]:
                if op.signal and not op.dma:
                    self.count[e] += 1
                    op.sigval = self.count[e]
        with nc.Block() as block:
            @block.tensor
            def _(g):
                emit("pe", g)

            @block.scalar
            def _(g):
                emit("act", g)

            @block.vector
            def _(g):
                emit("dve", g)

            @block.gpsimd
            def _(g):
                emit("pool", g)

            @block.sync
            def _(g):
                emit("sp", g)

    def finish(self):
        lasts = self.dma_ops[-ND:]
        self.add("sp", lambda g: g.nop(), extra=lasts)
        self.flush()


class Rot:
    def __init__(self, tiles):
        self.tiles = tiles
        self.i = 0

    def next(self):
        t = self.tiles[self.i % len(self.tiles)]
        self.i += 1
        return t


def build_program(n_layers=1, debug=False):
    nc = bass.Bass("TRN2", target_bir_lowering=False)
    stack = ExitStack()
    with stack:
        _build(nc, stack, n_layers, debug)
    return nc


def _build(nc, stack, n_layers, debug):
    L = n_layers

    def din(name, shape, dt=F32):
        return nc.dram_tensor(name, shape, dt, kind="ExternalInput").ap()

    def dscr(name, shape, dt):
        kind = "ExternalOutput" if debug else "Internal"
        return nc.dram_tensor(name, shape, dt, kind=kind).ap()

    xa_d = din("xa", [S, D])
    X1own_t = [nc.dram_tensor("X1own%d" % i, [512, D], F32) for i in range(4)]
    X1all_t = [nc.dram_tensor("X1all%d" % i, [1024, D], F32) for i in range(4)]
    xo_d = din("xo", [2048, D])
    pf_d = din("pf", [128, 4])
    w_in_d = din("w_in", [L, D, NCOLS])
    bfg_d = din("b_forget", [L, 8, 1])
    wbr_d = din("w_branch", [L, 3, 512, D])
    wout_d = din("w_out", [L, D, D])
    ln1g_d = din("ln1_g", [L, D])
    ln1b_d = din("ln1_b", [L, D])
    wff1_d = din("w_ff1", [L, D, DFF])
    wff2_d = din("w_ff2", [L, DFF, D])
    ln2g_d = din("ln2_g", [L, D])
    ln2b_d = din("ln2_b", [L, D])
    out_d = nc.dram_tensor("out", [2048, D], F32, kind="ExternalOutput").ap()

    QT_d = dscr("QT", [12, 128, 2048], BF16)
    KT_d = dscr("KT", [12, 128, S], BF16)
    VV_d = dscr("VV", [3, S, 512], BF16)
    QI_d = dscr("QI", [8, 128, 2048], BF16)
    KI_d = dscr("KI", [128, S], BF16)
    G_d = dscr("G", [24, 128, 2048], F32)
    NBA_d = dscr("NBA", [16, 128, S], BF16)
    OT_d = dscr("OTd", [3, 4, 128, 2048], BF16)

    P = Prog(nc, stack)

    uniq = [0]

    def sb(st, name, shape, dt):
        uniq[0] += 1
        return st.enter_context(nc.sbuf_tensor("t%d_%s" % (uniq[0], name), shape, dt))

    def ps(st, name, shape, dt=F32):
        uniq[0] += 1
        return st.enter_context(nc.psum_tensor("t%d_%s" % (uniq[0], name), shape, dt))

    ident_bf = sb(stack, "ident_bf", [128, 128], BF16)
    ident_f = sb(stack, "ident_f", [128, 128], F32)
    ones_bf = sb(stack, "ones_bf", [128, 128], BF16)
    ONESF = sb(stack, "ONESF", [128, 64], F32)
    pf = sb(stack, "pf", [128, 4], F32)
    CM = sb(stack, "CM", [128, 8, 512], BF16)
    NBQK = sb(stack, "NBQK", [128, 256], F32)
    M1 = sb(stack, "M1", [128, 256], F32)
    AB = sb(stack, "AB", [128, 32, 8], F32)
    WI = sb(stack, "WI", [128, 16, 16], F32)
    KMS = sb(stack, "KMS", [128, 16], F32)
    CNtok = sb(stack, "CNtok", [128, 32, 8], F32)
    CNown = sb(stack, "CNown", [8, 2048], F32)
    PS = [ps(stack, "ps%d" % i, [128, 512]) for i in range(8)]

    with ExitStack() as st:
        onesf = sb(st, "onesf", [128, 128], F32)
        V0 = sb(st, "V0", [128, 8, 512], F32)
        V2 = sb(st, "V2", [128, 256], F32)
        KP = sb(st, "KP", [128, 32], F32)
        P.add("sp", lambda g: g.dma_start(out=pf[:], in_=pf_d[:, :]), writes=["pf"], dma=True)
        P.add("pool", lambda g: g.memset(onesf[:], 1.0), writes=["onesf"])
        P.add("pool", lambda g: g.memset(ones_bf[:], 1.0), writes=["ones_bf"])
        P.add("pool", lambda g: g.memset(ONESF[:], 1.0), writes=["ONESF"])
        P.add("pool", lambda g: g.affine_select(out=ident_f[:], in_=onesf[:], pattern=[[-1, 128]],
                                                compare_op=ALU.is_equal, fill=0.0, base=0,
                                                channel_multiplier=1),
              reads=["onesf"], writes=["ident_f"])
        P.add("pool", lambda g: g.tensor_copy(out=ident_bf[:], in_=ident_f[:]),
              reads=["ident_f"], writes=["ident_bf"])
        P.add("pool", lambda g: g.iota(V0[:].rearrange("p j (t q) -> p j t q", q=128),
                                       pattern=[[-128, 8], [256, 4], [1, 128]], base=0,
                                       channel_multiplier=-1, allow_small_or_imprecise_dtypes=True),
              writes=["V0"])
        P.add("dve", lambda g: g.tensor_scalar(out=V0[:], in0=V0[:], scalar1=pf[:, 2:3], scalar2=None,
                                               op0=ALU.add), reads=["V0", "pf"], writes=["V0"])
        P.add("dve", lambda g: g.tensor_scalar(out=CM[:], in0=V0[:], scalar1=0.0, scalar2=-BIG,
                                               op0=ALU.is_lt, op1=ALU.mult), reads=["V0"], writes=["CM"])
        P.add("pool", lambda g: g.iota(V2[:].rearrange("p (j k) -> p j k", k=128),
                                       pattern=[[-128, 2], [-1, 128]], base=0,
                                       channel_multiplier=1, allow_small_or_imprecise_dtypes=True),
              writes=["V2"])
        P.add("dve", lambda g: g.tensor_scalar(out=V2[:], in0=V2[:], scalar1=pf[:, 2:3], scalar2=None,
                                               op0=ALU.add), reads=["V2", "pf"], writes=["V2"])
        P.add("dve", lambda g: g.tensor_scalar(out=NBQK[:], in0=V2[:], scalar1=0.0, scalar2=-BIG,
                                               op0=ALU.is_lt, op1=ALU.mult), reads=["V2"], writes=["NBQK"])
        P.add("dve", lambda g: g.tensor_scalar(out=M1[:], in0=V2[:], scalar1=0.0, scalar2=None,
                                               op0=ALU.is_le), reads=["V2"], writes=["M1"])
        P.add("pool", lambda g: g.iota(KP[:], pattern=[[128, 32]], base=0, channel_multiplier=1,
                                       allow_small_or_imprecise_dtypes=True), writes=["KP"])
        for h in range(8):
            sl = 2.0 ** (-(h + 1))
            P.add("dve", lambda g, h=h, sl=sl: g.tensor_scalar(out=AB[:, :, h], in0=KP[:], scalar1=sl,
                                                               scalar2=None, op0=ALU.mult),
                  reads=["KP"], writes=["AB"])
        P.flush()

    for layer in range(L):
        _layer(nc, P, stack, layer, locals(), layer == L - 1)
    P.finish()


def _layer(nc, P, stack, layer, E, is_last):
    xa_d, X1own_t, X1all_t = E["xa_d"], E["X1own_t"], E["X1all_t"]
    xo_d, w_in_d, bfg_d, wbr_d, wout_d = E["xo_d"], E["w_in_d"], E["bfg_d"], E["wbr_d"], E["wout_d"]
    ln1g_d, ln1b_d, wff1_d, wff2_d, ln2g_d, ln2b_d, out_d = (E["ln1g_d"], E["ln1b_d"], E["wff1_d"], E["wff2_d"],
                                                            E["ln2g_d"], E["ln2b_d"], E["out_d"])
    QT_d, KT_d, VV_d, QI_d, KI_d, G_d, NBA_d = E["QT_d"], E["KT_d"], E["VV_d"], E["QI_d"], E["KI_d"], E["G_d"], E["NBA_d"]
    ident_bf, ident_f, ones_bf, pf, CM, NBQK, M1, AB = (E["ident_bf"], E["ident_f"], E["ones_bf"], E["pf"], E["CM"],
                                                        E["NBQK"], E["M1"], E["AB"])
    ONESF = E["ONESF"]
    OT_d, WI, KMS, CNtok, CNown, PS = E["OT_d"], E["WI"], E["KMS"], E["CNtok"], E["CNown"], E["PS"]
    sb, ps = E["sb"], E["ps"]
    w_in = w_in_d[layer].rearrange("(c p) n -> p c n", p=128)

    psr = Rot(list(range(8)))

    def dma(out, in_, reads, writes, eng="sp"):
        return P.add(eng, lambda g: g.dma_start(out=out, in_=in_), reads=reads, writes=writes, dma=True)

    def mm(out, lhsT, rhs, start, stop, reads, writes):
        return P.add("pe", lambda g: g.matmul(out, lhsT, rhs, start=start, stop=stop), reads=reads, writes=writes)

    st_fg = ExitStack()
    FG = sb(st_fg, "FG", [8, S], F32)
    with ExitStack() as st:
        XT = sb(st, "XT", [128, 8, S], BF16)
        XTO = sb(st, "XTO", [128, 8, 2048], BF16)
        STG = [sb(st, "stg%d" % i, [128, 8, 512], F32) for i in range(2)]
        WB = [sb(st, "wb%d" % i, [128, 8, 512], BF16) for i in range(3)]
        OUTB = [sb(st, "outb%d" % i, [128, 512], BF16) for i in range(4)]
        OUTF = [sb(st, "outf%d" % i, [128, 512], F32) for i in range(3)]
        TMPX = sb(st, "tmpx", [128, 2048], BF16)
        stg_r, wb_r, outb_r, outf_r = Rot([0, 1]), Rot([0, 1, 2]), Rot([0, 1, 2, 3]), Rot([0, 1, 2])
        evac_flip = [0]

        def evac_copy(out, in_, reads, writes):
            evac_flip[0] ^= 1
            if evac_flip[0]:
                P.add("act", lambda g: g.activation(out=out, in_=in_, func=AF.Copy), reads=reads, writes=writes)
                return "act"
            P.add("dve", lambda g: g.tensor_copy(out=out, in_=in_), reads=reads, writes=writes)
            return "sp"

        for tc in range(8):
            i = stg_r.next()
            sv = STG[i][:].rearrange("p a b -> p (a b)").rearrange("p (t d) -> p t d", d=D)
            if layer == 0:
                dma(sv, xa_d[tc * 512:(tc + 1) * 512, :].rearrange("(t p) d -> p t d", p=128), [], ["stg%d" % i])
            else:
                for ii in range(2):
                    src = X1all_t[tc // 2].ap().rearrange("(r t p) d -> p t r d", r=2, p=128)[:, 2 * (tc % 2) + ii, :, :]
                    dma(sv[:, 2 * ii:2 * ii + 2, :], src, ["X1all%d" % (tc // 2)], ["stg%d" % i])
            for t in range(4):
                g_ = tc * 4 + t
                for c4 in range(2):
                    b = psr.next()
                    for cc in range(4):
                        c = c4 * 4 + cc
                        P.add("pe", lambda g, b=b, cc=cc, sv=sv, t=t, c=c: g.transpose(
                            PS[b][:, cc * 128:(cc + 1) * 128], sv[:, t, c * 128:(c + 1) * 128], ident_f[:]),
                              reads=["stg%d" % i], writes=["ps%d" % b])
                    evac_copy(XT[:, c4 * 4:c4 * 4 + 4, g_ * 128:(g_ + 1) * 128],
                              PS[b][:].rearrange("p (c q) -> p c q", q=128), ["ps%d" % b], [("XT", tc)])
        for c in range(8):
            v = XT[:, c, :].rearrange("p (i two q) -> p i two q", two=2, q=128)
            P.add("dve", lambda g, v=v: g.tensor_scalar(out=TMPX[:].rearrange("p (i q) -> p i q", q=128),
                                                        in0=v[:, :, 1, :], scalar1=pf[:, 0:1], scalar2=None,
                                                        op0=ALU.mult),
                  reads=[("XT", t) for t in range(8)], writes=["tmpx"])
            P.add("dve", lambda g, v=v, c=c: g.scalar_tensor_tensor(
                out=XTO[:, c, :].rearrange("p (i q) -> p i q", q=128), in0=v[:, :, 0, :], scalar=pf[:, 1:2],
                in1=TMPX[:].rearrange("p (i q) -> p i q", q=128), op0=ALU.mult, op1=ALU.add),
                  reads=[("XT", t) for t in range(8)] + ["tmpx"], writes=["XTO"])
        XTkeys = [("XT", t) for t in range(8)]

        def load_w(col0, n, dup=False):
            i = stg_r.next()
            j = wb_r.next()
            dma(STG[i][:, :, :n], w_in[:, :, col0:col0 + n], [], ["stg%d" % i])
            P.add("pool", lambda g: g.tensor_copy(out=WB[j][:, :, :n], in_=STG[i][:, :, :n]),
                  reads=["stg%d" % i], writes=["wb%d" % j])
            if dup:
                P.add("pool", lambda g: g.tensor_copy(out=WB[j][:, :, n:2 * n], in_=STG[i][:, :, :n]),
                      reads=["stg%d" % i], writes=["wb%d" % j])
            return j

        def proj_fm(j, ncols, own, dst_fn, gates=False, M=128):
            src = XTO if own else XT
            ntc = 4 if own else 8
            for gi in range(max(1, ncols // 128)):
                for tc in range(ntc):
                    b = psr.next()
                    for kc in range(8):
                        mm(PS[b][:M, :], WB[j][:, kc, gi * 128:gi * 128 + M], src[:, kc, tc * 512:(tc + 1) * 512],
                           kc == 0, kc == 7, ["wb%d" % j, "XTO"] + XTkeys, ["ps%d" % b])
                    if gates:
                        o = outf_r.next()
                        P.add("act", lambda g, b=b, o=o: g.activation(out=OUTF[o][:], in_=PS[b][:], func=AF.Sigmoid),
                              reads=["ps%d" % b], writes=["outf%d" % o])
                        dma(dst_fn(gi, tc), OUTF[o][:], ["outf%d" % o], [], eng="act")
                    else:
                        o = outb_r.next()
                        qe = evac_copy(OUTB[o][:M, :], PS[b][:M, :], ["ps%d" % b], ["outb%d" % o])
                        dma(dst_fn(gi, tc), OUTB[o][:M, :], ["outb%d" % o], [], eng=qe)

        def proj_tm(j, dst):
            for t in range(32):
                b = psr.next()
                for kc in range(8):
                    mm(PS[b][:], XT[:, kc, t * 128:(t + 1) * 128], WB[j][:, kc, :], kc == 0, kc == 7,
                       ["wb%d" % j] + XTkeys, ["ps%d" % b])
                o = outb_r.next()
                qe = evac_copy(OUTB[o][:], PS[b][:], ["ps%d" % b], ["outb%d" % o])
                dma(dst[t * 128:(t + 1) * 128, :], OUTB[o][:], ["outb%d" % o], [], eng=qe)

        for br, (qn, kn, vn) in enumerate((("qa", "ka", "va"), ("qf", "kf", "vf"), ("qs", "ks", "vs"))):
            j = load_w(OFF[qn], 512)
            proj_fm(j, 512, True, lambda gi, tc, br=br: QT_d[br * 4 + gi][:, tc * 512:(tc + 1) * 512])
            j = load_w(OFF[kn], 512)
            proj_fm(j, 512, False, lambda gi, tc, br=br: KT_d[br * 4 + gi][:, tc * 512:(tc + 1) * 512])
            j = load_w(OFF[vn], 512)
            proj_tm(j, VV_d[br])
        for half in range(2):
            j = load_w(OFF["qi"] + 512 * half, 512)
            proj_fm(j, 512, True, lambda gi, tc, half=half: QI_d[half * 4 + gi][:, tc * 512:(tc + 1) * 512])
        j = load_w(OFF["ki"], 64, dup=True)
        proj_fm(j, 128, False, lambda gi, tc: KI_d[:, tc * 512:(tc + 1) * 512])
        j = load_w(OFF["wi"], 16)
        for t in range(16):
            b = psr.next()
            for kc in range(8):
                mm(PS[b][:, :16], XTO[:, kc, t * 128:(t + 1) * 128], WB[j][:, kc, :16], kc == 0, kc == 7,
                   ["wb%d" % j, "XTO"], ["ps%d" % b])
            P.add("dve", lambda g, b=b, t=t: g.tensor_copy(out=WI[:, t, :], in_=PS[b][:, :16]),
                  reads=["ps%d" % b], writes=["WI"])
        j = load_w(OFF["fg"], 8)
        for tc in range(8):
            b = psr.next()
            for kc in range(8):
                mm(PS[b][:8, :], WB[j][:, kc, :8], XT[:, kc, tc * 512:(tc + 1) * 512], kc == 0, kc == 7,
                   ["wb%d" % j] + XTkeys, ["ps%d" % b])
            P.add("dve", lambda g, b=b, tc=tc: g.tensor_copy(out=FG[:, tc * 512:(tc + 1) * 512], in_=PS[b][:8, :]),
                  reads=["ps%d" % b], writes=["FG"])
        for gt in range(6):
            j = load_w(OFF["g"] + 512 * gt, 512)
            proj_fm(j, 512, True, lambda gi, tc, gt=gt: G_d[gt * 4 + gi][:, tc * 512:(tc + 1) * 512], gates=True)
        P.flush()
    with ExitStack() as st:
        LL = FG
        CN = sb(st, "CN", [8, S], F32)
        TMPC = sb(st, "tmpc", [8, 2048], F32)
        negb = sb(st, "negb", [8, 1], F32)
        dma(negb[:], bfg_d[layer], [], ["negb"])
        P.add("act", lambda g: g.activation(out=negb[:], in_=negb[:], func=AF.Copy, scale=-1.0),
              reads=["negb"], writes=["negb"])
        P.add("act", lambda g: g.activation(out=LL[:], in_=FG[:], func=AF.Exp, bias=negb[:, 0:1], scale=-1.0),
              reads=["FG", "negb"], writes=["FG"])
        P.add("act", lambda g: g.activation(out=LL[:], in_=LL[:], func=AF.Ln, bias=1.0, scale=1.0),
              reads=["FG"], writes=["FG"])
        P.add("dve", lambda g: g.tensor_tensor_scan(out=CN[:], data0=LL[:], data1=LL[:], initial=0.0,
                                                    op0=ALU.add, op1=ALU.bypass), reads=["FG"], writes=["CN"])
        b = psr.next()
        for t in range(32):
            P.add("pe", lambda g, t=t, b=b: g.transpose(PS[b][:, t * 8:(t + 1) * 8], CN[:, t * 128:(t + 1) * 128],
                                                        ident_f[:8, :8]),
                  reads=["CN"], writes=["ps%d" % b])
        P.add("dve", lambda g, b=b: g.tensor_copy(out=CNtok[:].rearrange("p t h -> p (t h)"), in_=PS[b][:, :256]),
              reads=["ps%d" % b], writes=["CNtok"])
        cv = CN[:].rearrange("p (i two q) -> p i two q", two=2, q=128)
        P.add("dve", lambda g: g.tensor_scalar(out=TMPC[:].rearrange("p (i q) -> p i q", q=128), in0=cv[:, :, 1, :],
                                               scalar1=pf[:8, 0:1], scalar2=None, op0=ALU.mult),
              reads=["CN"], writes=["tmpc"])
        P.add("dve", lambda g: g.scalar_tensor_tensor(out=CNown[:].rearrange("p (i q) -> p i q", q=128),
                                                      in0=cv[:, :, 0, :], scalar=pf[:8, 1:2],
                                                      in1=TMPC[:].rearrange("p (i q) -> p i q", q=128),
                                                      op0=ALU.mult, op1=ALU.add),
              reads=["CN", "tmpc"], writes=["CNown"])
        P.flush()
    st_fg.close()

    with ExitStack() as st:
        QI = sb(st, "QIs", [128, 8, 2048], BF16)
        KI = sb(st, "KIs", [128, S], BF16)
        SC = [sb(st, "SC%d" % i, [128, S], F32) for i in range(2)]
        TH = [sb(st, "TH%d" % i, [128, 512], F32) for i in range(3)]
        NBo = [sb(st, "NBo%d" % i, [128, S], BF16) for i in range(2)]
        JUNK = sb(st, "JUNK", [128, S], BF16)
        KPOS = sb(st, "KPOS", [128, S], F32)
        KSEL = sb(st, "KSEL", [128, S], F32)
        WABS = sb(st, "WABS", [128, 16, 16], F32)
        WSGN = sb(st, "WSGN", [128, 16, 16], F32)
        SM = sb(st, "SM", [128, 8], F32)
        for pr in range(8):
            dma(QI[:, pr, :], QI_d[pr], [], [("QI", pr)])
        dma(KI[:], KI_d[:, :], [], ["KI"])
        P.add("pool", lambda g: g.iota(KPOS[:], pattern=[[1, S]], base=0, channel_multiplier=0,
                                       allow_small_or_imprecise_dtypes=True), writes=["KPOS"])
        P.add("act", lambda g: g.activation(out=WABS[:], in_=WI[:], func=AF.Abs), reads=["WI"], writes=["WABS"])
        P.add("dve", lambda g: g.tensor_scalar(out=WSGN[:], in0=WI[:], scalar1=0.0, scalar2=2.0, op0=ALU.is_ge,
                                               op1=ALU.mult), reads=["WI"], writes=["WSGN"])
        P.add("dve", lambda g: g.tensor_scalar(out=WSGN[:], in0=WSGN[:], scalar1=-1.0, scalar2=None, op0=ALU.add),
              reads=["WSGN"], writes=["WSGN"])
        th_r = Rot([0, 1, 2])
        NSC = 3
        SC.append(sb(st, "SC2", [128, S], F32))
        JUNKA = sb(st, "JUNKA", [128, S], BF16)
        SMA = sb(st, "SMA", [128, 8], F32)

        def acc(i):
            nk = 128 * (2 * i + 2)
            sc = SC[i % NSC]
            sck = "SC%d" % (i % NSC)
            for kc in range((nk + 511) // 512):
                k0 = kc * 512
                kn = min(512, nk - k0)
                for h in range(16):
                    r0 = 64 * (h % 2)
                    b = psr.next()
                    mm(PS[b][:, :kn], QI[r0:r0 + 64, h // 2, i * 128:(i + 1) * 128], KI[r0:r0 + 64, k0:k0 + kn],
                       True, True, [("QI", h // 2), "KI"], ["ps%d" % b])
                    t = th_r.next()
                    P.add("act", lambda g, b=b, t=t, kn=kn, i=i, h=h: g.activation(
                        out=TH[t][:, :kn], in_=PS[b][:, :kn], func=AF.Relu, scale=WABS[:, i, h:h + 1]),
                          reads=["ps%d" % b, "WABS"], writes=["TH%d" % t])
                    if h == 0:
                        P.add("dve", lambda g, t=t, kn=kn, k0=k0, i=i, h=h, sc=sc: g.tensor_scalar(
                            out=sc[:, k0:k0 + kn], in0=TH[t][:, :kn], scalar1=WSGN[:, i, h:h + 1], scalar2=None,
                            op0=ALU.mult), reads=["TH%d" % t, "WSGN"], writes=[sck])
                    else:
                        P.add("dve", lambda g, t=t, kn=kn, k0=k0, i=i, h=h, sc=sc: g.scalar_tensor_tensor(
                            out=sc[:, k0:k0 + kn], in0=TH[t][:, :kn], scalar=WSGN[:, i, h:h + 1],
                            in1=sc[:, k0:k0 + kn], op0=ALU.mult, op1=ALU.add),
                              reads=["TH%d" % t, "WSGN", sck], writes=[sck])
            P.add("dve", lambda g, sc=sc, i=i: g.tensor_tensor(out=sc[:, 256 * i:256 * i + 256],
                                                              in0=sc[:, 256 * i:256 * i + 256], in1=NBQK[:],
                                                              op=ALU.add), reads=[sck], writes=[sck])

        def ctx(i):
            nk = 128 * (2 * i + 2)
            sc = SC[i % NSC]
            sck = "SC%d" % (i % NSC)
            use_act = (i % 2 == 1)
            smk = "SMA" if use_act else "SM"
            smt = SMA if use_act else SM
            return (nk, sc, sck, use_act, smk) + tuple(smt[:, c:c + 1] for c in range(6))

        def pre(i):
            nk, sc, sck, use_act, smk, lo, w0, mid, cnt, step, hi = ctx(i)
            if i == 0:
                P.add("dve", lambda g, lo=lo: g.memset(lo, -BIG / 2), writes=[smk])
            else:
                P.add("dve", lambda g, sc=sc, i=i, lo=lo: g.tensor_reduce(out=lo, in_=sc[:, :256 * i],
                                                                          axis=mybir.AxisListType.X, op=ALU.min),
                      reads=[sck], writes=[smk])
                P.add("dve", lambda g, sc=sc, nk=nk, hi=hi: g.tensor_reduce(out=hi, in_=sc[:, :nk],
                                                                            axis=mybir.AxisListType.X, op=ALU.max),
                      reads=[sck], writes=[smk])
                P.add("dve", lambda g, w0=w0, hi=hi, lo=lo: g.tensor_tensor(out=w0, in0=hi, in1=lo, op=ALU.subtract),
                      reads=[smk], writes=[smk])
                if use_act:
                    P.add("dve", lambda g, nlo=lo: g.tensor_scalar(out=nlo, in0=nlo, scalar1=-1.0, scalar2=None,
                                                                   op0=ALU.mult), reads=[smk], writes=[smk])

        def chain(i):
            nk, sc, sck, use_act, smk, lo, w0, mid, cnt, step, hi = ctx(i)
            if i > 0:
                if not use_act:
                    for it in range(NIT):
                        f = 2.0 ** (-(it + 1))
                        P.add("dve", lambda g, f=f, mid=mid, w0=w0, lo=lo: g.scalar_tensor_tensor(
                            out=mid, in0=w0, scalar=f, in1=lo, op0=ALU.mult, op1=ALU.add), reads=[smk], writes=[smk])
                        P.add("dve", lambda g, sc=sc, nk=nk, mid=mid, cnt=cnt: g.tensor_scalar(
                            out=JUNK[:, :nk], in0=sc[:, :nk], scalar1=mid, scalar2=None, op0=ALU.is_ge, op1=ALU.add,
                            accum_out=cnt), reads=[sck, smk], writes=["JUNK", smk])
                        P.add("dve", lambda g, f=f, step=step, cnt=cnt: g.tensor_scalar(
                            out=step, in0=cnt, scalar1=255.5, scalar2=f, op0=ALU.is_ge, op1=ALU.mult),
                              reads=[smk], writes=[smk])
                        P.add("dve", lambda g, lo=lo, w0=w0, step=step: g.scalar_tensor_tensor(
                            out=lo, in0=w0, scalar=step, in1=lo, op0=ALU.mult, op1=ALU.add),
                              reads=[smk], writes=[smk])
                else:
                    nlo, nmid, ssum, sg, tt = lo, mid, cnt, step, hi
                    for it in range(NIT):
                        f = 2.0 ** (-(it + 1))
                        P.add("act", lambda g, f=f, nmid=nmid, w0=w0, nlo=nlo: g.activation(
                            out=nmid, in_=w0, func=AF.Identity, scale=-f, bias=nlo), reads=[smk], writes=[smk])
                        P.add("act", lambda g, sc=sc, nk=nk, nmid=nmid, ssum=ssum: g.activation(
                            out=JUNKA[:, :nk], in_=sc[:, :nk], func=AF.Sign, scale=1.0, bias=nmid, accum_out=ssum),
                              reads=[sck, smk], writes=["JUNKA", smk])
                        P.add("act", lambda g, sg=sg, ssum=ssum, nk=nk: g.activation(
                            out=sg, in_=ssum, func=AF.Sign, scale=1.0, bias=SGB[:, (nk // 256) - 1:(nk // 256)]),
                              reads=[smk, "SGB"], writes=[smk])
                        P.add("act", lambda g, f=f, tt=tt, sg=sg, it=it: g.activation(
                            out=tt, in_=sg, func=AF.Identity, scale=-f / 2, bias=FB[:, it:it + 1]),
                              reads=[smk, "FB"], writes=[smk])
                        P.add("act", lambda g, nlo=nlo, w0=w0, tt=tt: g.activation(
                            out=nlo, in_=w0, func=AF.Identity, scale=tt, bias=nlo), reads=[smk], writes=[smk])

        def post(i):
            nk, sc, sck, use_act, smk, lo, w0, mid, cnt, step, hi = ctx(i)
            if use_act and i > 0:
                P.add("dve", lambda g, nlo=lo: g.tensor_scalar(out=nlo, in0=nlo, scalar1=-1.0, scalar2=None,
                                                               op0=ALU.mult), reads=[smk], writes=[smk])
            nbo = NBo[i % 2]
            nbk = "NBo%d" % (i % 2)
            P.add("dve", lambda g, nbo=nbo, sc=sc, nk=nk, lo=lo: g.tensor_scalar(
                out=nbo[:, :nk], in0=sc[:, :nk], scalar1=lo, scalar2=-BIG, op0=ALU.is_lt, op1=ALU.mult),
                  reads=[sck, smk], writes=[nbk])
            P.add("pool", lambda g, nbo=nbo, nk=nk: g.tensor_tensor(out=KSEL[:, :nk], in0=nbo[:, :nk], in1=KPOS[:, :nk],
                                                                   op=ALU.add), reads=[nbk, "KPOS"], writes=["KSEL"])
            P.add("dve", lambda g, nk=nk, i=i: g.tensor_reduce(out=KMS[:, i:i + 1], in_=KSEL[:, :nk],
                                                               axis=mybir.AxisListType.X, op=ALU.max),
                  reads=["KSEL"], writes=["KMS"])
            dma(NBA_d[i][:, :nk], nbo[:, :nk], [nbk], [])

        SGB = sb(st, "SGB", [128, 16], F32)
        FB = sb(st, "FB", [128, NIT], F32)
        for j in range(16):
            P.add("pool", lambda g, j=j: g.memset(SGB[:, j:j + 1], 256.0 * (j + 1) - 511.5), writes=["SGB"])
        for it in range(NIT):
            P.add("pool", lambda g, it=it: g.memset(FB[:, it:it + 1], -(2.0 ** (-(it + 2)))), writes=["FB"])
        for pp in range(8):
            acc(2 * pp)
            acc(2 * pp + 1)
            pre(2 * pp)
            pre(2 * pp + 1)
            chain(2 * pp)
            chain(2 * pp + 1)
            post(2 * pp)
            post(2 * pp + 1)
        P.flush()

    for br in range(2):
        with ExitStack() as st:
            KT = [sb(st, "KTa%d" % i, [128, S], BF16) for i in range(2)]
            VT = [sb(st, "VTa%d" % i, [128, 32, 128], BF16) for i in range(2)]
            QT = [sb(st, "QTa%d" % i, [128, 2048], BF16) for i in range(2)]
            SROW = [sb(st, "SROW%d" % i, [128, 512], F32) for i in range(2)]
            PT = [sb(st, "PT%d" % i, [128, 512], BF16) for i in range(4)]
            RS = [sb(st, "RS%d" % i, [128, 512], F32) for i in range(2)]
            OTS = [sb(st, "OTS%d" % i, [128, 512], BF16) for i in range(2)]
            NB4 = [sb(st, "NB4%d" % i, [128, 4, S], BF16) for i in range(2)] if br == 0 else None
            srow_r, ots_r = Rot([0, 1]), Rot([0, 1])
            pt_r, rs_r, nb_r = Rot([0, 1, 2, 3]), Rot([0, 1]), Rot([0, 1])
            sps = Rot([0, 1, 2])
            ops = Rot([3, 4, 5])
            BK = AB if br == 0 else CNtok
            nb_map = {}

            def nb_load(idx):
                qc_ = idx % 4
                n4_ = nb_r.next()
                nb_map[idx] = n4_
                for t in range(4):
                    nk = 128 * (2 * (4 * qc_ + t) + 2)
                    dma(NB4[n4_][:, t, :nk], NBA_d[4 * qc_ + t][:, :nk], [], [("NB4", n4_, t)], eng="act")

            for i in range(2):
                P.add("pool", lambda g, i=i: g.memset(KT[i][64:128, :], 0.0), writes=["KTa%d" % i])
                P.add("pool", lambda g, i=i: g.memset(KT[i][64:65, :], 1.0), writes=["KTa%d" % i])
                P.add("pool", lambda g, i=i: g.memset(QT[i][64:128, :], 0.0), writes=["QTa%d" % i])
                P.add("pool", lambda g, i=i: g.memset(VT[i][:, :, 64:128], 0.0), writes=["VTa%d" % i])
                P.add("pool", lambda g, i=i: g.memset(VT[i][:, :, 64:65], 1.0), writes=["VTa%d" % i])
            for h in range(8):
                pr, hh = h // 2, h % 2
                r0 = 64 * hh
                kt, vt, qt = KT[h % 2], VT[h % 2], QT[h % 2]
                kk, vk, qk = "KTa%d" % (h % 2), "VTa%d" % (h % 2), "QTa%d" % (h % 2)
                dma(kt[0:64, :], KT_d[br * 4 + pr][r0:r0 + 64, :], [], [kk])
                dma(vt[:, :, 0:64], VV_d[br].rearrange("(t p) c -> p t c", p=128)[:, :, h * 64:(h + 1) * 64], [], [vk])
                dma(qt[0:64, :], QT_d[br * 4 + pr][r0:r0 + 64, :], [], [qk])
                for qc in range(4):
                    if br == 0:
                        sl = 2.0 ** (-(h + 1))
                        for t in range(4):
                            P.add("pe", lambda g, t=t, qc=qc: g.matmul(
                                PS[7][64:65, t * 128:(t + 1) * 128], KMS[:, 4 * qc + t:4 * qc + t + 1], ident_f[:, :],
                                start=True, stop=True), reads=["KMS"], writes=["ps7"])
                        P.add("act", lambda g, qc=qc, sl=sl, qt=qt: g.activation(
                            out=qt[64:65, qc * 512:(qc + 1) * 512], in_=PS[7][64:65, :], func=AF.Copy,
                            scale=-8.0 * sl), reads=["ps7"], writes=[qk])
                    else:
                        P.add("pe", lambda g, h=h, qc=qc: g.matmul(PS[7][64:65, :], ident_f[:8, h:h + 1],
                                                                    CNown[:, qc * 512:(qc + 1) * 512],
                                                                    start=True, stop=True),
                              reads=["CNown"], writes=["ps7"])
                        P.add("act", lambda g, qc=qc, qt=qt: g.activation(
                            out=qt[64:65, qc * 512:(qc + 1) * 512], in_=PS[7][64:65, :], func=AF.Copy, scale=-8.0),
                              reads=["ps7"], writes=[qk])
                for qc in range(4):
                    nkt = 8 * qc + 8
                    n4 = None
                    if br == 0:
                        if h == 0 and qc == 0:
                            nb_load(0)
                        n4 = nb_map[h * 4 + qc]
                        if h * 4 + qc + 1 < 32:
                            nb_load(h * 4 + qc + 1)
                    oa = ops.next()
                    ptmap = {}

                    def sgrp(kti, h=h, qc=qc, n4=n4, kt=kt, qt=qt, kk=kk, qk=qk):
                        s = sps.next()
                        j = kti - 8 * qc
                        extra = []
                        if j >= 0:
                            extra.append(("cm", j))
                        if br == 0:
                            for t in range(4):
                                if kti <= 2 * (4 * qc + t) + 1:
                                    extra.append(("nb", t))
                        mm(PS[s][:], kt[:, kti * 128:(kti + 1) * 128], qt[:, qc * 512:(qc + 1) * 512], True,
                           len(extra) == 0, [kk, qk], ["ps%d" % s])
                        for ei, ex in enumerate(extra):
                            lastf = ei == len(extra) - 1
                            if ex[0] == "cm":
                                mm(PS[s][:], ident_bf[:], CM[:, ex[1], :], False, lastf, ["CM"], ["ps%d" % s])
                            else:
                                t = ex[1]
                                mm(PS[s][:, t * 128:(t + 1) * 128], NB4[n4][:, t, kti * 128:(kti + 1) * 128],
                                   ident_bf[:], False, lastf, [("NB4", n4, t)], ["ps%d" % s])
                        pt = pt_r.next()
                        ptmap[kti] = pt
                        P.add("act", lambda g, s=s, pt=pt, kti=kti, h=h: g.activation(
                            out=PT[pt][:], in_=PS[s][:], func=AF.Exp, bias=BK[:, kti, h:h + 1], scale=0.125),
                              reads=["ps%d" % s, "CNtok"], writes=["PT%d" % pt])

                    def pvg(kti, vt=vt, vk=vk, oa=oa, nkt=nkt):
                        pt = ptmap[kti]
                        mm(PS[oa][:], vt[:, kti, :], PT[pt][:], kti == 0, kti == nkt - 1,
                           [vk, "PT%d" % pt], ["ps%d" % oa])

                    LA = 2
                    for kti in range(min(LA, nkt)):
                        sgrp(kti)
                    for kti in range(nkt):
                        if kti + LA < nkt:
                            sgrp(kti + LA)
                        pvg(kti)
                    sr = srow_r.next()
                    r = rs_r.next()
                    P.add("act", lambda g, sr=sr, oa=oa: g.activation(out=SROW[sr][64:65, :], in_=PS[oa][64:65, :],
                                                                      func=AF.Copy),
                          reads=["ps%d" % oa], writes=["SROW%d" % sr])
                    P.add("dve", lambda g, sr=sr: g.reciprocal(out=SROW[sr][64:65, :], in_=SROW[sr][64:65, :]),
                          reads=["SROW%d" % sr], writes=["SROW%d" % sr])
                    P.add("pe", lambda g, sr=sr: g.matmul(PS[6][:64, :], ONESF[64:65, :], SROW[sr][64:65, :],
                                                          start=True, stop=True),
                          reads=["SROW%d" % sr], writes=["ps6"])
                    P.add("act", lambda g, r=r: g.activation(out=RS[r][:64, :], in_=PS[6][:64, :], func=AF.Copy),
                          reads=["ps6"], writes=["RS%d" % r])
                    o = ots_r.next()
                    P.add("dve", lambda g, r=r, oa=oa, o=o: g.tensor_tensor(
                        out=OTS[o][:64, :], in0=PS[oa][:64, :], in1=RS[r][:64, :], op=ALU.mult),
                          reads=["ps%d" % oa, "RS%d" % r], writes=["OTS%d" % o])
                    dma(OT_d[br, pr][r0:r0 + 64, qc * 512:(qc + 1) * 512], OTS[o][:64, :],
                        ["OTS%d" % o], [])
            P.flush()

    with ExitStack() as st:
        NSET = 4
        KT = [sb(st, "KTc%d" % i, [128, S], BF16) for i in range(1)] * 2
        VT = [sb(st, "VTc%d" % i, [128, 32, 128], BF16) for i in range(2)]
        QT = [sb(st, "QTc%d" % i, [128, 2048], BF16) for i in range(1)] * 2
        NSG = 3
        SG = [sb(st, "SG%d" % i, [128, S], F32) for i in range(NSG)]
        INC = [sb(st, "INC%d" % i, [128, S + 1], F32) for i in range(NSET)]
        AA = [sb(st, "AA%d" % i, [128, S], BF16) for i in range(NSET)]
        ATS = [sb(st, "ATS%d" % i, [128, 512], BF16) for i in range(3)]
        OTS = [sb(st, "OTSc%d" % i, [128, 128], BF16) for i in range(2)]
        ots_r = Rot([0, 1])
        zr, at_r, ats_r, oc_r = Rot([0, 1, 2]), Rot([3, 4]), Rot([0, 1, 2]), Rot([5, 6])
        items = [(pr, hh, i) for pr in range(4) for hh in range(2) for i in range(16)]

        def stage_a(n):
            pr, hh, i = items[n]
            kt, qt = KT[pr % 2], QT[pr % 2]
            kk, vk, qk = "KTc0", "VTc%d" % (pr % 2), "QTc0"
            if hh == 0 and i == 0:
                dma(kt[:], KT_d[8 + pr], [], [kk])
                dma(VT[pr % 2][:], VV_d[2].rearrange("(t p) c -> p t c", p=128)[:, :, pr * 128:(pr + 1) * 128], [], [vk])
                dma(qt[:], QT_d[8 + pr], [], [qk])
            r0 = 64 * hh
            nk = 128 * (2 * i + 2)
            sg, inc, aa = SG[n % NSG], INC[n % NSET], AA[n % NSET]
            sgk, inck, aak = "SG%d" % (n % NSG), "INC%d" % (n % NSET), "AA%d" % (n % NSET)
            for kc in range((nk + 511) // 512):
                k0 = kc * 512
                kn = min(512, nk - k0)
                b = zr.next()
                mm(PS[b][:, :kn], qt[r0:r0 + 64, i * 128:(i + 1) * 128], kt[r0:r0 + 64, k0:k0 + kn],
                   True, True, [kk, qk], ["ps%d" % b])
                P.add("act", lambda g, b=b, kn=kn, k0=k0, sg=sg: g.activation(
                    out=sg[:, k0:k0 + kn], in_=PS[b][:, :kn], func=AF.Sigmoid, scale=-0.125),
                      reads=["ps%d" % b], writes=[sgk])
            P.add("dve", lambda g, sg=sg, i=i: g.tensor_tensor(out=sg[:, 256 * i:256 * i + 256],
                                                                in0=sg[:, 256 * i:256 * i + 256], in1=M1[:],
                                                                op=ALU.max), reads=[sgk], writes=[sgk])
            P.add("pool", lambda g, inc=inc, nk=nk: g.memset(inc[:, nk:nk + 1], 1.0), writes=[inck])
            P.add("dve", lambda g, inc=inc, sg=sg, nk=nk: g.tensor_tensor_scan(
                out=inc[:, 0:nk][:, ::-1], data0=sg[:, 0:nk][:, ::-1],
                data1=sg[:, 0:nk][:, ::-1], initial=1.0, op0=ALU.mult, op1=ALU.bypass),
                  reads=[sgk, inck], writes=[inck])
            P.add("pool", lambda g, aa=aa, inc=inc, nk=nk: g.tensor_tensor(
                out=aa[:, :nk], in0=inc[:, 1:nk + 1], in1=inc[:, 0:nk], op=ALU.subtract),
                  reads=[inck], writes=[aak])

        def stage_b(n):
            pr, hh, i = items[n]
            vt = VT[pr % 2]
            vk = "VTc%d" % (pr % 2)
            r0 = 64 * hh
            aa = AA[n % NSET]
            aak = "AA%d" % (n % NSET)
            oc = oc_r.next()
            nkt = 2 * i + 2
            ng = (nkt + 3) // 4
            st_ = {}

            def tr(g4):
                n4 = min(4, nkt - 4 * g4)
                a = at_r.next()
                apv = PS[a][:].bitcast(BF16)
                for u in range(n4):
                    kti = 4 * g4 + u
                    P.add("pe", lambda g, apv=apv, u=u, kti=kti: g.transpose(
                        apv[:, u * 128:(u + 1) * 128], aa[:, kti * 128:(kti + 1) * 128], ident_bf[:]),
                          reads=[aak], writes=["ps%d" % a])
                s_ = ats_r.next()
                if True:
                    P.add("act", lambda g, s_=s_, apv=apv, n4=n4: g.activation(
                        out=ATS[s_][:, :n4 * 128], in_=apv[:, :n4 * 128], func=AF.Copy),
                          reads=["ps%d" % a], writes=["ATS%d" % s_])
                else:
                    P.add("dve", lambda g, s_=s_, apv=apv, n4=n4: g.tensor_copy(
                        out=ATS[s_][:, :n4 * 128], in_=apv[:, :n4 * 128]),
                          reads=["ps%d" % a], writes=["ATS%d" % s_])
                st_[g4] = (s_, n4)

            def pv(g4):
                s_, n4 = st_[g4]
                for u in range(n4):
                    kti = 4 * g4 + u
                    mm(PS[oc][:, :128], vt[:, kti, :], ATS[s_][:, u * 128:(u + 1) * 128], kti == 0,
                       kti == nkt - 1, [vk, "ATS%d" % s_], ["ps%d" % oc])

            tr(0)
            for g4 in range(ng):
                if g4 + 1 < ng:
                    tr(g4 + 1)
                pv(g4)
            o = ots_r.next()
            P.add("act", lambda g, oc=oc, r0=r0, o=o: g.activation(
                out=OTS[o][r0:r0 + 64, :], in_=PS[oc][r0:r0 + 64, :128], func=AF.Copy),
                  reads=["ps%d" % oc], writes=["OTSc%d" % o])
            dma(OT_d[2, pr][r0:r0 + 64, i * 128:(i + 1) * 128], OTS[o][r0:r0 + 64, :], ["OTSc%d" % o], [],
                eng="act")

        NI = len(items)
        DEPTH_A = NSET - 1
        for n in range(DEPTH_A):
            stage_a(n)
        for n in range(NI):
            stage_b(n)
            if n + DEPTH_A < NI:
                stage_a(n + DEPTH_A)
        P.flush()

    with ExitStack() as st:
        STG = [sb(st, "tstg%d" % i, [128, 8, 512], F32) for i in range(2)]
        WB = [sb(st, "twb%d" % i, [128, 8, 512], BF16) for i in range(3)]
        OTc = [sb(st, "OTc%d" % i, [128, 4, 512], BF16) for i in range(3)]
        GT = [sb(st, "GT%d" % i, [128, 512], F32) for i in range(3)]
        MF = sb(st, "MF", [128, 512], F32)
        MT = sb(st, "MT", [128, 8, 512], BF16)
        XO = sb(st, "XO", [128, 4, D], F32)
        X1 = sb(st, "X1", [128, 4, D], F32)
        X1T = sb(st, "X1T", [128, 8, 512], BF16)
        HR = [sb(st, "HR%d" % i, [128, 512], F32) for i in range(2)]
        HT = sb(st, "HT", [128, 32, 512], BF16)
        XN = sb(st, "XN", [128, D], F32)
        ST6 = sb(st, "ST6", [128, 2, 6], F32)
        MV = sb(st, "MV", [128, 4], F32)
        LNP = sb(st, "LNP", [128, 4, D], F32)
        stg_r, wb_r, gt_r, hr_r = Rot([0, 1]), Rot([0, 1, 2]), Rot([0, 1, 2]), Rot([0, 1])
        for n, src in enumerate((ln1g_d, ln1b_d, ln2g_d, ln2b_d)):
            dma(LNP[:, n, :], src[layer].partition_broadcast(128), [], ["LNP"])

        cast_flip = [0]

        def load_wt(src3, kc, n):
            i = stg_r.next()
            j = wb_r.next()
            dma(STG[i][:, :kc, :n], src3, [], ["tstg%d" % i])
            cast_flip[0] ^= 1
            if cast_flip[0]:
                P.add("dve", lambda g: g.tensor_copy(out=WB[j][:, :kc, :n], in_=STG[i][:, :kc, :n]),
                      reads=["tstg%d" % i], writes=["twb%d" % j])
            else:
                P.add("act", lambda g: g.activation(out=WB[j][:, :kc, :n], in_=STG[i][:, :kc, :n], func=AF.Copy),
                      reads=["tstg%d" % i], writes=["twb%d" % j])
            return j

        def layer_norm(src, dst, gi, key_src, key_dst):
            for hf in range(2):
                P.add("dve", lambda g, hf=hf: g.bn_stats(out=ST6[:, hf, :], in_=src[:, hf * 512:(hf + 1) * 512]),
                      reads=[key_src], writes=["ST6"])
            P.add("dve", lambda g: g.bn_aggr(out=MV[:, 0:2], in_=ST6[:].rearrange("p a b -> p (a b)")),
                  reads=["ST6"], writes=["MV"])
            P.add("act", lambda g: g.activation(out=MV[:, 2:3], in_=MV[:, 1:2], func=AF.Sqrt, bias=EPSB[:, 0:1],
                                                scale=1.0), reads=["MV", "EPSB"], writes=["MV"])
            P.add("dve", lambda g: g.reciprocal(out=MV[:, 3:4], in_=MV[:, 2:3]), reads=["MV"], writes=["MV"])
            P.add("dve", lambda g: g.tensor_scalar(out=XN[:], in0=src, scalar1=MV[:, 0:1], scalar2=MV[:, 3:4],
                                                   op0=ALU.subtract, op1=ALU.mult),
                  reads=[key_src, "MV"], writes=["XN"])
            P.add("dve", lambda g: g.tensor_tensor(out=XN[:], in0=XN[:], in1=LNP[:, gi, :], op=ALU.mult),
                  reads=["XN", "LNP"], writes=["XN"])
            P.add("dve", lambda g: g.tensor_tensor(out=dst, in0=XN[:], in1=LNP[:, gi + 1, :], op=ALU.add),
                  reads=["XN", "LNP"], writes=[key_dst])

        EPSB = sb(st, "EPSB", [128, 1], F32)
        P.add("pool", lambda g: g.memset(EPSB[:], EPS), writes=["EPSB"])
        wbr = wbr_d[layer]
        wout = wout_d[layer].rearrange("(c p) n -> p c n", p=128)
        wff1 = wff1_d[layer].rearrange("(c p) n -> p c n", p=128)
        wff2 = wff2_d[layer].rearrange("(c p) n -> p c n", p=128)
        for tcx in range(4):
            t0 = tcx * 512
            if layer == 0:
                dma(XO[:], xo_d[t0:t0 + 512, :].rearrange("(t p) d -> p t d", p=128), [], ["XO"])
            else:
                dma(XO[:], X1own_t[tcx].ap().rearrange("(t p) d -> p t d", p=128), ["X1own%d" % tcx], ["XO"])
            for bi in range(3):
                dma(OTc[bi][:], OT_d[bi].rearrange("w p t -> p w t")[:, :, t0:t0 + 512], [], [("OTc", bi)])
            for dh in range(2):
                wj = [load_wt(wbr[bi].rearrange("(c p) n -> p c n", p=128)[:, :, dh * 512:(dh + 1) * 512], 4, 512)
                      for bi in range(3)]
                for dq in range(4):
                    dc = dh * 4 + dq
                    for bi in range(3):
                        gt = gt_r.next()
                        dma(GT[gt][:], G_d[bi * 8 + dc][:, t0:t0 + 512], [], ["GT%d" % gt])
                        b = psr.next()
                        for wc in range(4):
                            mm(PS[b][:], WB[wj[bi]][:, wc, dq * 128:(dq + 1) * 128], OTc[bi][:, wc, :],
                               wc == 0, wc == 3, ["twb%d" % wj[bi], ("OTc", bi)], ["ps%d" % b])
                        if bi == 0:
                            P.add("dve", lambda g, b=b, gt=gt: g.tensor_tensor(out=MF[:], in0=PS[b][:], in1=GT[gt][:],
                                                                               op=ALU.mult),
                                  reads=["ps%d" % b, "GT%d" % gt], writes=["MF"])
                        else:
                            P.add("dve", lambda g, b=b, gt=gt: g.tensor_tensor(out=GT[gt][:], in0=PS[b][:],
                                                                               in1=GT[gt][:], op=ALU.mult),
                                  reads=["ps%d" % b, "GT%d" % gt], writes=["GT%d" % gt])
                            if bi == 1:
                                P.add("pool", lambda g, gt=gt: g.tensor_tensor(out=MF[:], in0=MF[:], in1=GT[gt][:],
                                                                               op=ALU.add),
                                      reads=["MF", "GT%d" % gt], writes=["MF"])
                            else:
                                P.add("pool", lambda g, gt=gt, dc=dc: g.tensor_tensor(out=MT[:, dc, :], in0=MF[:],
                                                                                      in1=GT[gt][:], op=ALU.add),
                                      reads=["MF", "GT%d" % gt], writes=["MT"])
            wo = [load_wt(wout[:, :, hf * 512:(hf + 1) * 512], 8, 512) for hf in range(2)]
            for t in range(4):
                for hf in range(2):
                    b = psr.next()
                    for dc in range(8):
                        mm(PS[b][:], MT[:, dc, t * 128:(t + 1) * 128], WB[wo[hf]][:, dc, :], dc == 0, dc == 7,
                           ["MT", "twb%d" % wo[hf]], ["ps%d" % b])
                    P.add("dve", lambda g, b=b, t=t, hf=hf: g.scalar_tensor_tensor(
                        out=X1[:, t, hf * 512:(hf + 1) * 512], in0=XO[:, t, hf * 512:(hf + 1) * 512], scalar=ALPHA,
                        in1=PS[b][:], op0=ALU.mult, op1=ALU.add), reads=["ps%d" % b, "XO"], writes=[("X1", t)])
            for t in range(4):
                layer_norm(X1[:, t, :], X1[:, t, :], 0, ("X1", t), ("X1", t))
            for t in range(4):
                for dc in range(8):
                    if dc % 4 == 0:
                        b = psr.next()
                    P.add("pe", lambda g, b=b, t=t, dc=dc: g.transpose(PS[b][:, (dc % 4) * 128:(dc % 4 + 1) * 128],
                                                                       X1[:, t, dc * 128:(dc + 1) * 128], ident_f[:]),
                          reads=[("X1", t)], writes=["ps%d" % b])
                    if dc % 4 == 3:
                        d0 = dc - 3
                        P.add("act", lambda g, b=b, t=t, d0=d0: g.activation(
                            out=X1T[:, d0:d0 + 4, t * 128:(t + 1) * 128],
                            in_=PS[b][:].rearrange("p (c q) -> p c q", q=128), func=AF.Copy),
                              reads=["ps%d" % b], writes=["X1T"])
            for ft in range(8):
                j = load_wt(wff1[:, :, ft * 512:(ft + 1) * 512], 8, 512)
                for fq in range(4):
                    fc = ft * 4 + fq
                    b = psr.next()
                    for dc in range(8):
                        mm(PS[b][:], WB[j][:, dc, fq * 128:(fq + 1) * 128], X1T[:, dc, :], dc == 0, dc == 7,
                           ["twb%d" % j, "X1T"], ["ps%d" % b])
                    hr = hr_r.next()
                    P.add("act", lambda g, b=b, hr=hr: g.activation(out=HR[hr][:], in_=PS[b][:], func=AF.Relu),
                          reads=["ps%d" % b], writes=["HR%d" % hr])
                    P.add("dve", lambda g, hr=hr, fc=fc: g.tensor_tensor(out=HT[:, fc, :], in0=HR[hr][:],
                                                                          in1=HR[hr][:], op=ALU.mult),
                          reads=["HR%d" % hr], writes=["HT"])
            ybanks = [psr.next() for _ in range(8)]
            for kg in range(4):
                for hf in range(2):
                    j = load_wt(wff2[:, kg * 8:(kg + 1) * 8, hf * 512:(hf + 1) * 512], 8, 512)
                    for t in range(4):
                        b = ybanks[t * 2 + hf]
                        for k8 in range(8):
                            fc = kg * 8 + k8
                            mm(PS[b][:], HT[:, fc, t * 128:(t + 1) * 128], WB[j][:, k8, :], fc == 0, fc == 31,
                               ["HT", "twb%d" % j], ["ps%d" % b])
            for t in range(4):
                for hf in range(2):
                    b = ybanks[t * 2 + hf]
                    P.add("dve", lambda g, b=b, t=t, hf=hf: g.scalar_tensor_tensor(
                        out=XO[:, t, hf * 512:(hf + 1) * 512], in0=X1[:, t, hf * 512:(hf + 1) * 512], scalar=ALPHA,
                        in1=PS[b][:], op0=ALU.mult, op1=ALU.add), reads=["ps%d" % b, ("X1", t)], writes=["XO"])
                layer_norm(XO[:, t, :], XO[:, t, :], 2, "XO", "XO")
            if is_last:
                dma(out_d[t0:t0 + 512, :].rearrange("(t p) d -> p t d", p=128), XO[:], ["XO"], [])
            else:
                dma(X1own_t[tcx].ap().rearrange("(t p) d -> p t d", p=128), XO[:], ["XO"], ["X1own%d" % tcx])
                P.add("pool", lambda g, tcx=tcx: g.collective_compute(
                    "AllGather", ALU.bypass, replica_groups=[[0, 1], [2, 3], [4, 5], [6, 7]],
                    ins=[X1own_t[tcx].ap().opt()], outs=[X1all_t[tcx].ap().opt()]),
                      reads=["X1own%d" % tcx], writes=["X1all%d" % tcx], cc=True)
        P.flush()


_NC_CACHE = {}


def _get_nc(n_layers):
    if n_layers not in _NC_CACHE:
        _NC_CACHE[n_layers] = build_program(n_layers)
    return _NC_CACHE[n_layers]


def _own_tiles(x_b, p):
    return np.ascontiguousarray(x_b.reshape(16, 2, 128, D)[:, p].reshape(2048, D))


def _run(xs, weights, n_layers):
    in_maps = []
    for c in range(8):
        b, p = c // 2, c % 2
        pfv = np.zeros((128, 4), np.float32)
        pfv[:, 0] = p
        pfv[:, 1] = 1 - p
        pfv[:, 2] = 128 * p
        m = {"xa": np.ascontiguousarray(xs[b]), "xo": _own_tiles(xs[b], p), "pf": pfv}
        for k, v in weights.items():
            a = v[:n_layers]
            if k == "b_forget":
                a = a.reshape(n_layers, 8, 1)
            m[k] = np.ascontiguousarray(a)
        in_maps.append(m)
    nc = _get_nc(n_layers)
    res = run_bass_kernel_spmd(nc, in_maps, core_ids=list(range(8)))
    out = np.empty((NB, S, D), np.float32)
    for c in range(8):
        b, p = c // 2, c % 2
        out[b].reshape(16, 2, 128, D)[:, p] = res.results[c]["out"].reshape(16, 128, D)
    return out


def kernel(x, w_in, b_forget, w_branch, w_out, ln1_g, ln1_b, w_ff1, w_ff2, ln2_g, ln2_b):
    weights = dict(w_in=np.asarray(w_in, np.float32), b_forget=np.asarray(b_forget, np.float32),
                   w_branch=np.asarray(w_branch, np.float32), w_out=np.asarray(w_out, np.float32),
                   ln1_g=np.asarray(ln1_g, np.float32), ln1_b=np.asarray(ln1_b, np.float32),
                   w_ff1=np.asarray(w_ff1, np.float32), w_ff2=np.asarray(w_ff2, np.float32),
                   ln2_g=np.asarray(ln2_g, np.float32), ln2_b=np.asarray(ln2_b, np.float32))
    return _run(np.asarray(x, np.float32), weights, DEPTH)
```

```python
from contextlib import ExitStack

import numpy as np
import concourse.bass as bass
import concourse.mybir as mybir
from concourse.bass_utils import run_bass_kernel_spmd

F32 = mybir.dt.float32
BF16 = mybir.dt.bfloat16
AF = mybir.ActivationFunctionType
ALU = mybir.AluOpType

D = 1024
S = 4096
NB = 4
DEPTH = 2
DFF = 4096
NCOLS = 8792
ALPHA = (2.0 * DEPTH) ** 0.25
EPS = 1e-5
BIG = 30000.0
NIT = 24
OFF = dict(qa=0, ka=512, va=1024, qi=1536, ki=2560, wi=2624, qf=2640, kf=3152, vf=3664,
           fg=4176, qs=4184, ks=4696, vs=5208, g=5720)
ENGS = ("pe", "act", "dve", "pool", "sp")
ND = 40


class Op:
    __slots__ = ("eng", "fn", "deps", "signal", "sigval", "dsem", "dval", "dma")


class Prog:
    def __init__(self, nc, stack):
        self.nc = nc
        self.ops = {e: [] for e in ENGS}
        self.writers = {}
        self.readers = {}
        self.dma_ops = []
        self.sem = {e: stack.enter_context(nc.semaphore("s_" + e)) for e in ENGS}
        self.dsem = [stack.enter_context(nc.semaphore("d%d" % i)) for i in range(ND)]
        self.ccsem = stack.enter_context(nc.semaphore("ccsem"))
        self.cc_ops = []
        self.count = {e: 0 for e in ENGS}
        self.known = {e: {} for e in ENGS}
        self.last = {e: None for e in ENGS}
        self.nblk = 0

    def add(self, eng, fn, reads=(), writes=(), extra=(), dma=False, force_signal=False, cc=False):
        op = Op()
        op.eng, op.fn, op.dma = eng, fn, dma or cc
        op.signal = force_signal
        op.sigval = op.dsem = op.dval = None
        deps = set(extra)
        for k in reads:
            deps.update(self.writers.get(k, {}).values())
        for k in writes:
            deps.update(self.writers.get(k, {}).values())
            deps.update(self.readers.get(k, {}).values())
        if cc:
            self.cc_ops.append(op)
            op.dsem = "cc"
            op.dval = len(self.cc_ops)
        if dma:
            n = len(self.dma_ops)
            op.dsem = n % ND
            op.dval = 16 * (n // ND + 1)
            if n >= ND:
                deps.add(self.dma_ops[n - ND])
            self.dma_ops.append(op)
        deps.discard(op)
        if eng == "pe":
            deps = {d for d in deps if d.eng != "pe"}
        op.deps = deps
        for d in deps:
            d.signal = True
        for k in writes:
            self.writers[k] = {eng: op}
            self.readers[k] = {}
        for k in reads:
            self.readers.setdefault(k, {})[eng] = op
        self.ops[eng].append(op)
        self.last[eng] = op
        return op

    def barrier(self):
        lasts = [self.last[e] for e in ENGS if e != "sp" and self.last[e] is not None]
        lasts += self.dma_ops[-ND:]
        lasts += self.cc_ops[-4:]
        for e in ENGS:
            self.add(e, lambda g: g.nop(), extra=lasts, force_signal=True)
        self.writers = {}
        self.readers = {}

    def flush(self):
        self.barrier()
        nc = self.nc
        engobj = {"pe": "tensor", "act": "scalar", "dve": "vector", "pool": "gpsimd", "sp": "sync"}

        def emit(e, g):
            known = self.known[e]
            for op in self.ops[e]:
                need = {}
                for d in op.deps:
                    if d.dma:
                        key, val = ("d", d.dsem), d.dval
                    else:
                        key, val = d.eng, d.sigval
                    if need.get(key, 0) < val:
                        need[key] = val
                for key, val in need.items():
                    if known.get(key, 0) < val:
                        if isinstance(key, tuple):
                            s = self.ccsem if key[1] == "cc" else self.dsem[key[1]]
                        else:
                            s = self.sem[key]
                        g.wait_ge(s, val)
                        known[key] = val
                ins = op.fn(g)
                if op.dsem == "cc":
                    ins.then_inc(self.ccsem, 1)
                elif op.dma:
                    ins.then_inc(self.dsem[op.dsem], 16)
                elif op.signal:
                    ins.then_inc(self.sem[e], 1)
            self.ops[e] = []

        for e in ENGS:
            for op in self.ops[e]:
                if op.signal and not op.dma:
                    self.count[e] += 1
                    op.sigval = self.count[e]
        with nc.Block() as block:
            @block.tensor
            def _(g):
                emit("pe", g)

            @block.scalar
            def _(g):
                emit("act", g)

            @block.vector
            def _(g):
                emit("dve", g)

            @block.gpsimd
            def _(g):
                emit("pool", g)

            @block.sync
            def _(g):
                emit("sp", g)

    def finish(self):
        lasts = self.dma_ops[-ND:]
        self.add("sp", lambda g: g.nop(), extra=lasts)
        self.flush()


class Rot:
    def __init__(self, tiles):
        self.tiles = tiles
        self.i = 0

    def next(self):
        t = self.tiles[self.i % len(self.tiles)]
        self.i += 1
        return t


def build_program(n_layers=1, debug=False):
    nc = bass.Bass("TRN2", target_bir_lowering=False)
    stack = ExitStack()
    with stack:
        _build(nc, stack, n_layers, debug)
    return nc


def _build(nc, stack, n_layers, debug):
    L = n_layers

    def din(name, shape, dt=F32):
        return nc.dram_tensor(name, shape, dt, kind="ExternalInput").ap()

    def dscr(name, shape, dt):
        kind = "ExternalOutput" if debug else "Internal"
        return nc.dram_tensor(name, shape, dt, kind=kind).ap()

    xa_d = din("xa", [S, D])
    X1own_t = [nc.dram_tensor("X1own%d" % i, [512, D], F32) for i in range(4)]
    X1all_t = [nc.dram_tensor("X1all%d" % i, [1024, D], F32) for i in range(4)]
    xo_d = din("xo", [2048, D])
    pf_d = din("pf", [128, 4])
    w_in_d = din("w_in", [L, D, NCOLS])
    bfg_d = din("b_forget", [L, 8, 1])
    wbr_d = din("w_branch", [L, 3, 512, D])
    wout_d = din("w_out", [L, D, D])
    ln1g_d = din("ln1_g", [L, D])
    ln1b_d = din("ln1_b", [L, D])
    wff1_d = din("w_ff1", [L, D, DFF])
    wff2_d = din("w_ff2", [L, DFF, D])
    ln2g_d = din("ln2_g", [L, D])
    ln2b_d = din("ln2_b", [L, D])
    out_d = nc.dram_tensor("out", [2048, D], F32, kind="ExternalOutput").ap()

    QT_d = dscr("QT", [12, 128, 2048], BF16)
    KT_d = dscr("KT", [12, 128, S], BF16)
    VV_d = dscr("VV", [3, S, 512], BF16)
    QI_d = dscr("QI", [8, 128, 2048], BF16)
    KI_d = dscr("KI", [128, S], BF16)
    G_d = dscr("G", [24, 128, 2048], F32)
    NBA_d = dscr("NBA", [16, 128, S], BF16)
    OT_d = dscr("OTd", [3, 4, 128, 2048], BF16)

    P = Prog(nc, stack)

    uniq = [0]

    def sb(st, name, shape, dt):
        uniq[0] += 1
        return st.enter_context(nc.sbuf_tensor("t%d_%s" % (uniq[0], name), shape, dt))

    def ps(st, name, shape, dt=F32):
        uniq[0] += 1
        return st.enter_context(nc.psum_tensor("t%d_%s" % (uniq[0], name), shape, dt))

    ident_bf = sb(stack, "ident_bf", [128, 128], BF16)
    ident_f = sb(stack, "ident_f", [128, 128], F32)
    ones_bf = sb(stack, "ones_bf", [128, 128], BF16)
    ONESF = sb(stack, "ONESF", [128, 64], F32)
    pf = sb(stack, "pf", [128, 4], F32)
    CM = sb(stack, "CM", [128, 8, 512], BF16)
    NBQK = sb(stack, "NBQK", [128, 256], F32)
    M1 = sb(stack, "M1", [128, 256], F32)
    AB = sb(stack, "AB", [128, 32, 8], F32)
    WI = sb(stack, "WI", [128, 16, 16], F32)
    KMS = sb(stack, "KMS", [128, 16], F32)
    CNtok = sb(stack, "CNtok", [128, 32, 8], F32)
    CNown = sb(stack, "CNown", [8, 2048], F32)
    PS = [ps(stack, "ps%d" % i, [128, 512]) for i in range(8)]

    with ExitStack() as st:
        onesf = sb(st, "onesf", [128, 128], F32)
        V0 = sb(st, "V0", [128, 8, 512], F32)
        V2 = sb(st, "V2", [128, 256], F32)
        KP = sb(st, "KP", [128, 32], F32)
        P.add("sp", lambda g: g.dma_start(out=pf[:], in_=pf_d[:, :]), writes=["pf"], dma=True)
        P.add("pool", lambda g: g.memset(onesf[:], 1.0), writes=["onesf"])
        P.add("pool", lambda g: g.memset(ones_bf[:], 1.0), writes=["ones_bf"])
        P.add("pool", lambda g: g.memset(ONESF[:], 1.0), writes=["ONESF"])
        P.add("pool", lambda g: g.affine_select(out=ident_f[:], in_=onesf[:], pattern=[[-1, 128]],
                                                compare_op=ALU.is_equal, fill=0.0, base=0,
                                                channel_multiplier=1),
              reads=["onesf"], writes=["ident_f"])
        P.add("pool", lambda g: g.tensor_copy(out=ident_bf[:], in_=ident_f[:]),
              reads=["ident_f"], writes=["ident_bf"])
        P.add("pool", lambda g: g.iota(V0[:].rearrange("p j (t q) -> p j t q", q=128),
                                       pattern=[[-128, 8], [256, 4], [1, 128]], base=0,
                                       channel_multiplier=-1, allow_small_or_imprecise_dtypes=True),
              writes=["V0"])
        P.add("dve", lambda g: g.tensor_scalar(out=V0[:], in0=V0[:], scalar1=pf[:, 2:3], scalar2=None,
                                               op0=ALU.add), reads=["V0", "pf"], writes=["V0"])
        P.add("dve", lambda g: g.tensor_scalar(out=CM[:], in0=V0[:], scalar1=0.0, scalar2=-BIG,
                                               op0=ALU.is_lt, op1=ALU.mult), reads=["V0"], writes=["CM"])
        P.add("pool", lambda g: g.iota(V2[:].rearrange("p (j k) -> p j k", k=128),
                                       pattern=[[-128, 2], [-1, 128]], base=0,
                                       channel_multiplier=1, allow_small_or_imprecise_dtypes=True),
              writes=["V2"])
        P.add("dve", lambda g: g.tensor_scalar(out=V2[:], in0=V2[:], scalar1=pf[:, 2:3], scalar2=None,
                                               op0=ALU.add), reads=["V2", "pf"], writes=["V2"])
        P.add("dve", lambda g: g.tensor_scalar(out=NBQK[:], in0=V2[:], scalar1=0.0, scalar2=-BIG,
                                               op0=ALU.is_lt, op1=ALU.mult), reads=["V2"], writes=["NBQK"])
        P.add("dve", lambda g: g.tensor_scalar(out=M1[:], in0=V2[:], scalar1=0.0, scalar2=None,
                                               op0=ALU.is_le), reads=["V2"], writes=["M1"])
        P.add("pool", lambda g: g.iota(KP[:], pattern=[[128, 32]], base=0, channel_multiplier=1,
                                       allow_small_or_imprecise_dtypes=True), writes=["KP"])
        for h in range(8):
            sl = 2.0 ** (-(h + 1))
            P.add("dve", lambda g, h=h, sl=sl: g.tensor_scalar(out=AB[:, :, h], in0=KP[:], scalar1=sl,
                                                               scalar2=None, op0=ALU.mult),
                  reads=["KP"], writes=["AB"])
        P.flush()

    for layer in range(L):
        _layer(nc, P, stack, layer, locals(), layer == L - 1)
    P.finish()


def _layer(nc, P, stack, layer, E, is_last):
    xa_d, X1own_t, X1all_t = E["xa_d"], E["X1own_t"], E["X1all_t"]
    xo_d, w_in_d, bfg_d, wbr_d, wout_d = E["xo_d"], E["w_in_d"], E["bfg_d"], E["wbr_d"], E["wout_d"]
    ln1g_d, ln1b_d, wff1_d, wff2_d, ln2g_d, ln2b_d, out_d = (E["ln1g_d"], E["ln1b_d"], E["wff1_d"], E["wff2_d"],
                                                            E["ln2g_d"], E["ln2b_d"], E["out_d"])
    QT_d, KT_d, VV_d, QI_d, KI_d, G_d, NBA_d = E["QT_d"], E["KT_d"], E["VV_d"], E["QI_d"], E["KI_d"], E["G_d"], E["NBA_d"]
    ident_bf, ident_f, ones_bf, pf, CM, NBQK, M1, AB = (E["ident_bf"], E["ident_f"], E["ones_bf"], E["pf"], E["CM"],
                                                        E["NBQK"], E["M1"], E["AB"])
    ONESF = E["ONESF"]
    OT_d, WI, KMS, CNtok, CNown, PS = E["OT_d"], E["WI"], E["KMS"], E["CNtok"], E["CNown"], E["PS"]
    sb, ps = E["sb"], E["ps"]
    w_in = w_in_d[layer].rearrange("(c p) n -> p c n", p=128)

    psr = Rot(list(range(8)))

    def dma(out, in_, reads, writes, eng="sp"):
        return P.add(eng, lambda g: g.dma_start(out=out, in_=in_), reads=reads, writes=writes, dma=True)

    def mm(out, lhsT, rhs, start, stop, reads, writes):
        return P.add("pe", lambda g: g.matmul(out, lhsT, rhs, start=start, stop=stop), reads=reads, writes=writes)

    st_fg = ExitStack()
    FG = sb(st_fg, "FG", [8, S], F32)
    with ExitStack() as st:
        XT = sb(st, "XT", [128, 8, S], BF16)
        XTO = sb(st, "XTO", [128, 8, 2048], BF16)
        STG = [sb(st, "stg%d" % i, [128, 8, 512], F32) for i in range(2)]
        WB = [sb(st, "wb%d" % i, [128, 8, 512], BF16) for i in range(3)]
        OUTB = [sb(st, "outb%d" % i, [128, 512], BF16) for i in range(4)]
        OUTF = [sb(st, "outf%d" % i, [128, 512], F32) for i in range(3)]
        TMPX = sb(st, "tmpx", [128, 2048], BF16)
        stg_r, wb_r, outb_r, outf_r = Rot([0, 1]), Rot([0, 1, 2]), Rot([0, 1, 2, 3]), Rot([0, 1, 2])
        evac_flip = [0]

        def evac_copy(out, in_, reads, writes):
            evac_flip[0] ^= 1
            if evac_flip[0]:
                P.add("act", lambda g: g.activation(out=out, in_=in_, func=AF.Copy), reads=reads, writes=writes)
                return "act"
            P.add("dve", lambda g: g.tensor_copy(out=out, in_=in_), reads=reads, writes=writes)
            return "sp"

        for tc in range(8):
            i = stg_r.next()
            sv = STG[i][:].rearrange("p a b -> p (a b)").rearrange("p (t d) -> p t d", d=D)
            if layer == 0:
                dma(sv, xa_d[tc * 512:(tc + 1) * 512, :].rearrange("(t p) d -> p t d", p=128), [], ["stg%d" % i])
            else:
                for ii in range(2):
                    src = X1all_t[tc // 2].ap().rearrange("(r t p) d -> p t r d", r=2, p=128)[:, 2 * (tc % 2) + ii, :, :]
                    dma(sv[:, 2 * ii:2 * ii + 2, :], src, ["X1all%d" % (tc // 2)], ["stg%d" % i])
            for t in range(4):
                g_ = tc * 4 + t
                for c4 in range(2):
                    b = psr.next()
                    for cc in range(4):
                        c = c4 * 4 + cc
                        P.add("pe", lambda g, b=b, cc=cc, sv=sv, t=t, c=c: g.transpose(
                            PS[b][:, cc * 128:(cc + 1) * 128], sv[:, t, c * 128:(c + 1) * 128], ident_f[:]),
                              reads=["stg%d" % i], writes=["ps%d" % b])
                    evac_copy(XT[:, c4 * 4:c4 * 4 + 4, g_ * 128:(g_ + 1) * 128],
                              PS[b][:].rearrange("p (c q) -> p c q", q=128), ["ps%d" % b], [("XT", tc)])
        for c in range(8):
            v = XT[:, c, :].rearrange("p (i two q) -> p i two q", two=2, q=128)
            P.add("dve", lambda g, v=v: g.tensor_scalar(out=TMPX[:].rearrange("p (i q) -> p i q", q=128),
                                                        in0=v[:, :, 1, :], scalar1=pf[:, 0:1], scalar2=None,
                                                        op0=ALU.mult),
                  reads=[("XT", t) for t in range(8)], writes=["tmpx"])
            P.add("dve", lambda g, v=v, c=c: g.scalar_tensor_tensor(
                out=XTO[:, c, :].rearrange("p (i q) -> p i q", q=128), in0=v[:, :, 0, :], scalar=pf[:, 1:2],
                in1=TMPX[:].rearrange("p (i q) -> p i q", q=128), op0=ALU.mult, op1=ALU.add),
                  reads=[("XT", t) for t in range(8)] + ["tmpx"], writes=["XTO"])
        XTkeys = [("XT", t) for t in range(8)]

        def load_w(col0, n, dup=False):
            i = stg_r.next()
            j = wb_r.next()
            dma(STG[i][:, :, :n], w_in[:, :, col0:col0 + n], [], ["stg%d" % i])
            P.add("pool", lambda g: g.tensor_copy(out=WB[j][:, :, :n], in_=STG[i][:, :, :n]),
                  reads=["stg%d" % i], writes=["wb%d" % j])
            if dup:
                P.add("pool", lambda g: g.tensor_copy(out=WB[j][:, :, n:2 * n], in_=STG[i][:, :, :n]),
                      reads=["stg%d" % i], writes=["wb%d" % j])
            return j

        def proj_fm(j, ncols, own, dst_fn, gates=False, M=128):
            src = XTO if own else XT
            ntc = 4 if own else 8
            for gi in range(max(1, ncols // 128)):
                for tc in range(ntc):
                    b = psr.next()
                    for kc in range(8):
                        mm(PS[b][:M, :], WB[j][:, kc, gi * 128:gi * 128 + M], src[:, kc, tc * 512:(tc + 1) * 512],
                           kc == 0, kc == 7, ["wb%d" % j, "XTO"] + XTkeys, ["ps%d" % b])
                    if gates:
                        o = outf_r.next()
                        P.add("act", lambda g, b=b, o=o: g.activation(out=OUTF[o][:], in_=PS[b][:], func=AF.Sigmoid),
                              reads=["ps%d" % b], writes=["outf%d" % o])
                        dma(dst_fn(gi, tc), OUTF[o][:], ["outf%d" % o], [], eng="act")
                    else:
                        o = outb_r.next()
                        qe = evac_copy(OUTB[o][:M, :], PS[b][:M, :], ["ps%d" % b], ["outb%d" % o])
                        dma(dst_fn(gi, tc), OUTB[o][:M, :], ["outb%d" % o], [], eng=qe)

        def proj_tm(j, dst):
            for t in range(32):
                b = psr.next()
                for kc in range(8):
                    mm(PS[b][:], XT[:, kc, t * 128:(t + 1) * 128], WB[j][:, kc, :], kc == 0, kc == 7,
                       ["wb%d" % j] + XTkeys, ["ps%d" % b])
                o = outb_r.next()
                qe = evac_copy(OUTB[o][:], PS[b][:], ["ps%d" % b], ["outb%d" % o])
                dma(dst[t * 128:(t + 1) * 128, :], OUTB[o][:], ["outb%d" % o], [], eng=qe)

        for br, (qn, kn, vn) in enumerate((("qa", "ka", "va"), ("qf", "kf", "vf"), ("qs", "ks", "vs"))):
            j = load_w(OFF[qn], 512)
            proj_fm(j, 512, True, lambda gi, tc, br=br: QT_d[br * 4 + gi][:, tc * 512:(tc + 1) * 512])
            j = load_w(OFF[kn], 512)
            proj_fm(j, 512, False, lambda gi, tc, br=br: KT_d[br * 4 + gi][:, tc * 512:(tc + 1) * 512])
            j = load_w(OFF[vn], 512)
            proj_tm(j, VV_d[br])
        for half in range(2):
            j = load_w(OFF["qi"] + 512 * half, 512)
            proj_fm(j, 512, True, lambda gi, tc, half=half: QI_d[half * 4 + gi][:, tc * 512:(tc + 1) * 512])
        j = load_w(OFF["ki"], 64, dup=True)
        proj_fm(j, 128, False, lambda gi, tc: KI_d[:, tc * 512:(tc + 1) * 512])
        j = load_w(OFF["wi"], 16)
        for t in range(16):
            b = psr.next()
            for kc in range(8):
                mm(PS[b][:, :16], XTO[:, kc, t * 128:(t + 1) * 128], WB[j][:, kc, :16], kc == 0, kc == 7,
                   ["wb%d" % j, "XTO"], ["ps%d" % b])
            P.add("dve", lambda g, b=b, t=t: g.tensor_copy(out=WI[:, t, :], in_=PS[b][:, :16]),
                  reads=["ps%d" % b], writes=["WI"])
        j = load_w(OFF["fg"], 8)
        for tc in range(8):
            b = psr.next()
            for kc in range(8):
                mm(PS[b][:8, :], WB[j][:, kc, :8], XT[:, kc, tc * 512:(tc + 1) * 512], kc == 0, kc == 7,
                   ["wb%d" % j] + XTkeys, ["ps%d" % b])
            P.add("dve", lambda g, b=b, tc=tc: g.tensor_copy(out=FG[:, tc * 512:(tc + 1) * 512], in_=PS[b][:8, :]),
                  reads=["ps%d" % b], writes=["FG"])
        for gt in range(6):
            j = load_w(OFF["g"] + 512 * gt, 512)
            proj_fm(j, 512, True, lambda gi, tc, gt=gt: G_d[gt * 4 + gi][:, tc * 512:(tc + 1) * 512], gates=True)
        P.flush()
    with ExitStack() as st:
        LL = FG
        CN = sb(st, "CN", [8, S], F32)
        TMPC = sb(st, "tmpc", [8, 2048], F32)
        negb = sb(st, "negb", [8, 1], F32)
        dma(negb[:], bfg_d[layer], [], ["negb"])
        P.add("act", lambda g: g.activation(out=negb[:], in_=negb[:], func=AF.Copy, scale=-1.0),
              reads=["negb"], writes=["negb"])
        P.add("act", lambda g: g.activation(out=LL[:], in_=FG[:], func=AF.Exp, bias=negb[:, 0:1], scale=-1.0),
              reads=["FG", "negb"], writes=["FG"])
        P.add("act", lambda g: g.activation(out=LL[:], in_=LL[:], func=AF.Ln, bias=1.0, scale=1.0),
              reads=["FG"], writes=["FG"])
        P.add("dve", lambda g: g.tensor_tensor_scan(out=CN[:], data0=LL[:], data1=LL[:], initial=0.0,
                                                    op0=ALU.add, op1=ALU.bypass), reads=["FG"], writes=["CN"])
        b = psr.next()
        for t in range(32):
            P.add("pe", lambda g, t=t, b=b: g.transpose(PS[b][:, t * 8:(t + 1) * 8], CN[:, t * 128:(t + 1) * 128],
                                                        ident_f[:8, :8]),
                  reads=["CN"], writes=["ps%d" % b])
        P.add("dve", lambda g, b=b: g.tensor_copy(out=CNtok[:].rearrange("p t h -> p (t h)"), in_=PS[b][:, :256]),
              reads=["ps%d" % b], writes=["CNtok"])
        cv = CN[:].rearrange("p (i two q) -> p i two q", two=2, q=128)
        P.add("dve", lambda g: g.tensor_scalar(out=TMPC[:].rearrange("p (i q) -> p i q", q=128), in0=cv[:, :, 1, :],
                                               scalar1=pf[:8, 0:1], scalar2=None, op0=ALU.mult),
              reads=["CN"], writes=["tmpc"])
        P.add("dve", lambda g: g.scalar_tensor_tensor(out=CNown[:].rearrange("p (i q) -> p i q", q=128),
                                                      in0=cv[:, :, 0, :], scalar=pf[:8, 1:2],
                                                      in1=TMPC[:].rearrange("p (i q) -> p i q", q=128),
                                                      op0=ALU.mult, op1=ALU.add),
              reads=["CN", "tmpc"], writes=["CNown"])
        P.flush()
    st_fg.close()

    with ExitStack() as st:
        QI = sb(st, "QIs", [128, 8, 2048], BF16)
        KI = sb(st, "KIs", [128, S], BF16)
        SC = [sb(st, "SC%d" % i, [128, S], F32) for i in range(2)]
        TH = [sb(st, "TH%d" % i, [128, 512], F32) for i in range(3)]
        NBo = [sb(st, "NBo%d" % i, [128, S], BF16) for i in range(2)]
        JUNK = sb(st, "JUNK", [128, S], BF16)
        KPOS = sb(st, "KPOS", [128, S], F32)
        KSEL = sb(st, "KSEL", [128, S], F32)
        WABS = sb(st, "WABS", [128, 16, 16], F32)
        WSGN = sb(st, "WSGN", [128, 16, 16], F32)
        SM = sb(st, "SM", [128, 8], F32)
        for pr in range(8):
            dma(QI[:, pr, :], QI_d[pr], [], [("QI", pr)])
        dma(KI[:], KI_d[:, :], [], ["KI"])
        P.add("pool", lambda g: g.iota(KPOS[:], pattern=[[1, S]], base=0, channel_multiplier=0,
                                       allow_small_or_imprecise_dtypes=True), writes=["KPOS"])
        P.add("act", lambda g: g.activation(out=WABS[:], in_=WI[:], func=AF.Abs), reads=["WI"], writes=["WABS"])
        P.add("dve", lambda g: g.tensor_scalar(out=WSGN[:], in0=WI[:], scalar1=0.0, scalar2=2.0, op0=ALU.is_ge,
                                               op1=ALU.mult), reads=["WI"], writes=["WSGN"])
        P.add("dve", lambda g: g.tensor_scalar(out=WSGN[:], in0=WSGN[:], scalar1=-1.0, scalar2=None, op0=ALU.add),
              reads=["WSGN"], writes=["WSGN"])
        th_r = Rot([0, 1, 2])
        NSC = 3
        SC.append(sb(st, "SC2", [128, S], F32))
        JUNKA = sb(st, "JUNKA", [128, S], BF16)
        SMA = sb(st, "SMA", [128, 8], F32)

        def acc(i):
            nk = 128 * (2 * i + 2)
            sc = SC[i % NSC]
            sck = "SC%d" % (i % NSC)
            for kc in range((nk + 511) // 512):
                k0 = kc * 512
                kn = min(512, nk - k0)
                for h in range(16):
                    r0 = 64 * (h % 2)
                    b = psr.next()
                    mm(PS[b][:, :kn], QI[r0:r0 + 64, h // 2, i * 128:(i + 1) * 128], KI[r0:r0 + 64, k0:k0 + kn],
                       True, True, [("QI", h // 2), "KI"], ["ps%d" % b])
                    t = th_r.next()
                    P.add("act", lambda g, b=b, t=t, kn=kn, i=i, h=h: g.activation(
                        out=TH[t][:, :kn], in_=PS[b][:, :kn], func=AF.Relu, scale=WABS[:, i, h:h + 1]),
                          reads=["ps%d" % b, "WABS"], writes=["TH%d" % t])
                    if h == 0:
                        P.add("dve", lambda g, t=t, kn=kn, k0=k0, i=i, h=h, sc=sc: g.tensor_scalar(
                            out=sc[:, k0:k0 + kn], in0=TH[t][:, :kn], scalar1=WSGN[:, i, h:h + 1], scalar2=None,
                            op0=ALU.mult), reads=["TH%d" % t, "WSGN"], writes=[sck])
                    else:
                        P.add("dve", lambda g, t=t, kn=kn, k0=k0, i=i, h=h, sc=sc: g.scalar_tensor_tensor(
                            out=sc[:, k0:k0 + kn], in0=TH[t][:, :kn], scalar=WSGN[:, i, h:h + 1],
                            in1=sc[:, k0:k0 + kn], op0=ALU.mult, op1=ALU.add),
                              reads=["TH%d" % t, "WSGN", sck], writes=[sck])
            P.add("dve", lambda g, sc=sc, i=i: g.tensor_tensor(out=sc[:, 256 * i:256 * i + 256],
                                                              in0=sc[:, 256 * i:256 * i + 256], in1=NBQK[:],
                                                              op=ALU.add), reads=[sck], writes=[sck])

        def ctx(i):
            nk = 128 * (2 * i + 2)
            sc = SC[i % NSC]
            sck = "SC%d" % (i % NSC)
            use_act = (i % 2 == 1)
            smk = "SMA" if use_act else "SM"
            smt = SMA if use_act else SM
            return (nk, sc, sck, use_act, smk) + tuple(smt[:, c:c + 1] for c in range(6))

        def pre(i):
            nk, sc, sck, use_act, smk, lo, w0, mid, cnt, step, hi = ctx(i)
            if i == 0:
                P.add("dve", lambda g, lo=lo: g.memset(lo, -BIG / 2), writes=[smk])
            else:
                P.add("dve", lambda g, sc=sc, i=i, lo=lo: g.tensor_reduce(out=lo, in_=sc[:, :256 * i],
                                                                          axis=mybir.AxisListType.X, op=ALU.min),
                      reads=[sck], writes=[smk])
                P.add("dve", lambda g, sc=sc, nk=nk, hi=hi: g.tensor_reduce(out=hi, in_=sc[:, :nk],
                                                                            axis=mybir.AxisListType.X, op=ALU.max),
                      reads=[sck], writes=[smk])
                P.add("dve", lambda g, w0=w0, hi=hi, lo=lo: g.tensor_tensor(out=w0, in0=hi, in1=lo, op=ALU.subtract),
                      reads=[smk], writes=[smk])
                if use_act:
                    P.add("dve", lambda g, nlo=lo: g.tensor_scalar(out=nlo, in0=nlo, scalar1=-1.0, scalar2=None,
                                                                   op0=ALU.mult), reads=[smk], writes=[smk])

        def chain(i):
            nk, sc, sck, use_act, smk, lo, w0, mid, cnt, step, hi = ctx(i)
            if i > 0:
                if not use_act:
                    for it in range(NIT):
                        f = 2.0 ** (-(it + 1))
                        P.add("dve", lambda g, f=f, mid=mid, w0=w0, lo=lo: g.scalar_tensor_tensor(
                            out=mid, in0=w0, scalar=f, in1=lo, op0=ALU.mult, op1=ALU.add), reads=[smk], writes=[smk])
                        P.add("dve", lambda g, sc=sc, nk=nk, mid=mid, cnt=cnt: g.tensor_scalar(
                            out=JUNK[:, :nk], in0=sc[:, :nk], scalar1=mid, scalar2=None, op0=ALU.is_ge, op1=ALU.add,
                            accum_out=cnt), reads=[sck, smk], writes=["JUNK", smk])
                        P.add("dve", lambda g, f=f, step=step, cnt=cnt: g.tensor_scalar(
                            out=step, in0=cnt, scalar1=255.5, scalar2=f, op0=ALU.is_ge, op1=ALU.mult),
                              reads=[smk], writes=[smk])
                        P.add("dve", lambda g, lo=lo, w0=w0, step=step: g.scalar_tensor_tensor(
                            out=lo, in0=w0, scalar=step, in1=lo, op0=ALU.mult, op1=ALU.add),
                              reads=[smk], writes=[smk])
                else:
                    nlo, nmid, ssum, sg, tt = lo, mid, cnt, step, hi
                    for it in range(NIT):
                        f = 2.0 ** (-(it + 1))
                        P.add("act", lambda g, f=f, nmid=nmid, w0=w0, nlo=nlo: g.activation(
                            out=nmid, in_=w0, func=AF.Identity, scale=-f, bias=nlo), reads=[smk], writes=[smk])
                        P.add("act", lambda g, sc=sc, nk=nk, nmid=nmid, ssum=ssum: g.activation(
                            out=JUNKA[:, :nk], in_=sc[:, :nk], func=AF.Sign, scale=1.0, bias=nmid, accum_out=ssum),
                              reads=[sck, smk], writes=["JUNKA", smk])
                        P.add("act", lambda g, sg=sg, ssum=ssum, nk=nk: g.activation(
                            out=sg, in_=ssum, func=AF.Sign, scale=1.0, bias=SGB[:, (nk // 256) - 1:(nk // 256)]),
                              reads=[smk, "SGB"], writes=[smk])
                        P.add("act", lambda g, f=f, tt=tt, sg=sg, it=it: g.activation(
                            out=tt, in_=sg, func=AF.Identity, scale=-f / 2, bias=FB[:, it:it + 1]),
                              reads=[smk, "FB"], writes=[smk])
                        P.add("act", lambda g, nlo=nlo, w0=w0, tt=tt: g.activation(
                            out=nlo, in_=w0, func=AF.Identity, scale=tt, bias=nlo), reads=[smk], writes=[smk])

        def post(i):
            nk, sc, sck, use_act, smk, lo, w0, mid, cnt, step, hi = ctx(i)
            if use_act and i > 0:
                P.add("dve", lambda g, nlo=lo: g.tensor_scalar(out=nlo, in0=nlo, scalar1=-1.0, scalar2=None,
                                                               op0=ALU.mult), reads=[smk], writes=[smk])
            nbo = NBo[i % 2]
            nbk = "NBo%d" % (i % 2)
            P.add("dve", lambda g, nbo=nbo, sc=sc, nk=nk, lo=lo: g.tensor_scalar(
                out=nbo[:, :nk], in0=sc[:, :nk], scalar1=lo, scalar2=-BIG, op0=ALU.is_lt, op1=ALU.mult),
                  reads=[sck, smk], writes=[nbk])
            P.add("pool", lambda g, nbo=nbo, nk=nk: g.tensor_tensor(out=KSEL[:, :nk], in0=nbo[:, :nk], in1=KPOS[:, :nk],
                                                                   op=ALU.add), reads=[nbk, "KPOS"], writes=["KSEL"])
            P.add("dve", lambda g, nk=nk, i=i: g.tensor_reduce(out=KMS[:, i:i + 1], in_=KSEL[:, :nk],
                                                               axis=mybir.AxisListType.X, op=ALU.max),
                  reads=["KSEL"], writes=["KMS"])
            dma(NBA_d[i][:, :nk], nbo[:, :nk], [nbk], [])

        SGB = sb(st, "SGB", [128, 16], F32)
        FB = sb(st, "FB", [128, NIT], F32)
        for j in range(16):
            P.add("pool", lambda g, j=j: g.memset(SGB[:, j:j + 1], 256.0 * (j + 1) - 511.5), writes=["SGB"])
        for it in range(NIT):
            P.add("pool", lambda g, it=it: g.memset(FB[:, it:it + 1], -(2.0 ** (-(it + 2)))), writes=["FB"])
        for pp in range(8):
            acc(2 * pp)
            acc(2 * pp + 1)
            pre(2 * pp)
            pre(2 * pp + 1)
            chain(2 * pp)
            chain(2 * pp + 1)
            post(2 * pp)
            post(2 * pp + 1)
        P.flush()

    for br in range(2):
        with ExitStack() as st:
            KT = [sb(st, "KTa%d" % i, [128, S], BF16) for i in range(2)]
            VT = [sb(st, "VTa%d" % i, [128, 32, 128], BF16) for i in range(2)]
            QT = [sb(st, "QTa%d" % i, [128, 2048], BF16) for i in range(2)]
            SROW = [sb(st, "SROW%d" % i, [128, 512], F32) for i in range(2)]
            PT = [sb(st, "PT%d" % i, [128, 512], BF16) for i in range(4)]
            RS = [sb(st, "RS%d" % i, [128, 512], F32) for i in range(2)]
            OTS = [sb(st, "OTS%d" % i, [128, 512], BF16) for i in range(2)]
            NB4 = [sb(st, "NB4%d" % i, [128, 4, S], BF16) for i in range(2)] if br == 0 else None
            srow_r, ots_r = Rot([0, 1]), Rot([0, 1])
            pt_r, rs_r, nb_r = Rot([0, 1, 2, 3]), Rot([0, 1]), Rot([0, 1])
            sps = Rot([0, 1, 2])
            ops = Rot([3, 4, 5])
            BK = AB if br == 0 else CNtok
            nb_map = {}

            def nb_load(idx):
                qc_ = idx % 4
                n4_ = nb_r.next()
                nb_map[idx] = n4_
                for t in range(4):
                    nk = 128 * (2 * (4 * qc_ + t) + 2)
                    dma(NB4[n4_][:, t, :nk], NBA_d[4 * qc_ + t][:, :nk], [], [("NB4", n4_, t)], eng="act")

            for i in range(2):
                P.add("pool", lambda g, i=i: g.memset(KT[i][64:128, :], 0.0), writes=["KTa%d" % i])
                P.add("pool", lambda g, i=i: g.memset(KT[i][64:65, :], 1.0), writes=["KTa%d" % i])
                P.add("pool", lambda g, i=i: g.memset(QT[i][64:128, :], 0.0), writes=["QTa%d" % i])
                P.add("pool", lambda g, i=i: g.memset(VT[i][:, :, 64:128], 0.0), writes=["VTa%d" % i])
                P.add("pool", lambda g, i=i: g.memset(VT[i][:, :, 64:65], 1.0), writes=["VTa%d" % i])
            def main_loop():
              for h in range(8):
                pr, hh = h // 2, h % 2
                r0 = 64 * hh
                kt, vt, qt = KT[h % 2], VT[h % 2], QT[h % 2]
                kk, vk, qk = "KTa%d" % (h % 2), "VTa%d" % (h % 2), "QTa%d" % (h % 2)
                dma(kt[0:64, :], KT_d[br * 4 + pr][r0:r0 + 64, :], [], [kk])
                dma(vt[:, :, 0:64], VV_d[br].rearrange("(t p) c -> p t c", p=128)[:, :, h * 64:(h + 1) * 64], [], [vk])
                dma(qt[0:64, :], QT_d[br * 4 + pr][r0:r0 + 64, :], [], [qk])
                for qc in range(4):
                    if br == 0:
                        sl = 2.0 ** (-(h + 1))
                        for t in range(4):
                            P.add("pe", lambda g, t=t, qc=qc: g.matmul(
                                PS[7][64:65, t * 128:(t + 1) * 128], KMS[:, 4 * qc + t:4 * qc + t + 1], ident_f[:, :],
                                start=True, stop=True), reads=["KMS"], writes=["ps7"])
                        P.add("act", lambda g, qc=qc, sl=sl, qt=qt: g.activation(
                            out=qt[64:65, qc * 512:(qc + 1) * 512], in_=PS[7][64:65, :], func=AF.Copy,
                            scale=-8.0 * sl), reads=["ps7"], writes=[qk])
                    else:
                        P.add("pe", lambda g, h=h, qc=qc: g.matmul(PS[7][64:65, :], ident_f[:8, h:h + 1],
                                                                    CNown[:, qc * 512:(qc + 1) * 512],
                                                                    start=True, stop=True),
                              reads=["CNown"], writes=["ps7"])
                        P.add("act", lambda g, qc=qc, qt=qt: g.activation(
                            out=qt[64:65, qc * 512:(qc + 1) * 512], in_=PS[7][64:65, :], func=AF.Copy, scale=-8.0),
                              reads=["ps7"], writes=[qk])
                for qc in range(4):
                    nkt = 8 * qc + 8
                    n4 = None
                    if br == 0:
                        if h == 0 and qc == 0:
                            nb_load(0)
                        n4 = nb_map[h * 4 + qc]
                        if h * 4 + qc + 1 < 32:
                            nb_load(h * 4 + qc + 1)
                    oa = ops.next()
                    ptmap = {}

                    def sgrp(kti, h=h, qc=qc, n4=n4, kt=kt, qt=qt, kk=kk, qk=qk):
                        s = sps.next()
                        j = kti - 8 * qc
                        extra = []
                        if j >= 0:
                            extra.append(("cm", j))
                        if br == 0:
                            for t in range(4):
                                if kti <= 2 * (4 * qc + t) + 1:
                                    extra.append(("nb", t))
                        mm(PS[s][:], kt[:, kti * 128:(kti + 1) * 128], qt[:, qc * 512:(qc + 1) * 512], True,
                           len(extra) == 0, [kk, qk], ["ps%d" % s])
                        for ei, ex in enumerate(extra):
                            lastf = ei == len(extra) - 1
                            if ex[0] == "cm":
                                mm(PS[s][:], ident_bf[:], CM[:, ex[1], :], False, lastf, ["CM"], ["ps%d" % s])
                            else:
                                t = ex[1]
                                mm(PS[s][:, t * 128:(t + 1) * 128], NB4[n4][:, t, kti * 128:(kti + 1) * 128],
                                   ident_bf[:], False, lastf, [("NB4", n4, t)], ["ps%d" % s])
                        pt = pt_r.next()
                        ptmap[kti] = pt
                        P.add("act", lambda g, s=s, pt=pt, kti=kti, h=h: g.activation(
                            out=PT[pt][:], in_=PS[s][:], func=AF.Exp, bias=BK[:, kti, h:h + 1], scale=0.125),
                              reads=["ps%d" % s, "CNtok"], writes=["PT%d" % pt])

                    def pvg(kti, vt=vt, vk=vk, oa=oa, nkt=nkt):
                        pt = ptmap[kti]
                        mm(PS[oa][:], vt[:, kti, :], PT[pt][:], kti == 0, kti == nkt - 1,
                           [vk, "PT%d" % pt], ["ps%d" % oa])

                    LA = 2
                    for kti in range(min(LA, nkt)):
                        sgrp(kti)
                    if pending:
                        pending.pop()()
                    for kti in range(nkt):
                        if kti + LA < nkt:
                            sgrp(kti + LA)
                        pvg(kti)
                    pending.append(lambda oa=oa, pr=pr, r0=r0, qc=qc: finalize(oa, pr, r0, qc))

            def finalize(oa, pr, r0, qc):
                if True:
                    sr = srow_r.next()
                    r = rs_r.next()
                    P.add("act", lambda g, sr=sr, oa=oa: g.activation(out=SROW[sr][64:65, :], in_=PS[oa][64:65, :],
                                                                      func=AF.Copy),
                          reads=["ps%d" % oa], writes=["SROW%d" % sr])
                    P.add("dve", lambda g, sr=sr: g.reciprocal(out=SROW[sr][64:65, :], in_=SROW[sr][64:65, :]),
                          reads=["SROW%d" % sr], writes=["SROW%d" % sr])
                    P.add("pe", lambda g, sr=sr: g.matmul(PS[6][:64, :], ONESF[64:65, :], SROW[sr][64:65, :],
                                                          start=True, stop=True),
                          reads=["SROW%d" % sr], writes=["ps6"])
                    P.add("act", lambda g, r=r: g.activation(out=RS[r][:64, :], in_=PS[6][:64, :], func=AF.Copy),
                          reads=["ps6"], writes=["RS%d" % r])
                    o = ots_r.next()
                    P.add("dve", lambda g, r=r, oa=oa, o=o: g.tensor_tensor(
                        out=OTS[o][:64, :], in0=PS[oa][:64, :], in1=RS[r][:64, :], op=ALU.mult),
                          reads=["ps%d" % oa, "RS%d" % r], writes=["OTS%d" % o])
                    dma(OT_d[br, pr][r0:r0 + 64, qc * 512:(qc + 1) * 512], OTS[o][:64, :],
                        ["OTS%d" % o], [], eng="act")

            pending = []
            main_loop()
            if pending:
                pending.pop()()
            P.flush()

    with ExitStack() as st:
        NSET = 4
        KT = [sb(st, "KTc%d" % i, [128, S], BF16) for i in range(1)] * 2
        VT = [sb(st, "VTc%d" % i, [128, 32, 128], BF16) for i in range(2)]
        QT = [sb(st, "QTc%d" % i, [128, 2048], BF16) for i in range(1)] * 2
        NSG = 3
        SG = [sb(st, "SG%d" % i, [128, S], F32) for i in range(NSG)]
        INC = [sb(st, "INC%d" % i, [128, S + 1], F32) for i in range(NSET)]
        AA = [sb(st, "AA%d" % i, [128, S], BF16) for i in range(NSET)]
        ATS = [sb(st, "ATS%d" % i, [128, 512], BF16) for i in range(3)]
        OTS = [sb(st, "OTSc%d" % i, [128, 128], BF16) for i in range(2)]
        ots_r = Rot([0, 1])
        zr, at_r, ats_r, oc_r = Rot([0, 1, 2]), Rot([3, 4]), Rot([0, 1, 2]), Rot([5, 6])
        items = [(pr, hh, i) for pr in range(4) for hh in range(2) for i in range(16)]

        def stage_a(n):
            pr, hh, i = items[n]
            kt, qt = KT[pr % 2], QT[pr % 2]
            kk, vk, qk = "KTc0", "VTc%d" % (pr % 2), "QTc0"
            r0 = 64 * hh
            if n == 0:
                P.add("pool", lambda g: g.memset(kt[64:128, :], 0.0), writes=[kk])
                P.add("pool", lambda g: g.memset(qt[64:128, :], 0.0), writes=[qk])
            if i == 0:
                dma(kt[0:64, :], KT_d[8 + pr][r0:r0 + 64, :], [], [kk])
                dma(qt[0:64, :], QT_d[8 + pr][r0:r0 + 64, :], [], [qk])
                if hh == 0:
                    dma(VT[pr % 2][:], VV_d[2].rearrange("(t p) c -> p t c", p=128)[:, :, pr * 128:(pr + 1) * 128],
                        [], [vk])
            nk = 128 * (2 * i + 2)
            sg, inc, aa = SG[n % NSG], INC[n % NSET], AA[n % NSET]
            sgk, inck, aak = "SG%d" % (n % NSG), "INC%d" % (n % NSET), "AA%d" % (n % NSET)
            for kc in range((nk + 511) // 512):
                k0 = kc * 512
                kn = min(512, nk - k0)
                b = zr.next()
                mm(PS[b][:, :kn], qt[:, i * 128:(i + 1) * 128], kt[:, k0:k0 + kn],
                   True, True, [kk, qk], ["ps%d" % b])
                P.add("act", lambda g, b=b, kn=kn, k0=k0, sg=sg: g.activation(
                    out=sg[:, k0:k0 + kn], in_=PS[b][:, :kn], func=AF.Sigmoid, scale=-0.125),
                      reads=["ps%d" % b], writes=[sgk])
            P.add("dve", lambda g, sg=sg, i=i: g.tensor_tensor(out=sg[:, 256 * i:256 * i + 256],
                                                                in0=sg[:, 256 * i:256 * i + 256], in1=M1[:],
                                                                op=ALU.max), reads=[sgk], writes=[sgk])
            P.add("pool", lambda g, inc=inc, nk=nk: g.memset(inc[:, nk:nk + 1], 1.0), writes=[inck])
            P.add("dve", lambda g, inc=inc, sg=sg, nk=nk: g.tensor_tensor_scan(
                out=inc[:, 0:nk][:, ::-1], data0=sg[:, 0:nk][:, ::-1],
                data1=sg[:, 0:nk][:, ::-1], initial=1.0, op0=ALU.mult, op1=ALU.bypass),
                  reads=[sgk, inck], writes=[inck])
            P.add("pool", lambda g, aa=aa, inc=inc, nk=nk: g.tensor_tensor(
                out=aa[:, :nk], in0=inc[:, 1:nk + 1], in1=inc[:, 0:nk], op=ALU.subtract),
                  reads=[inck], writes=[aak])

        def stage_b(n):
            pr, hh, i = items[n]
            vt = VT[pr % 2]
            vk = "VTc%d" % (pr % 2)
            r0 = 64 * hh
            aa = AA[n % NSET]
            aak = "AA%d" % (n % NSET)
            oc = oc_r.next()
            nkt = 2 * i + 2
            ng = (nkt + 3) // 4
            st_ = {}

            def tr(g4):
                n4 = min(4, nkt - 4 * g4)
                a = at_r.next()
                apv = PS[a][:].bitcast(BF16)
                for u in range(n4):
                    kti = 4 * g4 + u
                    P.add("pe", lambda g, apv=apv, u=u, kti=kti: g.transpose(
                        apv[:, u * 128:(u + 1) * 128], aa[:, kti * 128:(kti + 1) * 128], ident_bf[:]),
                          reads=[aak], writes=["ps%d" % a])
                s_ = ats_r.next()
                if True:
                    P.add("act", lambda g, s_=s_, apv=apv, n4=n4: g.activation(
                        out=ATS[s_][:, :n4 * 128], in_=apv[:, :n4 * 128], func=AF.Copy),
                          reads=["ps%d" % a], writes=["ATS%d" % s_])
                else:
                    P.add("dve", lambda g, s_=s_, apv=apv, n4=n4: g.tensor_copy(
                        out=ATS[s_][:, :n4 * 128], in_=apv[:, :n4 * 128]),
                          reads=["ps%d" % a], writes=["ATS%d" % s_])
                st_[g4] = (s_, n4)

            def pv(g4):
                s_, n4 = st_[g4]
                for u in range(n4):
                    kti = 4 * g4 + u
                    mm(PS[oc][:, :128], vt[:, kti, :], ATS[s_][:, u * 128:(u + 1) * 128], kti == 0,
                       kti == nkt - 1, [vk, "ATS%d" % s_], ["ps%d" % oc])

            tr(0)
            for g4 in range(ng):
                if g4 + 1 < ng:
                    tr(g4 + 1)
                pv(g4)
            o = ots_r.next()
            P.add("act", lambda g, oc=oc, r0=r0, o=o: g.activation(
                out=OTS[o][r0:r0 + 64, :], in_=PS[oc][r0:r0 + 64, :128], func=AF.Copy),
                  reads=["ps%d" % oc], writes=["OTSc%d" % o])
            dma(OT_d[2, pr][r0:r0 + 64, i * 128:(i + 1) * 128], OTS[o][r0:r0 + 64, :], ["OTSc%d" % o], [],
                eng="act")

        NI = len(items)
        DEPTH_A = NSET - 1
        for n in range(DEPTH_A):
            stage_a(n)
        for n in range(NI):
            stage_b(n)
            if n + DEPTH_A < NI:
                stage_a(n + DEPTH_A)
        P.flush()

    with ExitStack() as st:
        STG = [sb(st, "tstg%d" % i, [128, 8, 512], F32) for i in range(2)]
        WB = [sb(st, "twb%d" % i, [128, 8, 512], BF16) for i in range(3)]
        OTc = [sb(st, "OTc%d" % i, [128, 4, 512], BF16) for i in range(3)]
        GT = [sb(st, "GT%d" % i, [128, 512], F32) for i in range(3)]
        MF = sb(st, "MF", [128, 512], F32)
        MT = sb(st, "MT", [128, 8, 512], BF16)
        XO = sb(st, "XO", [128, 4, D], F32)
        X1 = sb(st, "X1", [128, 4, D], F32)
        X1T = sb(st, "X1T", [128, 8, 512], BF16)
        HR = [sb(st, "HR%d" % i, [128, 512], F32) for i in range(2)]
        HT = sb(st, "HT", [128, 32, 512], BF16)
        XN = sb(st, "XN", [128, D], F32)
        ST6 = sb(st, "ST6", [128, 2, 6], F32)
        MV = sb(st, "MV", [128, 4], F32)
        LNP = sb(st, "LNP", [128, 4, D], F32)
        stg_r, wb_r, gt_r, hr_r = Rot([0, 1]), Rot([0, 1, 2]), Rot([0, 1, 2]), Rot([0, 1])
        for n, src in enumerate((ln1g_d, ln1b_d, ln2g_d, ln2b_d)):
            dma(LNP[:, n, :], src[layer].partition_broadcast(128), [], ["LNP"])

        cast_flip = [0]

        def load_wt(src3, kc, n):
            i = stg_r.next()
            j = wb_r.next()
            dma(STG[i][:, :kc, :n], src3, [], ["tstg%d" % i])
            cast_flip[0] ^= 1
            if cast_flip[0]:
                P.add("dve", lambda g: g.tensor_copy(out=WB[j][:, :kc, :n], in_=STG[i][:, :kc, :n]),
                      reads=["tstg%d" % i], writes=["twb%d" % j])
            else:
                P.add("act", lambda g: g.activation(out=WB[j][:, :kc, :n], in_=STG[i][:, :kc, :n], func=AF.Copy),
                      reads=["tstg%d" % i], writes=["twb%d" % j])
            return j

        def layer_norm(src, dst, gi, key_src, key_dst):
            for hf in range(2):
                P.add("dve", lambda g, hf=hf: g.bn_stats(out=ST6[:, hf, :], in_=src[:, hf * 512:(hf + 1) * 512]),
                      reads=[key_src], writes=["ST6"])
            P.add("dve", lambda g: g.bn_aggr(out=MV[:, 0:2], in_=ST6[:].rearrange("p a b -> p (a b)")),
                  reads=["ST6"], writes=["MV"])
            P.add("act", lambda g: g.activation(out=MV[:, 2:3], in_=MV[:, 1:2], func=AF.Sqrt, bias=EPSB[:, 0:1],
                                                scale=1.0), reads=["MV", "EPSB"], writes=["MV"])
            P.add("dve", lambda g: g.reciprocal(out=MV[:, 3:4], in_=MV[:, 2:3]), reads=["MV"], writes=["MV"])
            P.add("dve", lambda g: g.tensor_scalar(out=XN[:], in0=src, scalar1=MV[:, 0:1], scalar2=MV[:, 3:4],
                                                   op0=ALU.subtract, op1=ALU.mult),
                  reads=[key_src, "MV"], writes=["XN"])
            P.add("dve", lambda g: g.tensor_tensor(out=XN[:], in0=XN[:], in1=LNP[:, gi, :], op=ALU.mult),
                  reads=["XN", "LNP"], writes=["XN"])
            P.add("dve", lambda g: g.tensor_tensor(out=dst, in0=XN[:], in1=LNP[:, gi + 1, :], op=ALU.add),
                  reads=["XN", "LNP"], writes=[key_dst])

        EPSB = sb(st, "EPSB", [128, 1], F32)
        P.add("pool", lambda g: g.memset(EPSB[:], EPS), writes=["EPSB"])
        wbr = wbr_d[layer]
        wout = wout_d[layer].rearrange("(c p) n -> p c n", p=128)
        wff1 = wff1_d[layer].rearrange("(c p) n -> p c n", p=128)
        wff2 = wff2_d[layer].rearrange("(c p) n -> p c n", p=128)
        for tcx in range(4):
            t0 = tcx * 512
            if layer == 0:
                dma(XO[:], xo_d[t0:t0 + 512, :].rearrange("(t p) d -> p t d", p=128), [], ["XO"])
            else:
                dma(XO[:], X1own_t[tcx].ap().rearrange("(t p) d -> p t d", p=128), ["X1own%d" % tcx], ["XO"])
            for bi in range(3):
                dma(OTc[bi][:], OT_d[bi].rearrange("w p t -> p w t")[:, :, t0:t0 + 512], [], [("OTc", bi)])
            for dh in range(2):
                wj = [load_wt(wbr[bi].rearrange("(c p) n -> p c n", p=128)[:, :, dh * 512:(dh + 1) * 512], 4, 512)
                      for bi in range(3)]
                for dq in range(4):
                    dc = dh * 4 + dq
                    for bi in range(3):
                        gt = gt_r.next()
                        dma(GT[gt][:], G_d[bi * 8 + dc][:, t0:t0 + 512], [], ["GT%d" % gt])
                        b = psr.next()
                        for wc in range(4):
                            mm(PS[b][:], WB[wj[bi]][:, wc, dq * 128:(dq + 1) * 128], OTc[bi][:, wc, :],
                               wc == 0, wc == 3, ["twb%d" % wj[bi], ("OTc", bi)], ["ps%d" % b])
                        if bi == 0:
                            P.add("dve", lambda g, b=b, gt=gt: g.tensor_tensor(out=MF[:], in0=PS[b][:], in1=GT[gt][:],
                                                                               op=ALU.mult),
                                  reads=["ps%d" % b, "GT%d" % gt], writes=["MF"])
                        else:
                            P.add("dve", lambda g, b=b, gt=gt: g.tensor_tensor(out=GT[gt][:], in0=PS[b][:],
                                                                               in1=GT[gt][:], op=ALU.mult),
                                  reads=["ps%d" % b, "GT%d" % gt], writes=["GT%d" % gt])
                            if bi == 1:
                                P.add("pool", lambda g, gt=gt: g.tensor_tensor(out=MF[:], in0=MF[:], in1=GT[gt][:],
                                                                               op=ALU.add),
                                      reads=["MF", "GT%d" % gt], writes=["MF"])
                            else:
                                P.add("pool", lambda g, gt=gt, dc=dc: g.tensor_tensor(out=MT[:, dc, :], in0=MF[:],
                                                                                      in1=GT[gt][:], op=ALU.add),
                                      reads=["MF", "GT%d" % gt], writes=["MT"])
            wo = [load_wt(wout[:, :, hf * 512:(hf + 1) * 512], 8, 512) for hf in range(2)]
            for t in range(4):
                for hf in range(2):
                    b = psr.next()
                    for dc in range(8):
                        mm(PS[b][:], MT[:, dc, t * 128:(t + 1) * 128], WB[wo[hf]][:, dc, :], dc == 0, dc == 7,
                           ["MT", "twb%d" % wo[hf]], ["ps%d" % b])
                    P.add("dve", lambda g, b=b, t=t, hf=hf: g.scalar_tensor_tensor(
                        out=X1[:, t, hf * 512:(hf + 1) * 512], in0=XO[:, t, hf * 512:(hf + 1) * 512], scalar=ALPHA,
                        in1=PS[b][:], op0=ALU.mult, op1=ALU.add), reads=["ps%d" % b, "XO"], writes=[("X1", t)])
            for t in range(4):
                layer_norm(X1[:, t, :], X1[:, t, :], 0, ("X1", t), ("X1", t))
            for t in range(4):
                for dc in range(8):
                    if dc % 4 == 0:
                        b = psr.next()
                    P.add("pe", lambda g, b=b, t=t, dc=dc: g.transpose(PS[b][:, (dc % 4) * 128:(dc % 4 + 1) * 128],
                                                                       X1[:, t, dc * 128:(dc + 1) * 128], ident_f[:]),
                          reads=[("X1", t)], writes=["ps%d" % b])
                    if dc % 4 == 3:
                        d0 = dc - 3
                        P.add("act", lambda g, b=b, t=t, d0=d0: g.activation(
                            out=X1T[:, d0:d0 + 4, t * 128:(t + 1) * 128],
                            in_=PS[b][:].rearrange("p (c q) -> p c q", q=128), func=AF.Copy),
                              reads=["ps%d" % b], writes=["X1T"])
            for ft in range(8):
                j = load_wt(wff1[:, :, ft * 512:(ft + 1) * 512], 8, 512)
                for fq in range(4):
                    fc = ft * 4 + fq
                    b = psr.next()
                    for dc in range(8):
                        mm(PS[b][:], WB[j][:, dc, fq * 128:(fq + 1) * 128], X1T[:, dc, :], dc == 0, dc == 7,
                           ["twb%d" % j, "X1T"], ["ps%d" % b])
                    hr = hr_r.next()
                    P.add("act", lambda g, b=b, hr=hr: g.activation(out=HR[hr][:], in_=PS[b][:], func=AF.Relu),
                          reads=["ps%d" % b], writes=["HR%d" % hr])
                    P.add("dve", lambda g, hr=hr, fc=fc: g.tensor_tensor(out=HT[:, fc, :], in0=HR[hr][:],
                                                                          in1=HR[hr][:], op=ALU.mult),
                          reads=["HR%d" % hr], writes=["HT"])
            ybanks = [psr.next() for _ in range(8)]
            for kg in range(4):
                for hf in range(2):
                    j = load_wt(wff2[:, kg * 8:(kg + 1) * 8, hf * 512:(hf + 1) * 512], 8, 512)
                    for t in range(4):
                        b = ybanks[t * 2 + hf]
                        for k8 in range(8):
                            fc = kg * 8 + k8
                            mm(PS[b][:], HT[:, fc, t * 128:(t + 1) * 128], WB[j][:, k8, :], fc == 0, fc == 31,
                               ["HT", "twb%d" % j], ["ps%d" % b])
            for t in range(4):
                for hf in range(2):
                    b = ybanks[t * 2 + hf]
                    P.add("dve", lambda g, b=b, t=t, hf=hf: g.scalar_tensor_tensor(
                        out=XO[:, t, hf * 512:(hf + 1) * 512], in0=X1[:, t, hf * 512:(hf + 1) * 512], scalar=ALPHA,
                        in1=PS[b][:], op0=ALU.mult, op1=ALU.add), reads=["ps%d" % b, ("X1", t)], writes=["XO"])
                layer_norm(XO[:, t, :], XO[:, t, :], 2, "XO", "XO")
            if is_last:
                dma(out_d[t0:t0 + 512, :].rearrange("(t p) d -> p t d", p=128), XO[:], ["XO"], [], eng="act")
            else:
                dma(X1own_t[tcx].ap().rearrange("(t p) d -> p t d", p=128), XO[:], ["XO"], ["X1own%d" % tcx],
                    eng="act")
                P.add("pool", lambda g, tcx=tcx: g.collective_compute(
                    "AllGather", ALU.bypass, replica_groups=[[0, 1], [2, 3], [4, 5], [6, 7]],
                    ins=[X1own_t[tcx].ap().opt()], outs=[X1all_t[tcx].ap().opt()]),
                      reads=["X1own%d" % tcx], writes=["X1all%d" % tcx], cc=True)
        P.flush()


_NC_CACHE = {}


def _get_nc(n_layers):
    if n_layers not in _NC_CACHE:
        _NC_CACHE[n_layers] = build_program(n_layers)
    return _NC_CACHE[n_layers]


def _own_tiles(x_b, p):
    return np.ascontiguousarray(x_b.reshape(16, 2, 128, D)[:, p].reshape(2048, D))


def _run(xs, weights, n_layers):
    in_maps = []
    for c in range(8):
        b, p = c // 2, c % 2
        pfv = np.zeros((128, 4), np.float32)
        pfv[:, 0] = p
        pfv[:, 1] = 1 - p
        pfv[:, 2] = 128 * p
        m = {"xa": np.ascontiguousarray(xs[b]), "xo": _own_tiles(xs[b], p), "pf": pfv}
        for k, v in weights.items():
            a = v[:n_layers]
            if k == "b_forget":
                a = a.reshape(n_layers, 8, 1)
            m[k] = np.ascontiguousarray(a)
        in_maps.append(m)
    nc = _get_nc(n_layers)
    res = run_bass_kernel_spmd(nc, in_maps, core_ids=list(range(8)))
    out = np.empty((NB, S, D), np.float32)
    for c in range(8):
        b, p = c // 2, c % 2
        out[b].reshape(16, 2, 128, D)[:, p] = res.results[c]["out"].reshape(16, 128, D)
    return out


def kernel(x, w_in, b_forget, w_branch, w_out, ln1_g, ln1_b, w_ff1, w_ff2, ln2_g, ln2_b):
    weights = dict(w_in=np.asarray(w_in, np.float32), b_forget=np.asarray(b_forget, np.float32),
                   w_branch=np.asarray(w_branch, np.float32), w_out=np.asarray(w_out, np.float32),
                   ln1_g=np.asarray(ln1_g, np.float32), ln1_b=np.asarray(ln1_b, np.float32),
                   w_ff1=np.asarray(w_ff1, np.float32), w_ff2=np.asarray(w_ff2, np.float32),
                   ln2_g=np.asarray(ln2_g, np.float32), ln2_b=np.asarray(ln2_b, np.float32))
    return _run(np.asarray(x, np.float32), weights, DEPTH)
```

```python
from contextlib import ExitStack

import numpy as np
import concourse.bass as bass
import concourse.mybir as mybir
from concourse.bass_utils import run_bass_kernel_spmd

F32 = mybir.dt.float32
BF16 = mybir.dt.bfloat16
AF = mybir.ActivationFunctionType
ALU = mybir.AluOpType

D = 1024
S = 4096
NB = 4
DEPTH = 2
DFF = 4096
NCOLS = 8792
ALPHA = (2.0 * DEPTH) ** 0.25
EPS = 1e-5
BIG = 30000.0
NIT = 24
OFF = dict(qa=0, ka=512, va=1024, qi=1536, ki=2560, wi=2624, qf=2640, kf=3152, vf=3664,
           fg=4176, qs=4184, ks=4696, vs=5208, g=5720)
ENGS = ("pe", "act", "dve", "pool", "sp")
ND = 40


class Op:
    __slots__ = ("eng", "fn", "deps", "signal", "sigval", "dsem", "dval", "dma")


class Prog:
    def __init__(self, nc, stack):
        self.nc = nc
        self.ops = {e: [] for e in ENGS}
        self.writers = {}
        self.readers = {}
        self.dma_ops = []
        self.sem = {e: stack.enter_context(nc.semaphore("s_" + e)) for e in ENGS}
        self.dsem = [stack.enter_context(nc.semaphore("d%d" % i)) for i in range(ND)]
        self.ccsem = stack.enter_context(nc.semaphore("ccsem"))
        self.cc_ops = []
        self.count = {e: 0 for e in ENGS}
        self.known = {e: {} for e in ENGS}
        self.last = {e: None for e in ENGS}
        self.nblk = 0

    def add(self, eng, fn, reads=(), writes=(), extra=(), dma=False, force_signal=False, cc=False):
        op = Op()
        op.eng, op.fn, op.dma = eng, fn, dma or cc
        op.signal = force_signal
        op.sigval = op.dsem = op.dval = None
        deps = set(extra)
        for k in reads:
            deps.update(self.writers.get(k, {}).values())
        for k in writes:
            deps.update(self.writers.get(k, {}).values())
            deps.update(self.readers.get(k, {}).values())
        if cc:
            self.cc_ops.append(op)
            op.dsem = "cc"
            op.dval = len(self.cc_ops)
        if dma:
            n = len(self.dma_ops)
            op.dsem = n % ND
            op.dval = 16 * (n // ND + 1)
            if n >= ND:
                deps.add(self.dma_ops[n - ND])
            self.dma_ops.append(op)
        deps.discard(op)
        if eng == "pe":
            deps = {d for d in deps if d.eng != "pe"}
        op.deps = deps
        for d in deps:
            d.signal = True
        for k in writes:
            self.writers[k] = {eng: op}
            self.readers[k] = {}
        for k in reads:
            self.readers.setdefault(k, {})[eng] = op
        self.ops[eng].append(op)
        self.last[eng] = op
        return op

    def barrier(self):
        lasts = [self.last[e] for e in ENGS if e != "sp" and self.last[e] is not None]
        lasts += self.dma_ops[-ND:]
        lasts += self.cc_ops[-4:]
        for e in ENGS:
            self.add(e, lambda g: g.nop(), extra=lasts, force_signal=True)
        self.writers = {}
        self.readers = {}

    def flush(self):
        self.barrier()
        nc = self.nc
        engobj = {"pe": "tensor", "act": "scalar", "dve": "vector", "pool": "gpsimd", "sp": "sync"}

        def emit(e, g):
            known = self.known[e]
            for op in self.ops[e]:
                need = {}
                for d in op.deps:
                    if d.dma:
                        key, val = ("d", d.dsem), d.dval
                    else:
                        key, val = d.eng, d.sigval
                    if need.get(key, 0) < val:
                        need[key] = val
                for key, val in need.items():
                    if known.get(key, 0) < val:
                        if isinstance(key, tuple):
                            s = self.ccsem if key[1] == "cc" else self.dsem[key[1]]
                        else:
                            s = self.sem[key]
                        g.wait_ge(s, val)
                        known[key] = val
                ins = op.fn(g)
                if op.dsem == "cc":
                    ins.then_inc(self.ccsem, 1)
                elif op.dma:
                    ins.then_inc(self.dsem[op.dsem], 16)
                elif op.signal:
                    ins.then_inc(self.sem[e], 1)
            self.ops[e] = []

        for e in ENGS:
            for op in self.ops[e]:
                if op.signal and not op.dma:
                    self.count[e] += 1
                    op.sigval = self.count[e]
        with nc.Block() as block:
            @block.tensor
            def _(g):
                emit("pe", g)

            @block.scalar
            def _(g):
                emit("act", g)

            @block.vector
            def _(g):
                emit("dve", g)

            @block.gpsimd
            def _(g):
                emit("pool", g)

            @block.sync
            def _(g):
                emit("sp", g)

    def finish(self):
        lasts = self.dma_ops[-ND:]
        self.add("sp", lambda g: g.nop(), extra=lasts)
        self.flush()


class Rot:
    def __init__(self, tiles):
        self.tiles = tiles
        self.i = 0

    def next(self):
        t = self.tiles[self.i % len(self.tiles)]
        self.i += 1
        return t


def build_program(n_layers=1, debug=False):
    nc = bass.Bass("TRN2", target_bir_lowering=False)
    stack = ExitStack()
    with stack:
        _build(nc, stack, n_layers, debug)
    return nc


def _build(nc, stack, n_layers, debug):
    L = n_layers

    def din(name, shape, dt=F32):
        return nc.dram_tensor(name, shape, dt, kind="ExternalInput").ap()

    def dscr(name, shape, dt):
        kind = "ExternalOutput" if debug else "Internal"
        return nc.dram_tensor(name, shape, dt, kind=kind).ap()

    xa_d = din("xa", [S, D])
    X1own_t = [nc.dram_tensor("X1own%d" % i, [512, D], F32) for i in range(4)]
    X1all_t = [nc.dram_tensor("X1all%d" % i, [1024, D], F32) for i in range(4)]
    xo_d = din("xo", [2048, D])
    pf_d = din("pf", [128, 4])
    w_in_d = din("w_in", [L, D, NCOLS])
    bfg_d = din("b_forget", [L, 8, 1])
    wbr_d = din("w_branch", [L, 3, 512, D])
    wout_d = din("w_out", [L, D, D])
    ln1g_d = din("ln1_g", [L, D])
    ln1b_d = din("ln1_b", [L, D])
    wff1_d = din("w_ff1", [L, D, DFF])
    wff2_d = din("w_ff2", [L, DFF, D])
    ln2g_d = din("ln2_g", [L, D])
    ln2b_d = din("ln2_b", [L, D])
    out_d = nc.dram_tensor("out", [2048, D], F32, kind="ExternalOutput").ap()

    QT_d = dscr("QT", [12, 128, 2048], BF16)
    KT_d = dscr("KT", [12, 128, S], BF16)
    VV_d = dscr("VV", [3, S, 512], BF16)
    QI_d = dscr("QI", [8, 128, 2048], BF16)
    KI_d = dscr("KI", [128, S], BF16)
    G_d = dscr("G", [24, 128, 2048], F32)
    NBA_d = dscr("NBA", [16, 128, S], BF16)
    OT_d = dscr("OTd", [3, 4, 128, 2048], BF16)

    P = Prog(nc, stack)

    uniq = [0]

    def sb(st, name, shape, dt):
        uniq[0] += 1
        return st.enter_context(nc.sbuf_tensor("t%d_%s" % (uniq[0], name), shape, dt))

    def ps(st, name, shape, dt=F32):
        uniq[0] += 1
        return st.enter_context(nc.psum_tensor("t%d_%s" % (uniq[0], name), shape, dt))

    ident_bf = sb(stack, "ident_bf", [128, 128], BF16)
    ident_f = sb(stack, "ident_f", [128, 128], F32)
    ones_bf = sb(stack, "ones_bf", [128, 128], BF16)
    ONESF = sb(stack, "ONESF", [128, 64], F32)
    pf = sb(stack, "pf", [128, 4], F32)
    CM = sb(stack, "CM", [128, 8, 512], BF16)
    NBQK = sb(stack, "NBQK", [128, 256], F32)
    M1 = sb(stack, "M1", [128, 256], F32)
    AB = sb(stack, "AB", [128, 32, 8], F32)
    WI = sb(stack, "WI", [128, 16, 16], F32)
    KMS = sb(stack, "KMS", [128, 16], F32)
    CNtok = sb(stack, "CNtok", [128, 32, 8], F32)
    CNown = sb(stack, "CNown", [8, 2048], F32)
    PS = [ps(stack, "ps%d" % i, [128, 512]) for i in range(8)]

    with ExitStack() as st:
        onesf = sb(st, "onesf", [128, 128], F32)
        V0 = sb(st, "V0", [128, 8, 512], F32)
        V2 = sb(st, "V2", [128, 256], F32)
        KP = sb(st, "KP", [128, 32], F32)
        P.add("sp", lambda g: g.dma_start(out=pf[:], in_=pf_d[:, :]), writes=["pf"], dma=True)
        P.add("pool", lambda g: g.memset(onesf[:], 1.0), writes=["onesf"])
        P.add("pool", lambda g: g.memset(ones_bf[:], 1.0), writes=["ones_bf"])
        P.add("pool", lambda g: g.memset(ONESF[:], 1.0), writes=["ONESF"])
        P.add("pool", lambda g: g.affine_select(out=ident_f[:], in_=onesf[:], pattern=[[-1, 128]],
                                                compare_op=ALU.is_equal, fill=0.0, base=0,
                                                channel_multiplier=1),
              reads=["onesf"], writes=["ident_f"])
        P.add("pool", lambda g: g.tensor_copy(out=ident_bf[:], in_=ident_f[:]),
              reads=["ident_f"], writes=["ident_bf"])
        P.add("pool", lambda g: g.iota(V0[:].rearrange("p j (t q) -> p j t q", q=128),
                                       pattern=[[-128, 8], [256, 4], [1, 128]], base=0,
                                       channel_multiplier=-1, allow_small_or_imprecise_dtypes=True),
              writes=["V0"])
        P.add("dve", lambda g: g.tensor_scalar(out=V0[:], in0=V0[:], scalar1=pf[:, 2:3], scalar2=None,
                                               op0=ALU.add), reads=["V0", "pf"], writes=["V0"])
        P.add("dve", lambda g: g.tensor_scalar(out=CM[:], in0=V0[:], scalar1=0.0, scalar2=-BIG,
                                               op0=ALU.is_lt, op1=ALU.mult), reads=["V0"], writes=["CM"])
        P.add("pool", lambda g: g.iota(V2[:].rearrange("p (j k) -> p j k", k=128),
                                       pattern=[[-128, 2], [-1, 128]], base=0,
                                       channel_multiplier=1, allow_small_or_imprecise_dtypes=True),
              writes=["V2"])
        P.add("dve", lambda g: g.tensor_scalar(out=V2[:], in0=V2[:], scalar1=pf[:, 2:3], scalar2=None,
                                               op0=ALU.add), reads=["V2", "pf"], writes=["V2"])
        P.add("dve", lambda g: g.tensor_scalar(out=NBQK[:], in0=V2[:], scalar1=0.0, scalar2=-BIG,
                                               op0=ALU.is_lt, op1=ALU.mult), reads=["V2"], writes=["NBQK"])
        P.add("dve", lambda g: g.tensor_scalar(out=M1[:], in0=V2[:], scalar1=0.0, scalar2=None,
                                               op0=ALU.is_le), reads=["V2"], writes=["M1"])
        P.add("pool", lambda g: g.iota(KP[:], pattern=[[128, 32]], base=0, channel_multiplier=1,
                                       allow_small_or_imprecise_dtypes=True), writes=["KP"])
        for h in range(8):
            sl = 2.0 ** (-(h + 1))
            P.add("dve", lambda g, h=h, sl=sl: g.tensor_scalar(out=AB[:, :, h], in0=KP[:], scalar1=sl,
                                                               scalar2=None, op0=ALU.mult),
                  reads=["KP"], writes=["AB"])
        P.flush()

    for layer in range(L):
        _layer(nc, P, stack, layer, locals(), layer == L - 1)
    P.finish()


def _layer(nc, P, stack, layer, E, is_last):
    xa_d, X1own_t, X1all_t = E["xa_d"], E["X1own_t"], E["X1all_t"]
    xo_d, w_in_d, bfg_d, wbr_d, wout_d = E["xo_d"], E["w_in_d"], E["bfg_d"], E["wbr_d"], E["wout_d"]
    ln1g_d, ln1b_d, wff1_d, wff2_d, ln2g_d, ln2b_d, out_d = (E["ln1g_d"], E["ln1b_d"], E["wff1_d"], E["wff2_d"],
                                                            E["ln2g_d"], E["ln2b_d"], E["out_d"])
    QT_d, KT_d, VV_d, QI_d, KI_d, G_d, NBA_d = E["QT_d"], E["KT_d"], E["VV_d"], E["QI_d"], E["KI_d"], E["G_d"], E["NBA_d"]
    ident_bf, ident_f, ones_bf, pf, CM, NBQK, M1, AB = (E["ident_bf"], E["ident_f"], E["ones_bf"], E["pf"], E["CM"],
                                                        E["NBQK"], E["M1"], E["AB"])
    ONESF = E["ONESF"]
    OT_d, WI, KMS, CNtok, CNown, PS = E["OT_d"], E["WI"], E["KMS"], E["CNtok"], E["CNown"], E["PS"]
    sb, ps = E["sb"], E["ps"]
    w_in = w_in_d[layer].rearrange("(c p) n -> p c n", p=128)

    psr = Rot(list(range(8)))

    def dma(out, in_, reads, writes, eng="sp"):
        return P.add(eng, lambda g: g.dma_start(out=out, in_=in_), reads=reads, writes=writes, dma=True)

    def mm(out, lhsT, rhs, start, stop, reads, writes):
        return P.add("pe", lambda g: g.matmul(out, lhsT, rhs, start=start, stop=stop), reads=reads, writes=writes)

    st_fg = ExitStack()
    FG = sb(st_fg, "FG", [8, S], F32)
    with ExitStack() as st:
        XT = sb(st, "XT", [128, 8, S], BF16)
        XTO = sb(st, "XTO", [128, 8, 2048], BF16)
        STG = [sb(st, "stg%d" % i, [128, 8, 512], F32) for i in range(2)]
        WB = [sb(st, "wb%d" % i, [128, 8, 512], BF16) for i in range(3)]
        OUTB = [sb(st, "outb%d" % i, [128, 512], BF16) for i in range(4)]
        OUTF = [sb(st, "outf%d" % i, [128, 512], F32) for i in range(3)]
        TMPX = sb(st, "tmpx", [128, 2048], BF16)
        stg_r, wb_r, outb_r, outf_r = Rot([0, 1]), Rot([0, 1, 2]), Rot([0, 1, 2, 3]), Rot([0, 1, 2])
        evac_flip = [0]

        def evac_copy(out, in_, reads, writes):
            evac_flip[0] ^= 1
            if evac_flip[0]:
                P.add("act", lambda g: g.activation(out=out, in_=in_, func=AF.Copy), reads=reads, writes=writes)
                return "act"
            P.add("dve", lambda g: g.tensor_copy(out=out, in_=in_), reads=reads, writes=writes)
            return "act"

        for tc in range(8):
            i = stg_r.next()
            sv = STG[i][:].rearrange("p a b -> p (a b)").rearrange("p (t d) -> p t d", d=D)
            if layer == 0:
                dma(sv, xa_d[tc * 512:(tc + 1) * 512, :].rearrange("(t p) d -> p t d", p=128), [], ["stg%d" % i])
            else:
                for ii in range(2):
                    src = X1all_t[tc // 2].ap().rearrange("(r t p) d -> p t r d", r=2, p=128)[:, 2 * (tc % 2) + ii, :, :]
                    dma(sv[:, 2 * ii:2 * ii + 2, :], src, ["X1all%d" % (tc // 2)], ["stg%d" % i])
            for t in range(4):
                g_ = tc * 4 + t
                for c4 in range(2):
                    b = psr.next()
                    for cc in range(4):
                        c = c4 * 4 + cc
                        P.add("pe", lambda g, b=b, cc=cc, sv=sv, t=t, c=c: g.transpose(
                            PS[b][:, cc * 128:(cc + 1) * 128], sv[:, t, c * 128:(c + 1) * 128], ident_f[:]),
                              reads=["stg%d" % i], writes=["ps%d" % b])
                    evac_copy(XT[:, c4 * 4:c4 * 4 + 4, g_ * 128:(g_ + 1) * 128],
                              PS[b][:].rearrange("p (c q) -> p c q", q=128), ["ps%d" % b], [("XT", tc)])
        for c in range(8):
            v = XT[:, c, :].rearrange("p (i two q) -> p i two q", two=2, q=128)
            P.add("dve", lambda g, v=v: g.tensor_scalar(out=TMPX[:].rearrange("p (i q) -> p i q", q=128),
                                                        in0=v[:, :, 1, :], scalar1=pf[:, 0:1], scalar2=None,
                                                        op0=ALU.mult),
                  reads=[("XT", t) for t in range(8)], writes=["tmpx"])
            P.add("dve", lambda g, v=v, c=c: g.scalar_tensor_tensor(
                out=XTO[:, c, :].rearrange("p (i q) -> p i q", q=128), in0=v[:, :, 0, :], scalar=pf[:, 1:2],
                in1=TMPX[:].rearrange("p (i q) -> p i q", q=128), op0=ALU.mult, op1=ALU.add),
                  reads=[("XT", t) for t in range(8)] + ["tmpx"], writes=["XTO"])
        XTkeys = [("XT", t) for t in range(8)]

        def load_w(col0, n, dup=False):
            i = stg_r.next()
            j = wb_r.next()
            dma(STG[i][:, :, :n], w_in[:, :, col0:col0 + n], [], ["stg%d" % i])
            P.add("pool", lambda g: g.tensor_copy(out=WB[j][:, :, :n], in_=STG[i][:, :, :n]),
                  reads=["stg%d" % i], writes=["wb%d" % j])
            if dup:
                P.add("pool", lambda g: g.tensor_copy(out=WB[j][:, :, n:2 * n], in_=STG[i][:, :, :n]),
                      reads=["stg%d" % i], writes=["wb%d" % j])
            return j

        def proj_fm(j, ncols, own, dst_fn, gates=False, M=128):
            src = XTO if own else XT
            ntc = 4 if own else 8
            for gi in range(max(1, ncols // 128)):
                for tc in range(ntc):
                    b = psr.next()
                    for kc in range(8):
                        mm(PS[b][:M, :], WB[j][:, kc, gi * 128:gi * 128 + M], src[:, kc, tc * 512:(tc + 1) * 512],
                           kc == 0, kc == 7, ["wb%d" % j, "XTO"] + XTkeys, ["ps%d" % b])
                    if gates:
                        o = outf_r.next()
                        P.add("act", lambda g, b=b, o=o: g.activation(out=OUTF[o][:], in_=PS[b][:], func=AF.Sigmoid),
                              reads=["ps%d" % b], writes=["outf%d" % o])
                        dma(dst_fn(gi, tc), OUTF[o][:], ["outf%d" % o], [], eng="act")
                    else:
                        o = outb_r.next()
                        qe = evac_copy(OUTB[o][:M, :], PS[b][:M, :], ["ps%d" % b], ["outb%d" % o])
                        dma(dst_fn(gi, tc), OUTB[o][:M, :], ["outb%d" % o], [], eng=qe)

        def proj_tm(j, dst):
            for t in range(32):
                b = psr.next()
                for kc in range(8):
                    mm(PS[b][:], XT[:, kc, t * 128:(t + 1) * 128], WB[j][:, kc, :], kc == 0, kc == 7,
                       ["wb%d" % j] + XTkeys, ["ps%d" % b])
                o = outb_r.next()
                qe = evac_copy(OUTB[o][:], PS[b][:], ["ps%d" % b], ["outb%d" % o])
                dma(dst[t * 128:(t + 1) * 128, :], OUTB[o][:], ["outb%d" % o], [], eng=qe)

        for br, (qn, kn, vn) in enumerate((("qa", "ka", "va"), ("qf", "kf", "vf"), ("qs", "ks", "vs"))):
            j = load_w(OFF[qn], 512)
            proj_fm(j, 512, True, lambda gi, tc, br=br: QT_d[br * 4 + gi][:, tc * 512:(tc + 1) * 512])
            j = load_w(OFF[kn], 512)
            proj_fm(j, 512, False, lambda gi, tc, br=br: KT_d[br * 4 + gi][:, tc * 512:(tc + 1) * 512])
            j = load_w(OFF[vn], 512)
            proj_tm(j, VV_d[br])
        for half in range(2):
            j = load_w(OFF["qi"] + 512 * half, 512)
            proj_fm(j, 512, True, lambda gi, tc, half=half: QI_d[half * 4 + gi][:, tc * 512:(tc + 1) * 512])
        j = load_w(OFF["ki"], 64, dup=True)
        proj_fm(j, 128, False, lambda gi, tc: KI_d[:, tc * 512:(tc + 1) * 512])
        j = load_w(OFF["wi"], 16)
        for t in range(16):
            b = psr.next()
            for kc in range(8):
                mm(PS[b][:, :16], XTO[:, kc, t * 128:(t + 1) * 128], WB[j][:, kc, :16], kc == 0, kc == 7,
                   ["wb%d" % j, "XTO"], ["ps%d" % b])
            P.add("dve", lambda g, b=b, t=t: g.tensor_copy(out=WI[:, t, :], in_=PS[b][:, :16]),
                  reads=["ps%d" % b], writes=["WI"])
        j = load_w(OFF["fg"], 8)
        for tc in range(8):
            b = psr.next()
            for kc in range(8):
                mm(PS[b][:8, :], WB[j][:, kc, :8], XT[:, kc, tc * 512:(tc + 1) * 512], kc == 0, kc == 7,
                   ["wb%d" % j] + XTkeys, ["ps%d" % b])
            P.add("dve", lambda g, b=b, tc=tc: g.tensor_copy(out=FG[:, tc * 512:(tc + 1) * 512], in_=PS[b][:8, :]),
                  reads=["ps%d" % b], writes=["FG"])
        for gt in range(6):
            j = load_w(OFF["g"] + 512 * gt, 512)
            proj_fm(j, 512, True, lambda gi, tc, gt=gt: G_d[gt * 4 + gi][:, tc * 512:(tc + 1) * 512], gates=True)
        P.flush()
    with ExitStack() as st:
        LL = FG
        CN = sb(st, "CN", [8, S], F32)
        TMPC = sb(st, "tmpc", [8, 2048], F32)
        negb = sb(st, "negb", [8, 1], F32)
        dma(negb[:], bfg_d[layer], [], ["negb"])
        P.add("act", lambda g: g.activation(out=negb[:], in_=negb[:], func=AF.Copy, scale=-1.0),
              reads=["negb"], writes=["negb"])
        P.add("act", lambda g: g.activation(out=LL[:], in_=FG[:], func=AF.Exp, bias=negb[:, 0:1], scale=-1.0),
              reads=["FG", "negb"], writes=["FG"])
        P.add("act", lambda g: g.activation(out=LL[:], in_=LL[:], func=AF.Ln, bias=1.0, scale=1.0),
              reads=["FG"], writes=["FG"])
        P.add("dve", lambda g: g.tensor_tensor_scan(out=CN[:], data0=LL[:], data1=LL[:], initial=0.0,
                                                    op0=ALU.add, op1=ALU.bypass), reads=["FG"], writes=["CN"])
        b = psr.next()
        for t in range(32):
            P.add("pe", lambda g, t=t, b=b: g.transpose(PS[b][:, t * 8:(t + 1) * 8], CN[:, t * 128:(t + 1) * 128],
                                                        ident_f[:8, :8]),
                  reads=["CN"], writes=["ps%d" % b])
        P.add("dve", lambda g, b=b: g.tensor_copy(out=CNtok[:].rearrange("p t h -> p (t h)"), in_=PS[b][:, :256]),
              reads=["ps%d" % b], writes=["CNtok"])
        cv = CN[:].rearrange("p (i two q) -> p i two q", two=2, q=128)
        P.add("dve", lambda g: g.tensor_scalar(out=TMPC[:].rearrange("p (i q) -> p i q", q=128), in0=cv[:, :, 1, :],
                                               scalar1=pf[:8, 0:1], scalar2=None, op0=ALU.mult),
              reads=["CN"], writes=["tmpc"])
        P.add("dve", lambda g: g.scalar_tensor_tensor(out=CNown[:].rearrange("p (i q) -> p i q", q=128),
                                                      in0=cv[:, :, 0, :], scalar=pf[:8, 1:2],
                                                      in1=TMPC[:].rearrange("p (i q) -> p i q", q=128),
                                                      op0=ALU.mult, op1=ALU.add),
              reads=["CN", "tmpc"], writes=["CNown"])
        P.flush()
    st_fg.close()

    with ExitStack() as st:
        QI = sb(st, "QIs", [128, 8, 2048], BF16)
        KI = sb(st, "KIs", [128, S], BF16)
        SC = [sb(st, "SC%d" % i, [128, S], F32) for i in range(2)]
        TH = [sb(st, "TH%d" % i, [128, 512], F32) for i in range(3)]
        NBo = [sb(st, "NBo%d" % i, [128, S], BF16) for i in range(2)]
        JUNK = sb(st, "JUNK", [128, S], BF16)
        KPOS = sb(st, "KPOS", [128, S], F32)
        KSEL = sb(st, "KSEL", [128, S], F32)
        WABS = sb(st, "WABS", [128, 16, 16], F32)
        WSGN = sb(st, "WSGN", [128, 16, 16], F32)
        SM = sb(st, "SM", [128, 8], F32)
        for pr in range(8):
            dma(QI[:, pr, :], QI_d[pr], [], [("QI", pr)])
        dma(KI[:], KI_d[:, :], [], ["KI"])
        P.add("pool", lambda g: g.iota(KPOS[:], pattern=[[1, S]], base=0, channel_multiplier=0,
                                       allow_small_or_imprecise_dtypes=True), writes=["KPOS"])
        P.add("act", lambda g: g.activation(out=WABS[:], in_=WI[:], func=AF.Abs), reads=["WI"], writes=["WABS"])
        P.add("dve", lambda g: g.tensor_scalar(out=WSGN[:], in0=WI[:], scalar1=0.0, scalar2=2.0, op0=ALU.is_ge,
                                               op1=ALU.mult), reads=["WI"], writes=["WSGN"])
        P.add("dve", lambda g: g.tensor_scalar(out=WSGN[:], in0=WSGN[:], scalar1=-1.0, scalar2=None, op0=ALU.add),
              reads=["WSGN"], writes=["WSGN"])
        th_r = Rot([0, 1, 2])
        NSC = 3
        SC.append(sb(st, "SC2", [128, S], F32))
        JUNKA = sb(st, "JUNKA", [128, S], BF16)
        SMA = sb(st, "SMA", [128, 8], F32)

        def acc(i):
            nk = 128 * (2 * i + 2)
            sc = SC[i % NSC]
            sck = "SC%d" % (i % NSC)
            for kc in range((nk + 511) // 512):
                k0 = kc * 512
                kn = min(512, nk - k0)
                for h in range(16):
                    r0 = 64 * (h % 2)
                    b = psr.next()
                    mm(PS[b][:, :kn], QI[r0:r0 + 64, h // 2, i * 128:(i + 1) * 128], KI[r0:r0 + 64, k0:k0 + kn],
                       True, True, [("QI", h // 2), "KI"], ["ps%d" % b])
                    t = th_r.next()
                    P.add("act", lambda g, b=b, t=t, kn=kn, i=i, h=h: g.activation(
                        out=TH[t][:, :kn], in_=PS[b][:, :kn], func=AF.Relu, scale=WABS[:, i, h:h + 1]),
                          reads=["ps%d" % b, "WABS"], writes=["TH%d" % t])
                    if h == 0:
                        P.add("dve", lambda g, t=t, kn=kn, k0=k0, i=i, h=h, sc=sc: g.tensor_scalar(
                            out=sc[:, k0:k0 + kn], in0=TH[t][:, :kn], scalar1=WSGN[:, i, h:h + 1], scalar2=None,
                            op0=ALU.mult), reads=["TH%d" % t, "WSGN"], writes=[sck])
                    else:
                        P.add("dve", lambda g, t=t, kn=kn, k0=k0, i=i, h=h, sc=sc: g.scalar_tensor_tensor(
                            out=sc[:, k0:k0 + kn], in0=TH[t][:, :kn], scalar=WSGN[:, i, h:h + 1],
                            in1=sc[:, k0:k0 + kn], op0=ALU.mult, op1=ALU.add),
                              reads=["TH%d" % t, "WSGN", sck], writes=[sck])
            P.add("dve", lambda g, sc=sc, i=i: g.tensor_tensor(out=sc[:, 256 * i:256 * i + 256],
                                                              in0=sc[:, 256 * i:256 * i + 256], in1=NBQK[:],
                                                              op=ALU.add), reads=[sck], writes=[sck])

        def ctx(i):
            nk = 128 * (2 * i + 2)
            sc = SC[i % NSC]
            sck = "SC%d" % (i % NSC)
            use_act = (i % 2 == 1)
            smk = "SMA" if use_act else "SM"
            smt = SMA if use_act else SM
            return (nk, sc, sck, use_act, smk) + tuple(smt[:, c:c + 1] for c in range(6))

        def pre(i):
            nk, sc, sck, use_act, smk, lo, w0, mid, cnt, step, hi = ctx(i)
            if i == 0:
                P.add("dve", lambda g, lo=lo: g.memset(lo, -BIG / 2), writes=[smk])
            else:
                P.add("dve", lambda g, sc=sc, i=i, lo=lo: g.tensor_reduce(out=lo, in_=sc[:, :256 * i],
                                                                          axis=mybir.AxisListType.X, op=ALU.min),
                      reads=[sck], writes=[smk])
                P.add("dve", lambda g, sc=sc, nk=nk, hi=hi: g.tensor_reduce(out=hi, in_=sc[:, :nk],
                                                                            axis=mybir.AxisListType.X, op=ALU.max),
                      reads=[sck], writes=[smk])
                P.add("dve", lambda g, w0=w0, hi=hi, lo=lo: g.tensor_tensor(out=w0, in0=hi, in1=lo, op=ALU.subtract),
                      reads=[smk], writes=[smk])
                if use_act:
                    P.add("dve", lambda g, nlo=lo: g.tensor_scalar(out=nlo, in0=nlo, scalar1=-1.0, scalar2=None,
                                                                   op0=ALU.mult), reads=[smk], writes=[smk])

        def chain(i):
            nk, sc, sck, use_act, smk, lo, w0, mid, cnt, step, hi = ctx(i)
            if i > 0:
                if not use_act:
                    for it in range(NIT):
                        f = 2.0 ** (-(it + 1))
                        P.add("dve", lambda g, f=f, mid=mid, w0=w0, lo=lo: g.scalar_tensor_tensor(
                            out=mid, in0=w0, scalar=f, in1=lo, op0=ALU.mult, op1=ALU.add), reads=[smk], writes=[smk])
                        P.add("dve", lambda g, sc=sc, nk=nk, mid=mid, cnt=cnt: g.tensor_scalar(
                            out=JUNK[:, :nk], in0=sc[:, :nk], scalar1=mid, scalar2=None, op0=ALU.is_ge, op1=ALU.add,
                            accum_out=cnt), reads=[sck, smk], writes=["JUNK", smk])
                        P.add("dve", lambda g, f=f, step=step, cnt=cnt: g.tensor_scalar(
                            out=step, in0=cnt, scalar1=255.5, scalar2=f, op0=ALU.is_ge, op1=ALU.mult),
                              reads=[smk], writes=[smk])
                        P.add("dve", lambda g, lo=lo, w0=w0, step=step: g.scalar_tensor_tensor(
                            out=lo, in0=w0, scalar=step, in1=lo, op0=ALU.mult, op1=ALU.add),
                              reads=[smk], writes=[smk])
                else:
                    nlo, nmid, ssum, sg, tt = lo, mid, cnt, step, hi
                    for it in range(NIT):
                        f = 2.0 ** (-(it + 1))
                        P.add("act", lambda g, f=f, nmid=nmid, w0=w0, nlo=nlo: g.activation(
                            out=nmid, in_=w0, func=AF.Identity, scale=-f, bias=nlo), reads=[smk], writes=[smk])
                        P.add("act", lambda g, sc=sc, nk=nk, nmid=nmid, ssum=ssum: g.activation(
                            out=JUNKA[:, :nk], in_=sc[:, :nk], func=AF.Sign, scale=1.0, bias=nmid, accum_out=ssum),
                              reads=[sck, smk], writes=["JUNKA", smk])
                        P.add("act", lambda g, sg=sg, ssum=ssum, nk=nk: g.activation(
                            out=sg, in_=ssum, func=AF.Sign, scale=1.0, bias=SGB[:, (nk // 256) - 1:(nk // 256)]),
                              reads=[smk, "SGB"], writes=[smk])
                        P.add("act", lambda g, f=f, tt=tt, sg=sg, it=it: g.activation(
                            out=tt, in_=sg, func=AF.Identity, scale=-f / 2, bias=FB[:, it:it + 1]),
                              reads=[smk, "FB"], writes=[smk])
                        P.add("act", lambda g, nlo=nlo, w0=w0, tt=tt: g.activation(
                            out=nlo, in_=w0, func=AF.Identity, scale=tt, bias=nlo), reads=[smk], writes=[smk])

        def post(i):
            nk, sc, sck, use_act, smk, lo, w0, mid, cnt, step, hi = ctx(i)
            if use_act and i > 0:
                P.add("dve", lambda g, nlo=lo: g.tensor_scalar(out=nlo, in0=nlo, scalar1=-1.0, scalar2=None,
                                                               op0=ALU.mult), reads=[smk], writes=[smk])
            nbo = NBo[i % 2]
            nbk = "NBo%d" % (i % 2)
            P.add("dve", lambda g, nbo=nbo, sc=sc, nk=nk, lo=lo: g.tensor_scalar(
                out=nbo[:, :nk], in0=sc[:, :nk], scalar1=lo, scalar2=-BIG, op0=ALU.is_lt, op1=ALU.mult),
                  reads=[sck, smk], writes=[nbk])
            P.add("pool", lambda g, nbo=nbo, nk=nk: g.tensor_tensor(out=KSEL[:, :nk], in0=nbo[:, :nk], in1=KPOS[:, :nk],
                                                                   op=ALU.add), reads=[nbk, "KPOS"], writes=["KSEL"])
            P.add("dve", lambda g, nk=nk, i=i: g.tensor_reduce(out=KMS[:, i:i + 1], in_=KSEL[:, :nk],
                                                               axis=mybir.AxisListType.X, op=ALU.max),
                  reads=["KSEL"], writes=["KMS"])
            dma(NBA_d[i][:, :nk], nbo[:, :nk], [nbk], [])

        SGB = sb(st, "SGB", [128, 16], F32)
        FB = sb(st, "FB", [128, NIT], F32)
        for j in range(16):
            P.add("pool", lambda g, j=j: g.memset(SGB[:, j:j + 1], 256.0 * (j + 1) - 511.5), writes=["SGB"])
        for it in range(NIT):
            P.add("pool", lambda g, it=it: g.memset(FB[:, it:it + 1], -(2.0 ** (-(it + 2)))), writes=["FB"])
        for pp in range(8):
            acc(2 * pp)
            acc(2 * pp + 1)
            pre(2 * pp)
            pre(2 * pp + 1)
            chain(2 * pp)
            chain(2 * pp + 1)
            post(2 * pp)
            post(2 * pp + 1)
        P.flush()

    for br in range(2):
        with ExitStack() as st:
            KT = [sb(st, "KTa%d" % i, [128, S], BF16) for i in range(2)]
            VT = [sb(st, "VTa%d" % i, [128, 32, 128], BF16) for i in range(2)]
            QT = [sb(st, "QTa%d" % i, [128, 2048], BF16) for i in range(2)]
            SROW = [sb(st, "SROW%d" % i, [128, 512], F32) for i in range(2)]
            PT = [sb(st, "PT%d" % i, [128, 512], BF16) for i in range(4)]
            RS = [sb(st, "RS%d" % i, [128, 512], F32) for i in range(2)]
            OTS = [sb(st, "OTS%d" % i, [128, 512], BF16) for i in range(2)]
            NB4 = [sb(st, "NB4%d" % i, [128, 4, S], BF16) for i in range(2)] if br == 0 else None
            srow_r, ots_r = Rot([0, 1]), Rot([0, 1])
            pt_r, rs_r, nb_r = Rot([0, 1, 2, 3]), Rot([0, 1]), Rot([0, 1])
            sps = Rot([0, 1, 2])
            ops = Rot([3, 4, 5])
            BK = AB if br == 0 else CNtok
            nb_map = {}

            def nb_load(idx):
                qc_ = idx % 4
                n4_ = nb_r.next()
                nb_map[idx] = n4_
                for t in range(4):
                    nk = 128 * (2 * (4 * qc_ + t) + 2)
                    dma(NB4[n4_][:, t, :nk], NBA_d[4 * qc_ + t][:, :nk], [], [("NB4", n4_, t)])

            for i in range(2):
                P.add("pool", lambda g, i=i: g.memset(KT[i][64:128, :], 0.0), writes=["KTa%d" % i])
                P.add("pool", lambda g, i=i: g.memset(KT[i][64:65, :], 1.0), writes=["KTa%d" % i])
                P.add("pool", lambda g, i=i: g.memset(QT[i][64:128, :], 0.0), writes=["QTa%d" % i])
                P.add("pool", lambda g, i=i: g.memset(VT[i][:, :, 64:128], 0.0), writes=["VTa%d" % i])
                P.add("pool", lambda g, i=i: g.memset(VT[i][:, :, 64:65], 1.0), writes=["VTa%d" % i])
            def main_loop():
              for h in range(8):
                pr, hh = h // 2, h % 2
                r0 = 64 * hh
                kt, vt, qt = KT[h % 2], VT[h % 2], QT[h % 2]
                kk, vk, qk = "KTa%d" % (h % 2), "VTa%d" % (h % 2), "QTa%d" % (h % 2)
                dma(kt[0:64, :], KT_d[br * 4 + pr][r0:r0 + 64, :], [], [kk])
                dma(vt[:, :, 0:64], VV_d[br].rearrange("(t p) c -> p t c", p=128)[:, :, h * 64:(h + 1) * 64], [], [vk])
                dma(qt[0:64, :], QT_d[br * 4 + pr][r0:r0 + 64, :], [], [qk])
                for qc in range(4):
                    if br == 0:
                        sl = 2.0 ** (-(h + 1))
                        for t in range(4):
                            P.add("pe", lambda g, t=t, qc=qc: g.matmul(
                                PS[7][64:65, t * 128:(t + 1) * 128], KMS[:, 4 * qc + t:4 * qc + t + 1], ident_f[:, :],
                                start=True, stop=True), reads=["KMS"], writes=["ps7"])
                        P.add("act", lambda g, qc=qc, sl=sl, qt=qt: g.activation(
                            out=qt[64:65, qc * 512:(qc + 1) * 512], in_=PS[7][64:65, :], func=AF.Copy,
                            scale=-8.0 * sl), reads=["ps7"], writes=[qk])
                    else:
                        P.add("pe", lambda g, h=h, qc=qc: g.matmul(PS[7][64:65, :], ident_f[:8, h:h + 1],
                                                                    CNown[:, qc * 512:(qc + 1) * 512],
                                                                    start=True, stop=True),
                              reads=["CNown"], writes=["ps7"])
                        P.add("act", lambda g, qc=qc, qt=qt: g.activation(
                            out=qt[64:65, qc * 512:(qc + 1) * 512], in_=PS[7][64:65, :], func=AF.Copy, scale=-8.0),
                              reads=["ps7"], writes=[qk])
                for qc in range(4):
                    nkt = 8 * qc + 8
                    n4 = None
                    if br == 0:
                        if h == 0 and qc == 0:
                            nb_load(0)
                        n4 = nb_map[h * 4 + qc]
                        if h * 4 + qc + 1 < 32:
                            nb_load(h * 4 + qc + 1)
                    oa = ops.next()
                    ptmap = {}

                    def sgrp(kti, h=h, qc=qc, n4=n4, kt=kt, qt=qt, kk=kk, qk=qk):
                        s = sps.next()
                        j = kti - 8 * qc
                        extra = []
                        if j >= 0:
                            extra.append(("cm", j))
                        if br == 0:
                            for t in range(4):
                                if kti <= 2 * (4 * qc + t) + 1:
                                    extra.append(("nb", t))
                        mm(PS[s][:], kt[:, kti * 128:(kti + 1) * 128], qt[:, qc * 512:(qc + 1) * 512], True,
                           len(extra) == 0, [kk, qk], ["ps%d" % s])
                        for ei, ex in enumerate(extra):
                            lastf = ei == len(extra) - 1
                            if ex[0] == "cm":
                                mm(PS[s][:], ident_bf[:], CM[:, ex[1], :], False, lastf, ["CM"], ["ps%d" % s])
                            else:
                                t = ex[1]
                                mm(PS[s][:, t * 128:(t + 1) * 128], NB4[n4][:, t, kti * 128:(kti + 1) * 128],
                                   ident_bf[:], False, lastf, [("NB4", n4, t)], ["ps%d" % s])
                        pt = pt_r.next()
                        ptmap[kti] = pt
                        P.add("act", lambda g, s=s, pt=pt, kti=kti, h=h: g.activation(
                            out=PT[pt][:], in_=PS[s][:], func=AF.Exp, bias=BK[:, kti, h:h + 1], scale=0.125),
                              reads=["ps%d" % s, "CNtok"], writes=["PT%d" % pt])

                    def pvg(kti, vt=vt, vk=vk, oa=oa, nkt=nkt):
                        pt = ptmap[kti]
                        mm(PS[oa][:], vt[:, kti, :], PT[pt][:], kti == 0, kti == nkt - 1,
                           [vk, "PT%d" % pt], ["ps%d" % oa])

                    LA = 2
                    for kti in range(min(LA, nkt)):
                        sgrp(kti)
                    if pending:
                        pending.pop()()
                    for kti in range(nkt):
                        if kti + LA < nkt:
                            sgrp(kti + LA)
                        pvg(kti)
                    pending.append(lambda oa=oa, pr=pr, r0=r0, qc=qc: finalize(oa, pr, r0, qc))

            def finalize(oa, pr, r0, qc):
                if True:
                    sr = srow_r.next()
                    r = rs_r.next()
                    P.add("act", lambda g, sr=sr, oa=oa: g.activation(out=SROW[sr][64:65, :], in_=PS[oa][64:65, :],
                                                                      func=AF.Copy),
                          reads=["ps%d" % oa], writes=["SROW%d" % sr])
                    P.add("dve", lambda g, sr=sr: g.reciprocal(out=SROW[sr][64:65, :], in_=SROW[sr][64:65, :]),
                          reads=["SROW%d" % sr], writes=["SROW%d" % sr])
                    P.add("pe", lambda g, sr=sr: g.matmul(PS[6][:64, :], ONESF[64:65, :], SROW[sr][64:65, :],
                                                          start=True, stop=True),
                          reads=["SROW%d" % sr], writes=["ps6"])
                    P.add("act", lambda g, r=r: g.activation(out=RS[r][:64, :], in_=PS[6][:64, :], func=AF.Copy),
                          reads=["ps6"], writes=["RS%d" % r])
                    o = ots_r.next()
                    P.add("dve", lambda g, r=r, oa=oa, o=o: g.tensor_tensor(
                        out=OTS[o][:64, :], in0=PS[oa][:64, :], in1=RS[r][:64, :], op=ALU.mult),
                          reads=["ps%d" % oa, "RS%d" % r], writes=["OTS%d" % o])
                    dma(OT_d[br, pr][r0:r0 + 64, qc * 512:(qc + 1) * 512], OTS[o][:64, :],
                        ["OTS%d" % o], [], eng="act")

            pending = []
            main_loop()
            if pending:
                pending.pop()()
            P.flush()

    with ExitStack() as st:
        NSET = 4
        KT = [sb(st, "KTc%d" % i, [128, S], BF16) for i in range(1)] * 2
        VT = [sb(st, "VTc%d" % i, [128, 32, 128], BF16) for i in range(2)]
        QT = [sb(st, "QTc%d" % i, [128, 2048], BF16) for i in range(1)] * 2
        NSG = 3
        SG = [sb(st, "SG%d" % i, [128, S], F32) for i in range(NSG)]
        INC = [sb(st, "INC%d" % i, [128, S + 1], F32) for i in range(NSET)]
        AA = [sb(st, "AA%d" % i, [128, S], BF16) for i in range(NSET)]
        ATS = [sb(st, "ATS%d" % i, [128, 512], BF16) for i in range(3)]
        OTS = [sb(st, "OTSc%d" % i, [128, 128], BF16) for i in range(2)]
        ots_r = Rot([0, 1])
        zr, at_r, ats_r, oc_r = Rot([0, 1, 2]), Rot([3, 4]), Rot([0, 1, 2]), Rot([5, 6])
        items = [(pr, hh, i) for pr in range(4) for hh in range(2) for i in range(16)]

        def stage_a(n):
            pr, hh, i = items[n]
            kt, qt = KT[pr % 2], QT[pr % 2]
            kk, vk, qk = "KTc0", "VTc%d" % (pr % 2), "QTc0"
            if hh == 0 and i == 0:
                dma(kt[:], KT_d[8 + pr], [], [kk])
                dma(VT[pr % 2][:], VV_d[2].rearrange("(t p) c -> p t c", p=128)[:, :, pr * 128:(pr + 1) * 128], [], [vk])
                dma(qt[:], QT_d[8 + pr], [], [qk])
            r0 = 64 * hh
            nk = 128 * (2 * i + 2)
            sg, inc, aa = SG[n % NSG], INC[n % NSET], AA[n % NSET]
            sgk, inck, aak = "SG%d" % (n % NSG), "INC%d" % (n % NSET), "AA%d" % (n % NSET)
            for kc in range((nk + 511) // 512):
                k0 = kc * 512
                kn = min(512, nk - k0)
                b = zr.next()
                mm(PS[b][:, :kn], qt[r0:r0 + 64, i * 128:(i + 1) * 128], kt[r0:r0 + 64, k0:k0 + kn],
                   True, True, [kk, qk], ["ps%d" % b])
                P.add("act", lambda g, b=b, kn=kn, k0=k0, sg=sg: g.activation(
                    out=sg[:, k0:k0 + kn], in_=PS[b][:, :kn], func=AF.Sigmoid, scale=-0.125),
                      reads=["ps%d" % b], writes=[sgk])
            P.add("dve", lambda g, sg=sg, i=i: g.tensor_tensor(out=sg[:, 256 * i:256 * i + 256],
                                                                in0=sg[:, 256 * i:256 * i + 256], in1=M1[:],
                                                                op=ALU.max), reads=[sgk], writes=[sgk])
            P.add("pool", lambda g, inc=inc, nk=nk: g.memset(inc[:, nk:nk + 1], 1.0), writes=[inck])
            P.add("dve", lambda g, inc=inc, sg=sg, nk=nk: g.tensor_tensor_scan(
                out=inc[:, 0:nk][:, ::-1], data0=sg[:, 0:nk][:, ::-1],
                data1=sg[:, 0:nk][:, ::-1], initial=1.0, op0=ALU.mult, op1=ALU.bypass),
                  reads=[sgk, inck], writes=[inck])
            P.add("pool", lambda g, aa=aa, inc=inc, nk=nk: g.tensor_tensor(
                out=aa[:, :nk], in0=inc[:, 1:nk + 1], in1=inc[:, 0:nk], op=ALU.subtract),
                  reads=[inck], writes=[aak])

        def stage_b(n):
            pr, hh, i = items[n]
            vt = VT[pr % 2]
            vk = "VTc%d" % (pr % 2)
            r0 = 64 * hh
            aa = AA[n % NSET]
            aak = "AA%d" % (n % NSET)
            oc = oc_r.next()
            nkt = 2 * i + 2
            ng = (nkt + 3) // 4
            st_ = {}

            def tr(g4):
                n4 = min(4, nkt - 4 * g4)
                a = at_r.next()
                apv = PS[a][:].bitcast(BF16)
                for u in range(n4):
                    kti = 4 * g4 + u
                    P.add("pe", lambda g, apv=apv, u=u, kti=kti: g.transpose(
                        apv[:, u * 128:(u + 1) * 128], aa[:, kti * 128:(kti + 1) * 128], ident_bf[:]),
                          reads=[aak], writes=["ps%d" % a])
                s_ = ats_r.next()
                if True:
                    P.add("act", lambda g, s_=s_, apv=apv, n4=n4: g.activation(
                        out=ATS[s_][:, :n4 * 128], in_=apv[:, :n4 * 128], func=AF.Copy),
                          reads=["ps%d" % a], writes=["ATS%d" % s_])
                else:
                    P.add("dve", lambda g, s_=s_, apv=apv, n4=n4: g.tensor_copy(
                        out=ATS[s_][:, :n4 * 128], in_=apv[:, :n4 * 128]),
                          reads=["ps%d" % a], writes=["ATS%d" % s_])
                st_[g4] = (s_, n4)

            def pv(g4):
                s_, n4 = st_[g4]
                for u in range(n4):
                    kti = 4 * g4 + u
                    mm(PS[oc][:, :128], vt[:, kti, :], ATS[s_][:, u * 128:(u + 1) * 128], kti == 0,
                       kti == nkt - 1, [vk, "ATS%d" % s_], ["ps%d" % oc])

            tr(0)
            for g4 in range(ng):
                if g4 + 1 < ng:
                    tr(g4 + 1)
                pv(g4)
            o = ots_r.next()
            P.add("act", lambda g, oc=oc, r0=r0, o=o: g.activation(
                out=OTS[o][r0:r0 + 64, :], in_=PS[oc][r0:r0 + 64, :128], func=AF.Copy),
                  reads=["ps%d" % oc], writes=["OTSc%d" % o])
            dma(OT_d[2, pr][r0:r0 + 64, i * 128:(i + 1) * 128], OTS[o][r0:r0 + 64, :], ["OTSc%d" % o], [],
                eng="act")

        NI = len(items)
        DEPTH_A = NSET - 1
        for n in range(DEPTH_A):
            stage_a(n)
        for n in range(NI):
            stage_b(n)
            if n + DEPTH_A < NI:
                stage_a(n + DEPTH_A)
        P.flush()

    with ExitStack() as st:
        STG = [sb(st, "tstg%d" % i, [128, 8, 512], F32) for i in range(2)]
        WB = [sb(st, "twb%d" % i, [128, 8, 512], BF16) for i in range(3)]
        OTc = [sb(st, "OTc%d" % i, [128, 4, 512], BF16) for i in range(3)]
        GT = [sb(st, "GT%d" % i, [128, 512], F32) for i in range(3)]
        MF = sb(st, "MF", [128, 512], F32)
        MT = sb(st, "MT", [128, 8, 512], BF16)
        XO = sb(st, "XO", [128, 4, D], F32)
        X1 = sb(st, "X1", [128, 4, D], F32)
        X1T = sb(st, "X1T", [128, 8, 512], BF16)
        HR = [sb(st, "HR%d" % i, [128, 512], F32) for i in range(2)]
        HT = sb(st, "HT", [128, 32, 512], BF16)
        XN = sb(st, "XN", [128, D], F32)
        ST6 = sb(st, "ST6", [128, 2, 6], F32)
        MV = sb(st, "MV", [128, 4], F32)
        LNP = sb(st, "LNP", [128, 4, D], F32)
        stg_r, wb_r, gt_r, hr_r = Rot([0, 1]), Rot([0, 1, 2]), Rot([0, 1, 2]), Rot([0, 1])
        for n, src in enumerate((ln1g_d, ln1b_d, ln2g_d, ln2b_d)):
            dma(LNP[:, n, :], src[layer].partition_broadcast(128), [], ["LNP"])

        cast_flip = [0]

        def load_wt(src3, kc, n):
            i = stg_r.next()
            j = wb_r.next()
            dma(STG[i][:, :kc, :n], src3, [], ["tstg%d" % i])
            cast_flip[0] ^= 1
            if cast_flip[0]:
                P.add("dve", lambda g: g.tensor_copy(out=WB[j][:, :kc, :n], in_=STG[i][:, :kc, :n]),
                      reads=["tstg%d" % i], writes=["twb%d" % j])
            else:
                P.add("act", lambda g: g.activation(out=WB[j][:, :kc, :n], in_=STG[i][:, :kc, :n], func=AF.Copy),
                      reads=["tstg%d" % i], writes=["twb%d" % j])
            return j

        def layer_norm(src, dst, gi, key_src, key_dst):
            for hf in range(2):
                P.add("dve", lambda g, hf=hf: g.bn_stats(out=ST6[:, hf, :], in_=src[:, hf * 512:(hf + 1) * 512]),
                      reads=[key_src], writes=["ST6"])
            P.add("dve", lambda g: g.bn_aggr(out=MV[:, 0:2], in_=ST6[:].rearrange("p a b -> p (a b)")),
                  reads=["ST6"], writes=["MV"])
            P.add("act", lambda g: g.activation(out=MV[:, 2:3], in_=MV[:, 1:2], func=AF.Sqrt, bias=EPSB[:, 0:1],
                                                scale=1.0), reads=["MV", "EPSB"], writes=["MV"])
            P.add("dve", lambda g: g.reciprocal(out=MV[:, 3:4], in_=MV[:, 2:3]), reads=["MV"], writes=["MV"])
            P.add("dve", lambda g: g.tensor_scalar(out=XN[:], in0=src, scalar1=MV[:, 0:1], scalar2=MV[:, 3:4],
                                                   op0=ALU.subtract, op1=ALU.mult),
                  reads=[key_src, "MV"], writes=["XN"])
            P.add("dve", lambda g: g.tensor_tensor(out=XN[:], in0=XN[:], in1=LNP[:, gi, :], op=ALU.mult),
                  reads=["XN", "LNP"], writes=["XN"])
            P.add("dve", lambda g: g.tensor_tensor(out=dst, in0=XN[:], in1=LNP[:, gi + 1, :], op=ALU.add),
                  reads=["XN", "LNP"], writes=[key_dst])

        EPSB = sb(st, "EPSB", [128, 1], F32)
        P.add("pool", lambda g: g.memset(EPSB[:], EPS), writes=["EPSB"])
        wbr = wbr_d[layer]
        wout = wout_d[layer].rearrange("(c p) n -> p c n", p=128)
        wff1 = wff1_d[layer].rearrange("(c p) n -> p c n", p=128)
        wff2 = wff2_d[layer].rearrange("(c p) n -> p c n", p=128)
        for tcx in range(4):
            t0 = tcx * 512
            if layer == 0:
                dma(XO[:], xo_d[t0:t0 + 512, :].rearrange("(t p) d -> p t d", p=128), [], ["XO"])
            else:
                dma(XO[:], X1own_t[tcx].ap().rearrange("(t p) d -> p t d", p=128), ["X1own%d" % tcx], ["XO"])
            for bi in range(3):
                dma(OTc[bi][:], OT_d[bi].rearrange("w p t -> p w t")[:, :, t0:t0 + 512], [], [("OTc", bi)])
            for dh in range(2):
                wj = [load_wt(wbr[bi].rearrange("(c p) n -> p c n", p=128)[:, :, dh * 512:(dh + 1) * 512], 4, 512)
                      for bi in range(3)]
                for dq in range(4):
                    dc = dh * 4 + dq
                    for bi in range(3):
                        gt = gt_r.next()
                        dma(GT[gt][:], G_d[bi * 8 + dc][:, t0:t0 + 512], [], ["GT%d" % gt])
                        b = psr.next()
                        for wc in range(4):
                            mm(PS[b][:], WB[wj[bi]][:, wc, dq * 128:(dq + 1) * 128], OTc[bi][:, wc, :],
                               wc == 0, wc == 3, ["twb%d" % wj[bi], ("OTc", bi)], ["ps%d" % b])
                        if bi == 0:
                            P.add("dve", lambda g, b=b, gt=gt: g.tensor_tensor(out=MF[:], in0=PS[b][:], in1=GT[gt][:],
                                                                               op=ALU.mult),
                                  reads=["ps%d" % b, "GT%d" % gt], writes=["MF"])
                        else:
                            P.add("dve", lambda g, b=b, gt=gt: g.tensor_tensor(out=GT[gt][:], in0=PS[b][:],
                                                                               in1=GT[gt][:], op=ALU.mult),
                                  reads=["ps%d" % b, "GT%d" % gt], writes=["GT%d" % gt])
                            if bi == 1:
                                P.add("pool", lambda g, gt=gt: g.tensor_tensor(out=MF[:], in0=MF[:], in1=GT[gt][:],
                                                                               op=ALU.add),
                                      reads=["MF", "GT%d" % gt], writes=["MF"])
                            else:
                                P.add("pool", lambda g, gt=gt, dc=dc: g.tensor_tensor(out=MT[:, dc, :], in0=MF[:],
                                                                                      in1=GT[gt][:], op=ALU.add),
                                      reads=["MF", "GT%d" % gt], writes=["MT"])
            wo = [load_wt(wout[:, :, hf * 512:(hf + 1) * 512], 8, 512) for hf in range(2)]
            for t in range(4):
                for hf in range(2):
                    b = psr.next()
                    for dc in range(8):
                        mm(PS[b][:], MT[:, dc, t * 128:(t + 1) * 128], WB[wo[hf]][:, dc, :], dc == 0, dc == 7,
                           ["MT", "twb%d" % wo[hf]], ["ps%d" % b])
                    P.add("dve", lambda g, b=b, t=t, hf=hf: g.scalar_tensor_tensor(
                        out=X1[:, t, hf * 512:(hf + 1) * 512], in0=XO[:, t, hf * 512:(hf + 1) * 512], scalar=ALPHA,
                        in1=PS[b][:], op0=ALU.mult, op1=ALU.add), reads=["ps%d" % b, "XO"], writes=[("X1", t)])
            for t in range(4):
                layer_norm(X1[:, t, :], X1[:, t, :], 0, ("X1", t), ("X1", t))
            for t in range(4):
                for dc in range(8):
                    if dc % 4 == 0:
                        b = psr.next()
                    P.add("pe", lambda g, b=b, t=t, dc=dc: g.transpose(PS[b][:, (dc % 4) * 128:(dc % 4 + 1) * 128],
                                                                       X1[:, t, dc * 128:(dc + 1) * 128], ident_f[:]),
                          reads=[("X1", t)], writes=["ps%d" % b])
                    if dc % 4 == 3:
                        d0 = dc - 3
                        P.add("act", lambda g, b=b, t=t, d0=d0: g.activation(
                            out=X1T[:, d0:d0 + 4, t * 128:(t + 1) * 128],
                            in_=PS[b][:].rearrange("p (c q) -> p c q", q=128), func=AF.Copy),
                              reads=["ps%d" % b], writes=["X1T"])
            for ft in range(8):
                j = load_wt(wff1[:, :, ft * 512:(ft + 1) * 512], 8, 512)
                for fq in range(4):
                    fc = ft * 4 + fq
                    b = psr.next()
                    for dc in range(8):
                        mm(PS[b][:], WB[j][:, dc, fq * 128:(fq + 1) * 128], X1T[:, dc, :], dc == 0, dc == 7,
                           ["twb%d" % j, "X1T"], ["ps%d" % b])
                    hr = hr_r.next()
                    P.add("act", lambda g, b=b, hr=hr: g.activation(out=HR[hr][:], in_=PS[b][:], func=AF.Relu),
                          reads=["ps%d" % b], writes=["HR%d" % hr])
                    P.add("dve", lambda g, hr=hr, fc=fc: g.tensor_tensor(out=HT[:, fc, :], in0=HR[hr][:],
                                                                          in1=HR[hr][:], op=ALU.mult),
                          reads=["HR%d" % hr], writes=["HT"])
            ybanks = [psr.next() for _ in range(8)]
            for kg in range(4):
                for hf in range(2):
                    j = load_wt(wff2[:, kg * 8:(kg + 1) * 8, hf * 512:(hf + 1) * 512], 8, 512)
                    for t in range(4):
                        b = ybanks[t * 2 + hf]
                        for k8 in range(8):
                            fc = kg * 8 + k8
                            mm(PS[b][:], HT[:, fc, t * 128:(t + 1) * 128], WB[j][:, k8, :], fc == 0, fc == 31,
                               ["HT", "twb%d" % j], ["ps%d" % b])
            for t in range(4):
                for hf in range(2):
                    b = ybanks[t * 2 + hf]
                    P.add("dve", lambda g, b=b, t=t, hf=hf: g.scalar_tensor_tensor(
                        out=XO[:, t, hf * 512:(hf + 1) * 512], in0=X1[:, t, hf * 512:(hf + 1) * 512], scalar=ALPHA,
                        in1=PS[b][:], op0=ALU.mult, op1=ALU.add), reads=["ps%d" % b, ("X1", t)], writes=["XO"])
                layer_norm(XO[:, t, :], XO[:, t, :], 2, "XO", "XO")
            if is_last:
                dma(out_d[t0:t0 + 512, :].rearrange("(t p) d -> p t d", p=128), XO[:], ["XO"], [], eng="act")
            else:
                dma(X1own_t[tcx].ap().rearrange("(t p) d -> p t d", p=128), XO[:], ["XO"], ["X1own%d" % tcx],
                    eng="act")
                P.add("pool", lambda g, tcx=tcx: g.collective_compute(
                    "AllGather", ALU.bypass, replica_groups=[[0, 1], [2, 3], [4, 5], [6, 7]],
                    ins=[X1own_t[tcx].ap().opt()], outs=[X1all_t[tcx].ap().opt()]),
                      reads=["X1own%d" % tcx], writes=["X1all%d" % tcx], cc=True)
        P.flush()


_NC_CACHE = {}


def _get_nc(n_layers):
    if n_layers not in _NC_CACHE:
        _NC_CACHE[n_layers] = build_program(n_layers)
    return _NC_CACHE[n_layers]


def _own_tiles(x_b, p):
    return np.ascontiguousarray(x_b.reshape(16, 2, 128, D)[:, p].reshape(2048, D))


def _run(xs, weights, n_layers):
    in_maps = []
    for c in range(8):
        b, p = c // 2, c % 2
        pfv = np.zeros((128, 4), np.float32)
        pfv[:, 0] = p
        pfv[:, 1] = 1 - p
        pfv[:, 2] = 128 * p
        m = {"xa": np.ascontiguousarray(xs[b]), "xo": _own_tiles(xs[b], p), "pf": pfv}
        for k, v in weights.items():
            a = v[:n_layers]
            if k == "b_forget":
                a = a.reshape(n_layers, 8, 1)
            m[k] = np.ascontiguousarray(a)
        in_maps.append(m)
    nc = _get_nc(n_layers)
    res = run_bass_kernel_spmd(nc, in_maps, core_ids=list(range(8)))
    out = np.empty((NB, S, D), np.float32)
    for c in range(8):
        b, p = c // 2, c % 2
        out[b].reshape(16, 2, 128, D)[:, p] = res.results[c]["out"].reshape(16, 128, D)
    return out


def kernel(x, w_in, b_forget, w_branch, w_out, ln1_g, ln1_b, w_ff1, w_ff2, ln2_g, ln2_b):
    weights = dict(w_in=np.asarray(w_in, np.float32), b_forget=np.asarray(b_forget, np.float32),
                   w_branch=np.asarray(w_branch, np.float32), w_out=np.asarray(w_out, np.float32),
                   ln1_g=np.asarray(ln1_g, np.float32), ln1_b=np.asarray(ln1_b, np.float32),
                   w_ff1=np.asarray(w_ff1, np.float32), w_ff2=np.asarray(w_ff2, np.float32),
                   ln2_g=np.asarray(ln2_g, np.float32), ln2_b=np.asarray(ln2_b, np.float32))
    return _run(np.asarray(x, np.float32), weights, DEPTH)
```

```python
from contextlib import ExitStack

import numpy as np
import concourse.bass as bass
import concourse.mybir as mybir
from concourse.bass_utils import run_bass_kernel_spmd

F32 = mybir.dt.float32
BF16 = mybir.dt.bfloat16
AF = mybir.ActivationFunctionType
ALU = mybir.AluOpType

D = 1024
S = 4096
NB = 4
DEPTH = 2
DFF = 4096
NCOLS = 8792
ALPHA = (2.0 * DEPTH) ** 0.25
EPS = 1e-5
BIG = 30000.0
NIT = 24
OFF = dict(qa=0, ka=512, va=1024, qi=1536, ki=2560, wi=2624, qf=2640, kf=3152, vf=3664,
           fg=4176, qs=4184, ks=4696, vs=5208, g=5720)
ENGS = ("pe", "act", "dve", "pool", "sp")
ND = 40


class Op:
    __slots__ = ("eng", "fn", "deps", "signal", "sigval", "dsem", "dval", "dma")


class Prog:
    def __init__(self, nc, stack):
        self.nc = nc
        self.ops = {e: [] for e in ENGS}
        self.writers = {}
        self.readers = {}
        self.dma_ops = []
        self.sem = {e: stack.enter_context(nc.semaphore("s_" + e)) for e in ENGS}
        self.dsem = [stack.enter_context(nc.semaphore("d%d" % i)) for i in range(ND)]
        self.ccsem = stack.enter_context(nc.semaphore("ccsem"))
        self.cc_ops = []
        self.count = {e: 0 for e in ENGS}
        self.known = {e: {} for e in ENGS}
        self.last = {e: None for e in ENGS}
        self.nblk = 0

    def add(self, eng, fn, reads=(), writes=(), extra=(), dma=False, force_signal=False, cc=False):
        op = Op()
        op.eng, op.fn, op.dma = eng, fn, dma or cc
        op.signal = force_signal
        op.sigval = op.dsem = op.dval = None
        deps = set(extra)
        for k in reads:
            deps.update(self.writers.get(k, {}).values())
        for k in writes:
            deps.update(self.writers.get(k, {}).values())
            deps.update(self.readers.get(k, {}).values())
        if cc:
            self.cc_ops.append(op)
            op.dsem = "cc"
            op.dval = len(self.cc_ops)
        if dma:
            n = len(self.dma_ops)
            op.dsem = n % ND
            op.dval = 16 * (n // ND + 1)
            if n >= ND:
                deps.add(self.dma_ops[n - ND])
            self.dma_ops.append(op)
        deps.discard(op)
        if eng == "pe":
            deps = {d for d in deps if d.eng != "pe"}
        op.deps = deps
        for d in deps:
            d.signal = True
        for k in writes:
            self.writers[k] = {eng: op}
            self.readers[k] = {}
        for k in reads:
            self.readers.setdefault(k, {})[eng] = op
        self.ops[eng].append(op)
        self.last[eng] = op
        return op

    def barrier(self):
        lasts = [self.last[e] for e in ENGS if e != "sp" and self.last[e] is not None]
        lasts += self.dma_ops[-ND:]
        lasts += self.cc_ops[-4:]
        for e in ENGS:
            self.add(e, lambda g: g.nop(), extra=lasts, force_signal=True)
        self.writers = {}
        self.readers = {}

    def flush(self):
        self.barrier()
        nc = self.nc
        engobj = {"pe": "tensor", "act": "scalar", "dve": "vector", "pool": "gpsimd", "sp": "sync"}

        def emit(e, g):
            known = self.known[e]
            for op in self.ops[e]:
                need = {}
                for d in op.deps:
                    if d.dma:
                        key, val = ("d", d.dsem), d.dval
                    else:
                        key, val = d.eng, d.sigval
                    if need.get(key, 0) < val:
                        need[key] = val
                for key, val in need.items():
                    if known.get(key, 0) < val:
                        if isinstance(key, tuple):
                            s = self.ccsem if key[1] == "cc" else self.dsem[key[1]]
                        else:
                            s = self.sem[key]
                        g.wait_ge(s, val)
                        known[key] = val
                ins = op.fn(g)
                if op.dsem == "cc":
                    ins.then_inc(self.ccsem, 1)
                elif op.dma:
                    ins.then_inc(self.dsem[op.dsem], 16)
                elif op.signal:
                    ins.then_inc(self.sem[e], 1)
            self.ops[e] = []

        for e in ENGS:
            for op in self.ops[e]:
                if op.signal and not op.dma:
                    self.count[e] += 1
                    op.sigval = self.count[e]
        with nc.Block() as block:
            @block.tensor
            def _(g):
                emit("pe", g)

            @block.scalar
            def _(g):
                emit("act", g)

            @block.vector
            def _(g):
                emit("dve", g)

            @block.gpsimd
            def _(g):
                emit("pool", g)

            @block.sync
            def _(g):
                emit("sp", g)

    def finish(self):
        lasts = self.dma_ops[-ND:]
        self.add("sp", lambda g: g.nop(), extra=lasts)
        self.flush()


class Rot:
    def __init__(self, tiles):
        self.tiles = tiles
        self.i = 0

    def next(self):
        t = self.tiles[self.i % len(self.tiles)]
        self.i += 1
        return t


def build_program(n_layers=1, debug=False):
    nc = bass.Bass("TRN2", target_bir_lowering=False)
    stack = ExitStack()
    with stack:
        _build(nc, stack, n_layers, debug)
    return nc


def _build(nc, stack, n_layers, debug):
    L = n_layers

    def din(name, shape, dt=F32):
        return nc.dram_tensor(name, shape, dt, kind="ExternalInput").ap()

    def dscr(name, shape, dt):
        kind = "ExternalOutput" if debug else "Internal"
        return nc.dram_tensor(name, shape, dt, kind=kind).ap()

    xa_d = din("xa", [S, D])
    X1own_t = [nc.dram_tensor("X1own%d" % i, [512, D], F32) for i in range(4)]
    X1all_t = [nc.dram_tensor("X1all%d" % i, [1024, D], F32) for i in range(4)]
    xo_d = din("xo", [2048, D])
    pf_d = din("pf", [128, 4])
    w_in_d = din("w_in", [L, D, NCOLS])
    bfg_d = din("b_forget", [L, 8, 1])
    wbr_d = din("w_branch", [L, 3, 512, D])
    wout_d = din("w_out", [L, D, D])
    ln1g_d = din("ln1_g", [L, D])
    ln1b_d = din("ln1_b", [L, D])
    wff1_d = din("w_ff1", [L, D, DFF])
    wff2_d = din("w_ff2", [L, DFF, D])
    ln2g_d = din("ln2_g", [L, D])
    ln2b_d = din("ln2_b", [L, D])
    out_d = nc.dram_tensor("out", [2048, D], F32, kind="ExternalOutput").ap()

    QT_d = dscr("QT", [12, 128, 2048], BF16)
    KT_d = dscr("KT", [12, 128, S], BF16)
    VV_d = dscr("VV", [3, S, 512], BF16)
    QI_d = dscr("QI", [8, 128, 2048], BF16)
    KI_d = dscr("KI", [128, S], BF16)
    G_d = dscr("G", [24, 128, 2048], F32)
    NBA_d = dscr("NBA", [16, 128, S], BF16)
    OT_d = dscr("OTd", [3, 4, 128, 2048], BF16)

    P = Prog(nc, stack)

    uniq = [0]

    def sb(st, name, shape, dt):
        uniq[0] += 1
        return st.enter_context(nc.sbuf_tensor("t%d_%s" % (uniq[0], name), shape, dt))

    def ps(st, name, shape, dt=F32):
        uniq[0] += 1
        return st.enter_context(nc.psum_tensor("t%d_%s" % (uniq[0], name), shape, dt))

    ident_bf = sb(stack, "ident_bf", [128, 128], BF16)
    ident_f = sb(stack, "ident_f", [128, 128], F32)
    ones_bf = sb(stack, "ones_bf", [128, 128], BF16)
    ONESF = sb(stack, "ONESF", [128, 64], F32)
    pf = sb(stack, "pf", [128, 4], F32)
    CM = sb(stack, "CM", [128, 8, 512], BF16)
    NBQK = sb(stack, "NBQK", [128, 256], F32)
    M1 = sb(stack, "M1", [128, 256], F32)
    AB = sb(stack, "AB", [128, 32, 8], F32)
    WI = sb(stack, "WI", [128, 16, 16], F32)
    KMS = sb(stack, "KMS", [128, 16], F32)
    CNtok = sb(stack, "CNtok", [128, 32, 8], F32)
    CNown = sb(stack, "CNown", [8, 2048], F32)
    PS = [ps(stack, "ps%d" % i, [128, 512]) for i in range(8)]

    with ExitStack() as st:
        onesf = sb(st, "onesf", [128, 128], F32)
        V0 = sb(st, "V0", [128, 8, 512], F32)
        V2 = sb(st, "V2", [128, 256], F32)
        KP = sb(st, "KP", [128, 32], F32)
        P.add("sp", lambda g: g.dma_start(out=pf[:], in_=pf_d[:, :]), writes=["pf"], dma=True)
        P.add("pool", lambda g: g.memset(onesf[:], 1.0), writes=["onesf"])
        P.add("pool", lambda g: g.memset(ones_bf[:], 1.0), writes=["ones_bf"])
        P.add("pool", lambda g: g.memset(ONESF[:], 1.0), writes=["ONESF"])
        P.add("pool", lambda g: g.affine_select(out=ident_f[:], in_=onesf[:], pattern=[[-1, 128]],
                                                compare_op=ALU.is_equal, fill=0.0, base=0,
                                                channel_multiplier=1),
              reads=["onesf"], writes=["ident_f"])
        P.add("pool", lambda g: g.tensor_copy(out=ident_bf[:], in_=ident_f[:]),
              reads=["ident_f"], writes=["ident_bf"])
        P.add("pool", lambda g: g.iota(V0[:].rearrange("p j (t q) -> p j t q", q=128),
                                       pattern=[[-128, 8], [256, 4], [1, 128]], base=0,
                                       channel_multiplier=-1, allow_small_or_imprecise_dtypes=True),
              writes=["V0"])
        P.add("dve", lambda g: g.tensor_scalar(out=V0[:], in0=V0[:], scalar1=pf[:, 2:3], scalar2=None,
                                               op0=ALU.add), reads=["V0", "pf"], writes=["V0"])
        P.add("dve", lambda g: g.tensor_scalar(out=CM[:], in0=V0[:], scalar1=0.0, scalar2=-BIG,
                                               op0=ALU.is_lt, op1=ALU.mult), reads=["V0"], writes=["CM"])
        P.add("pool", lambda g: g.iota(V2[:].rearrange("p (j k) -> p j k", k=128),
                                       pattern=[[-128, 2], [-1, 128]], base=0,
                                       channel_multiplier=1, allow_small_or_imprecise_dtypes=True),
              writes=["V2"])
        P.add("dve", lambda g: g.tensor_scalar(out=V2[:], in0=V2[:], scalar1=pf[:, 2:3], scalar2=None,
                                               op0=ALU.add), reads=["V2", "pf"], writes=["V2"])
        P.add("dve", lambda g: g.tensor_scalar(out=NBQK[:], in0=V2[:], scalar1=0.0, scalar2=-BIG,
                                               op0=ALU.is_lt, op1=ALU.mult), reads=["V2"], writes=["NBQK"])
        P.add("dve", lambda g: g.tensor_scalar(out=M1[:], in0=V2[:], scalar1=0.0, scalar2=None,
                                               op0=ALU.is_le), reads=["V2"], writes=["M1"])
        P.add("pool", lambda g: g.iota(KP[:], pattern=[[128, 32]], base=0, channel_multiplier=1,
                                       allow_small_or_imprecise_dtypes=True), writes=["KP"])
        for h in range(8):
            sl = 2.0 ** (-(h + 1))
            P.add("dve", lambda g, h=h, sl=sl: g.tensor_scalar(out=AB[:, :, h], in0=KP[:], scalar1=sl,
                                                               scalar2=None, op0=ALU.mult),
                  reads=["KP"], writes=["AB"])
        P.flush()

    for layer in range(L):
        _layer(nc, P, stack, layer, locals(), layer == L - 1)
    P.finish()


def _layer(nc, P, stack, layer, E, is_last):
    xa_d, X1own_t, X1all_t = E["xa_d"], E["X1own_t"], E["X1all_t"]
    xo_d, w_in_d, bfg_d, wbr_d, wout_d = E["xo_d"], E["w_in_d"], E["bfg_d"], E["wbr_d"], E["wout_d"]
    ln1g_d, ln1b_d, wff1_d, wff2_d, ln2g_d, ln2b_d, out_d = (E["ln1g_d"], E["ln1b_d"], E["wff1_d"], E["wff2_d"],
                                                            E["ln2g_d"], E["ln2b_d"], E["out_d"])
    QT_d, KT_d, VV_d, QI_d, KI_d, G_d, NBA_d = E["QT_d"], E["KT_d"], E["VV_d"], E["QI_d"], E["KI_d"], E["G_d"], E["NBA_d"]
    ident_bf, ident_f, ones_bf, pf, CM, NBQK, M1, AB = (E["ident_bf"], E["ident_f"], E["ones_bf"], E["pf"], E["CM"],
                                                        E["NBQK"], E["M1"], E["AB"])
    ONESF = E["ONESF"]
    OT_d, WI, KMS, CNtok, CNown, PS = E["OT_d"], E["WI"], E["KMS"], E["CNtok"], E["CNown"], E["PS"]
    sb, ps = E["sb"], E["ps"]
    w_in = w_in_d[layer].rearrange("(c p) n -> p c n", p=128)

    psr = Rot(list(range(8)))

    def dma(out, in_, reads, writes, eng="sp"):
        return P.add(eng, lambda g: g.dma_start(out=out, in_=in_), reads=reads, writes=writes, dma=True)

    def mm(out, lhsT, rhs, start, stop, reads, writes):
        return P.add("pe", lambda g: g.matmul(out, lhsT, rhs, start=start, stop=stop), reads=reads, writes=writes)

    st_fg = ExitStack()
    FG = sb(st_fg, "FG", [8, S], F32)
    with ExitStack() as st:
        XT = sb(st, "XT", [128, 8, S], BF16)
        XTO = sb(st, "XTO", [128, 8, 2048], BF16)
        STG = [sb(st, "stg%d" % i, [128, 8, 512], F32) for i in range(2)]
        WB = [sb(st, "wb%d" % i, [128, 8, 512], BF16) for i in range(3)]
        OUTB = [sb(st, "outb%d" % i, [128, 512], BF16) for i in range(4)]
        OUTF = [sb(st, "outf%d" % i, [128, 512], F32) for i in range(3)]
        TMPX = sb(st, "tmpx", [128, 2048], BF16)
        stg_r, wb_r, outb_r, outf_r = Rot([0, 1]), Rot([0, 1, 2]), Rot([0, 1, 2, 3]), Rot([0, 1, 2])
        evac_flip = [0]

        def evac_copy(out, in_, reads, writes):
            evac_flip[0] ^= 1
            if evac_flip[0]:
                P.add("act", lambda g: g.activation(out=out, in_=in_, func=AF.Copy), reads=reads, writes=writes)
                return "act"
            P.add("dve", lambda g: g.tensor_copy(out=out, in_=in_), reads=reads, writes=writes)
            return "act"

        for tc in range(8):
            i = stg_r.next()
            sv = STG[i][:].rearrange("p a b -> p (a b)").rearrange("p (t d) -> p t d", d=D)
            if layer == 0:
                dma(sv, xa_d[tc * 512:(tc + 1) * 512, :].rearrange("(t p) d -> p t d", p=128), [], ["stg%d" % i])
            else:
                for ii in range(2):
                    src = X1all_t[tc // 2].ap().rearrange("(r t p) d -> p t r d", r=2, p=128)[:, 2 * (tc % 2) + ii, :, :]
                    dma(sv[:, 2 * ii:2 * ii + 2, :], src, ["X1all%d" % (tc // 2)], ["stg%d" % i])
            for t in range(4):
                g_ = tc * 4 + t
                for c4 in range(2):
                    b = psr.next()
                    for cc in range(4):
                        c = c4 * 4 + cc
                        P.add("pe", lambda g, b=b, cc=cc, sv=sv, t=t, c=c: g.transpose(
                            PS[b][:, cc * 128:(cc + 1) * 128], sv[:, t, c * 128:(c + 1) * 128], ident_f[:]),
                              reads=["stg%d" % i], writes=["ps%d" % b])
                    evac_copy(XT[:, c4 * 4:c4 * 4 + 4, g_ * 128:(g_ + 1) * 128],
                              PS[b][:].rearrange("p (c q) -> p c q", q=128), ["ps%d" % b], [("XT", tc)])
        for c in range(8):
            v = XT[:, c, :].rearrange("p (i two q) -> p i two q", two=2, q=128)
            P.add("dve", lambda g, v=v: g.tensor_scalar(out=TMPX[:].rearrange("p (i q) -> p i q", q=128),
                                                        in0=v[:, :, 1, :], scalar1=pf[:, 0:1], scalar2=None,
                                                        op0=ALU.mult),
                  reads=[("XT", t) for t in range(8)], writes=["tmpx"])
            P.add("dve", lambda g, v=v, c=c: g.scalar_tensor_tensor(
                out=XTO[:, c, :].rearrange("p (i q) -> p i q", q=128), in0=v[:, :, 0, :], scalar=pf[:, 1:2],
                in1=TMPX[:].rearrange("p (i q) -> p i q", q=128), op0=ALU.mult, op1=ALU.add),
                  reads=[("XT", t) for t in range(8)] + ["tmpx"], writes=["XTO"])
        XTkeys = [("XT", t) for t in range(8)]

        def load_w(col0, n, dup=False):
            i = stg_r.next()
            j = wb_r.next()
            dma(STG[i][:, :, :n], w_in[:, :, col0:col0 + n], [], ["stg%d" % i])
            P.add("pool", lambda g: g.tensor_copy(out=WB[j][:, :, :n], in_=STG[i][:, :, :n]),
                  reads=["stg%d" % i], writes=["wb%d" % j])
            if dup:
                P.add("pool", lambda g: g.tensor_copy(out=WB[j][:, :, n:2 * n], in_=STG[i][:, :, :n]),
                      reads=["stg%d" % i], writes=["wb%d" % j])
            return j

        def proj_fm(j, ncols, own, dst_fn, gates=False, M=128):
            src = XTO if own else XT
            ntc = 4 if own else 8
            for gi in range(max(1, ncols // 128)):
                for tc in range(ntc):
                    b = psr.next()
                    for kc in range(8):
                        mm(PS[b][:M, :], WB[j][:, kc, gi * 128:gi * 128 + M], src[:, kc, tc * 512:(tc + 1) * 512],
                           kc == 0, kc == 7, ["wb%d" % j, "XTO"] + XTkeys, ["ps%d" % b])
                    if gates:
                        o = outf_r.next()
                        P.add("act", lambda g, b=b, o=o: g.activation(out=OUTF[o][:], in_=PS[b][:], func=AF.Sigmoid),
                              reads=["ps%d" % b], writes=["outf%d" % o])
                        dma(dst_fn(gi, tc), OUTF[o][:], ["outf%d" % o], [], eng="act")
                    else:
                        o = outb_r.next()
                        qe = evac_copy(OUTB[o][:M, :], PS[b][:M, :], ["ps%d" % b], ["outb%d" % o])
                        dma(dst_fn(gi, tc), OUTB[o][:M, :], ["outb%d" % o], [], eng=qe)

        def proj_tm(j, dst):
            for t in range(32):
                b = psr.next()
                for kc in range(8):
                    mm(PS[b][:], XT[:, kc, t * 128:(t + 1) * 128], WB[j][:, kc, :], kc == 0, kc == 7,
                       ["wb%d" % j] + XTkeys, ["ps%d" % b])
                o = outb_r.next()
                qe = evac_copy(OUTB[o][:], PS[b][:], ["ps%d" % b], ["outb%d" % o])
                dma(dst[t * 128:(t + 1) * 128, :], OUTB[o][:], ["outb%d" % o], [], eng=qe)

        for br, (qn, kn, vn) in enumerate((("qa", "ka", "va"), ("qf", "kf", "vf"), ("qs", "ks", "vs"))):
            j = load_w(OFF[qn], 512)
            proj_fm(j, 512, True, lambda gi, tc, br=br: QT_d[br * 4 + gi][:, tc * 512:(tc + 1) * 512])
            j = load_w(OFF[kn], 512)
            proj_fm(j, 512, False, lambda gi, tc, br=br: KT_d[br * 4 + gi][:, tc * 512:(tc + 1) * 512])
            j = load_w(OFF[vn], 512)
            proj_tm(j, VV_d[br])
        for half in range(2):
            j = load_w(OFF["qi"] + 512 * half, 512)
            proj_fm(j, 512, True, lambda gi, tc, half=half: QI_d[half * 4 + gi][:, tc * 512:(tc + 1) * 512])
        j = load_w(OFF["ki"], 64, dup=True)
        proj_fm(j, 128, False, lambda gi, tc: KI_d[:, tc * 512:(tc + 1) * 512])
        j = load_w(OFF["wi"], 16)
        for t in range(16):
            b = psr.next()
            for kc in range(8):
                mm(PS[b][:, :16], XTO[:, kc, t * 128:(t + 1) * 128], WB[j][:, kc, :16], kc == 0, kc == 7,
                   ["wb%d" % j, "XTO"], ["ps%d" % b])
            P.add("dve", lambda g, b=b, t=t: g.tensor_copy(out=WI[:, t, :], in_=PS[b][:, :16]),
                  reads=["ps%d" % b], writes=["WI"])
        j = load_w(OFF["fg"], 8)
        for tc in range(8):
            b = psr.next()
            for kc in range(8):
                mm(PS[b][:8, :], WB[j][:, kc, :8], XT[:, kc, tc * 512:(tc + 1) * 512], kc == 0, kc == 7,
                   ["wb%d" % j] + XTkeys, ["ps%d" % b])
            P.add("dve", lambda g, b=b, tc=tc: g.tensor_copy(out=FG[:, tc * 512:(tc + 1) * 512], in_=PS[b][:8, :]),
                  reads=["ps%d" % b], writes=["FG"])
        for gt in range(6):
            j = load_w(OFF["g"] + 512 * gt, 512)
            proj_fm(j, 512, True, lambda gi, tc, gt=gt: G_d[gt * 4 + gi][:, tc * 512:(tc + 1) * 512], gates=True)
        P.flush()
    with ExitStack() as st:
        LL = FG
        CN = sb(st, "CN", [8, S], F32)
        TMPC = sb(st, "tmpc", [8, 2048], F32)
        negb = sb(st, "negb", [8, 1], F32)
        dma(negb[:], bfg_d[layer], [], ["negb"])
        P.add("act", lambda g: g.activation(out=negb[:], in_=negb[:], func=AF.Copy, scale=-1.0),
              reads=["negb"], writes=["negb"])
        P.add("act", lambda g: g.activation(out=LL[:], in_=FG[:], func=AF.Exp, bias=negb[:, 0:1], scale=-1.0),
              reads=["FG", "negb"], writes=["FG"])
        P.add("act", lambda g: g.activation(out=LL[:], in_=LL[:], func=AF.Ln, bias=1.0, scale=1.0),
              reads=["FG"], writes=["FG"])
        P.add("dve", lambda g: g.tensor_tensor_scan(out=CN[:], data0=LL[:], data1=LL[:], initial=0.0,
                                                    op0=ALU.add, op1=ALU.bypass), reads=["FG"], writes=["CN"])
        b = psr.next()
        for t in range(32):
            P.add("pe", lambda g, t=t, b=b: g.transpose(PS[b][:, t * 8:(t + 1) * 8], CN[:, t * 128:(t + 1) * 128],
                                                        ident_f[:8, :8]),
                  reads=["CN"], writes=["ps%d" % b])
        P.add("dve", lambda g, b=b: g.tensor_copy(out=CNtok[:].rearrange("p t h -> p (t h)"), in_=PS[b][:, :256]),
              reads=["ps%d" % b], writes=["CNtok"])
        cv = CN[:].rearrange("p (i two q) -> p i two q", two=2, q=128)
        P.add("dve", lambda g: g.tensor_scalar(out=TMPC[:].rearrange("p (i q) -> p i q", q=128), in0=cv[:, :, 1, :],
                                               scalar1=pf[:8, 0:1], scalar2=None, op0=ALU.mult),
              reads=["CN"], writes=["tmpc"])
        P.add("dve", lambda g: g.scalar_tensor_tensor(out=CNown[:].rearrange("p (i q) -> p i q", q=128),
                                                      in0=cv[:, :, 0, :], scalar=pf[:8, 1:2],
                                                      in1=TMPC[:].rearrange("p (i q) -> p i q", q=128),
                                                      op0=ALU.mult, op1=ALU.add),
              reads=["CN", "tmpc"], writes=["CNown"])
        P.flush()
    st_fg.close()

    with ExitStack() as st:
        QI = sb(st, "QIs", [128, 8, 2048], BF16)
        KI = sb(st, "KIs", [128, S], BF16)
        SC = [sb(st, "SC%d" % i, [128, S], F32) for i in range(2)]
        TH = [sb(st, "TH%d" % i, [128, 512], F32) for i in range(3)]
        NBo = [sb(st, "NBo%d" % i, [128, S], BF16) for i in range(2)]
        JUNK = sb(st, "JUNK", [128, S], BF16)
        KPOS = sb(st, "KPOS", [128, S], F32)
        KSEL = sb(st, "KSEL", [128, S], F32)
        WABS = sb(st, "WABS", [128, 16, 16], F32)
        WSGN = sb(st, "WSGN", [128, 16, 16], F32)
        SM = sb(st, "SM", [128, 8], F32)
        for pr in range(8):
            dma(QI[:, pr, :], QI_d[pr], [], [("QI", pr)])
        dma(KI[:], KI_d[:, :], [], ["KI"])
        P.add("pool", lambda g: g.iota(KPOS[:], pattern=[[1, S]], base=0, channel_multiplier=0,
                                       allow_small_or_imprecise_dtypes=True), writes=["KPOS"])
        P.add("act", lambda g: g.activation(out=WABS[:], in_=WI[:], func=AF.Abs), reads=["WI"], writes=["WABS"])
        P.add("dve", lambda g: g.tensor_scalar(out=WSGN[:], in0=WI[:], scalar1=0.0, scalar2=2.0, op0=ALU.is_ge,
                                               op1=ALU.mult), reads=["WI"], writes=["WSGN"])
        P.add("dve", lambda g: g.tensor_scalar(out=WSGN[:], in0=WSGN[:], scalar1=-1.0, scalar2=None, op0=ALU.add),
              reads=["WSGN"], writes=["WSGN"])
        th_r = Rot([0, 1, 2])
        NSC = 3
        SC.append(sb(st, "SC2", [128, S], F32))
        JUNKA = sb(st, "JUNKA", [128, S], BF16)
        SMA = sb(st, "SMA", [128, 8], F32)

        def acc(i):
            nk = 128 * (2 * i + 2)
            sc = SC[i % NSC]
            sck = "SC%d" % (i % NSC)
            for kc in range((nk + 511) // 512):
                k0 = kc * 512
                kn = min(512, nk - k0)
                for h in range(16):
                    r0 = 64 * (h % 2)
                    b = psr.next()
                    mm(PS[b][:, :kn], QI[r0:r0 + 64, h // 2, i * 128:(i + 1) * 128], KI[r0:r0 + 64, k0:k0 + kn],
                       True, True, [("QI", h // 2), "KI"], ["ps%d" % b])
                    t = th_r.next()
                    P.add("act", lambda g, b=b, t=t, kn=kn, i=i, h=h: g.activation(
                        out=TH[t][:, :kn], in_=PS[b][:, :kn], func=AF.Relu, scale=WABS[:, i, h:h + 1]),
                          reads=["ps%d" % b, "WABS"], writes=["TH%d" % t])
                    if h == 0:
                        P.add("dve", lambda g, t=t, kn=kn, k0=k0, i=i, h=h, sc=sc: g.tensor_scalar(
                            out=sc[:, k0:k0 + kn], in0=TH[t][:, :kn], scalar1=WSGN[:, i, h:h + 1], scalar2=None,
                            op0=ALU.mult), reads=["TH%d" % t, "WSGN"], writes=[sck])
                    else:
                        P.add("dve", lambda g, t=t, kn=kn, k0=k0, i=i, h=h, sc=sc: g.scalar_tensor_tensor(
                            out=sc[:, k0:k0 + kn], in0=TH[t][:, :kn], scalar=WSGN[:, i, h:h + 1],
                            in1=sc[:, k0:k0 + kn], op0=ALU.mult, op1=ALU.add),
                              reads=["TH%d" % t, "WSGN", sck], writes=[sck])
            P.add("dve", lambda g, sc=sc, i=i: g.tensor_tensor(out=sc[:, 256 * i:256 * i + 256],
                                                              in0=sc[:, 256 * i:256 * i + 256], in1=NBQK[:],
                                                              op=ALU.add), reads=[sck], writes=[sck])

        def ctx(i):
            nk = 128 * (2 * i + 2)
            sc = SC[i % NSC]
            sck = "SC%d" % (i % NSC)
            use_act = (i % 2 == 1)
            smk = "SMA" if use_act else "SM"
            smt = SMA if use_act else SM
            return (nk, sc, sck, use_act, smk) + tuple(smt[:, c:c + 1] for c in range(6))

        def pre(i):
            nk, sc, sck, use_act, smk, lo, w0, mid, cnt, step, hi = ctx(i)
            if i == 0:
                P.add("dve", lambda g, lo=lo: g.memset(lo, -BIG / 2), writes=[smk])
            else:
                P.add("dve", lambda g, sc=sc, i=i, lo=lo: g.tensor_reduce(out=lo, in_=sc[:, :256 * i],
                                                                          axis=mybir.AxisListType.X, op=ALU.min),
                      reads=[sck], writes=[smk])
                P.add("dve", lambda g, sc=sc, nk=nk, hi=hi: g.tensor_reduce(out=hi, in_=sc[:, :nk],
                                                                            axis=mybir.AxisListType.X, op=ALU.max),
                      reads=[sck], writes=[smk])
                P.add("dve", lambda g, w0=w0, hi=hi, lo=lo: g.tensor_tensor(out=w0, in0=hi, in1=lo, op=ALU.subtract),
                      reads=[smk], writes=[smk])
                if use_act:
                    P.add("dve", lambda g, nlo=lo: g.tensor_scalar(out=nlo, in0=nlo, scalar1=-1.0, scalar2=None,
                                                                   op0=ALU.mult), reads=[smk], writes=[smk])

        def chain(i):
            nk, sc, sck, use_act, smk, lo, w0, mid, cnt, step, hi = ctx(i)
            if i > 0:
                if not use_act:
                    for it in range(NIT):
                        f = 2.0 ** (-(it + 1))
                        P.add("dve", lambda g, f=f, mid=mid, w0=w0, lo=lo: g.scalar_tensor_tensor(
                            out=mid, in0=w0, scalar=f, in1=lo, op0=ALU.mult, op1=ALU.add), reads=[smk], writes=[smk])
                        P.add("dve", lambda g, sc=sc, nk=nk, mid=mid, cnt=cnt: g.tensor_scalar(
                            out=JUNK[:, :nk], in0=sc[:, :nk], scalar1=mid, scalar2=None, op0=ALU.is_ge, op1=ALU.add,
                            accum_out=cnt), reads=[sck, smk], writes=["JUNK", smk])
                        P.add("dve", lambda g, f=f, step=step, cnt=cnt: g.tensor_scalar(
                            out=step, in0=cnt, scalar1=255.5, scalar2=f, op0=ALU.is_ge, op1=ALU.mult),
                              reads=[smk], writes=[smk])
                        P.add("dve", lambda g, lo=lo, w0=w0, step=step: g.scalar_tensor_tensor(
                            out=lo, in0=w0, scalar=step, in1=lo, op0=ALU.mult, op1=ALU.add),
                              reads=[smk], writes=[smk])
                else:
                    nlo, nmid, ssum, sg, tt = lo, mid, cnt, step, hi
                    for it in range(NIT):
                        f = 2.0 ** (-(it + 1))
                        P.add("act", lambda g, f=f, nmid=nmid, w0=w0, nlo=nlo: g.activation(
                            out=nmid, in_=w0, func=AF.Identity, scale=-f, bias=nlo), reads=[smk], writes=[smk])
                        P.add("act", lambda g, sc=sc, nk=nk, nmid=nmid, ssum=ssum: g.activation(
                            out=JUNKA[:, :nk], in_=sc[:, :nk], func=AF.Sign, scale=1.0, bias=nmid, accum_out=ssum),
                              reads=[sck, smk], writes=["JUNKA", smk])
                        P.add("act", lambda g, sg=sg, ssum=ssum, nk=nk: g.activation(
                            out=sg, in_=ssum, func=AF.Sign, scale=1.0, bias=SGB[:, (nk // 256) - 1:(nk // 256)]),
                              reads=[smk, "SGB"], writes=[smk])
                        P.add("act", lambda g, f=f, tt=tt, sg=sg, it=it: g.activation(
                            out=tt, in_=sg, func=AF.Identity, scale=-f / 2, bias=FB[:, it:it + 1]),
                              reads=[smk, "FB"], writes=[smk])
                        P.add("act", lambda g, nlo=nlo, w0=w0, tt=tt: g.activation(
                            out=nlo, in_=w0, func=AF.Identity, scale=tt, bias=nlo), reads=[smk], writes=[smk])

        def post(i):
            nk, sc, sck, use_act, smk, lo, w0, mid, cnt, step, hi = ctx(i)
            if use_act and i > 0:
                P.add("dve", lambda g, nlo=lo: g.tensor_scalar(out=nlo, in0=nlo, scalar1=-1.0, scalar2=None,
                                                               op0=ALU.mult), reads=[smk], writes=[smk])
            nbo = NBo[i % 2]
            nbk = "NBo%d" % (i % 2)
            P.add("dve", lambda g, nbo=nbo, sc=sc, nk=nk, lo=lo: g.tensor_scalar(
                out=nbo[:, :nk], in0=sc[:, :nk], scalar1=lo, scalar2=-BIG, op0=ALU.is_lt, op1=ALU.mult),
                  reads=[sck, smk], writes=[nbk])
            P.add("pool", lambda g, nbo=nbo, nk=nk: g.tensor_tensor(out=KSEL[:, :nk], in0=nbo[:, :nk], in1=KPOS[:, :nk],
                                                                   op=ALU.add), reads=[nbk, "KPOS"], writes=["KSEL"])
            P.add("dve", lambda g, nk=nk, i=i: g.tensor_reduce(out=KMS[:, i:i + 1], in_=KSEL[:, :nk],
                                                               axis=mybir.AxisListType.X, op=ALU.max),
                  reads=["KSEL"], writes=["KMS"])
            dma(NBA_d[i][:, :nk], nbo[:, :nk], [nbk], [])

        SGB = sb(st, "SGB", [128, 16], F32)
        FB = sb(st, "FB", [128, NIT], F32)
        for j in range(16):
            P.add("pool", lambda g, j=j: g.memset(SGB[:, j:j + 1], 256.0 * (j + 1) - 511.5), writes=["SGB"])
        for it in range(NIT):
            P.add("pool", lambda g, it=it: g.memset(FB[:, it:it + 1], -(2.0 ** (-(it + 2)))), writes=["FB"])
        for pp in range(8):
            acc(2 * pp)
            acc(2 * pp + 1)
            pre(2 * pp)
            pre(2 * pp + 1)
            chain(2 * pp)
            chain(2 * pp + 1)
            post(2 * pp)
            post(2 * pp + 1)
        P.flush()

    for br in range(2):
        with ExitStack() as st:
            KT = [sb(st, "KTa%d" % i, [128, S], BF16) for i in range(2)]
            VT = [sb(st, "VTa%d" % i, [128, 32, 128], BF16) for i in range(2)]
            QT = [sb(st, "QTa%d" % i, [128, 2048], BF16) for i in range(2)]
            SROW = [sb(st, "SROW%d" % i, [128, 512], F32) for i in range(2)]
            PT = [sb(st, "PT%d" % i, [128, 512], BF16) for i in range(4)]
            RS = [sb(st, "RS%d" % i, [128, 512], F32) for i in range(2)]
            OTS = [sb(st, "OTS%d" % i, [128, 512], BF16) for i in range(2)]
            NB4 = [sb(st, "NB4%d" % i, [128, 4, S], BF16) for i in range(2)] if br == 0 else None
            srow_r, ots_r = Rot([0, 1]), Rot([0, 1])
            pt_r, rs_r, nb_r = Rot([0, 1, 2, 3]), Rot([0, 1]), Rot([0, 1])
            sps = Rot([0, 1, 2])
            ops = Rot([3, 4, 5])
            BK = AB if br == 0 else CNtok
            nb_map = {}

            def nb_load(idx):
                qc_ = idx % 4
                n4_ = nb_r.next()
                nb_map[idx] = n4_
                for t in range(4):
                    nk = 128 * (2 * (4 * qc_ + t) + 2)
                    dma(NB4[n4_][:, t, :nk], NBA_d[4 * qc_ + t][:, :nk], [], [("NB4", n4_, t)], eng="act")

            for i in range(2):
                P.add("pool", lambda g, i=i: g.memset(KT[i][64:128, :], 0.0), writes=["KTa%d" % i])
                P.add("pool", lambda g, i=i: g.memset(KT[i][64:65, :], 1.0), writes=["KTa%d" % i])
                P.add("pool", lambda g, i=i: g.memset(QT[i][64:128, :], 0.0), writes=["QTa%d" % i])
                P.add("pool", lambda g, i=i: g.memset(VT[i][:, :, 64:128], 0.0), writes=["VTa%d" % i])
                P.add("pool", lambda g, i=i: g.memset(VT[i][:, :, 64:65], 1.0), writes=["VTa%d" % i])
            def main_loop():
              for h in range(8):
                pr, hh = h // 2, h % 2
                r0 = 64 * hh
                kt, vt, qt = KT[h % 2], VT[h % 2], QT[h % 2]
                kk, vk, qk = "KTa%d" % (h % 2), "VTa%d" % (h % 2), "QTa%d" % (h % 2)
                dma(kt[0:64, :], KT_d[br * 4 + pr][r0:r0 + 64, :], [], [kk])
                dma(vt[:, :, 0:64], VV_d[br].rearrange("(t p) c -> p t c", p=128)[:, :, h * 64:(h + 1) * 64], [], [vk])
                dma(qt[0:64, :], QT_d[br * 4 + pr][r0:r0 + 64, :], [], [qk])
                for qc in range(4):
                    if br == 0:
                        sl = 2.0 ** (-(h + 1))
                        for t in range(4):
                            P.add("pe", lambda g, t=t, qc=qc: g.matmul(
                                PS[7][64:65, t * 128:(t + 1) * 128], KMS[:, 4 * qc + t:4 * qc + t + 1], ident_f[:, :],
                                start=True, stop=True), reads=["KMS"], writes=["ps7"])
                        P.add("act", lambda g, qc=qc, sl=sl, qt=qt: g.activation(
                            out=qt[64:65, qc * 512:(qc + 1) * 512], in_=PS[7][64:65, :], func=AF.Copy,
                            scale=-8.0 * sl), reads=["ps7"], writes=[qk])
                    else:
                        P.add("pe", lambda g, h=h, qc=qc: g.matmul(PS[7][64:65, :], ident_f[:8, h:h + 1],
                                                                    CNown[:, qc * 512:(qc + 1) * 512],
                                                                    start=True, stop=True),
                              reads=["CNown"], writes=["ps7"])
                        P.add("act", lambda g, qc=qc, qt=qt: g.activation(
                            out=qt[64:65, qc * 512:(qc + 1) * 512], in_=PS[7][64:65, :], func=AF.Copy, scale=-8.0),
                              reads=["ps7"], writes=[qk])
                for qc in range(4):
                    nkt = 8 * qc + 8
                    n4 = None
                    if br == 0:
                        if h == 0 and qc == 0:
                            nb_load(0)
                        n4 = nb_map[h * 4 + qc]
                        if h * 4 + qc + 1 < 32:
                            nb_load(h * 4 + qc + 1)
                    oa = ops.next()
                    ptmap = {}

                    def sgrp(kti, h=h, qc=qc, n4=n4, kt=kt, qt=qt, kk=kk, qk=qk):
                        s = sps.next()
                        j = kti - 8 * qc
                        extra = []
                        if j >= 0:
                            extra.append(("cm", j))
                        if br == 0:
                            for t in range(4):
                                if kti <= 2 * (4 * qc + t) + 1:
                                    extra.append(("nb", t))
                        mm(PS[s][:], kt[:, kti * 128:(kti + 1) * 128], qt[:, qc * 512:(qc + 1) * 512], True,
                           len(extra) == 0, [kk, qk], ["ps%d" % s])
                        for ei, ex in enumerate(extra):
                            lastf = ei == len(extra) - 1
                            if ex[0] == "cm":
                                mm(PS[s][:], ident_bf[:], CM[:, ex[1], :], False, lastf, ["CM"], ["ps%d" % s])
                            else:
                                t = ex[1]
                                mm(PS[s][:, t * 128:(t + 1) * 128], NB4[n4][:, t, kti * 128:(kti + 1) * 128],
                                   ident_bf[:], False, lastf, [("NB4", n4, t)], ["ps%d" % s])
                        pt = pt_r.next()
                        ptmap[kti] = pt
                        P.add("act", lambda g, s=s, pt=pt, kti=kti, h=h: g.activation(
                            out=PT[pt][:], in_=PS[s][:], func=AF.Exp, bias=BK[:, kti, h:h + 1], scale=0.125),
                              reads=["ps%d" % s, "CNtok"], writes=["PT%d" % pt])

                    def pvg(kti, vt=vt, vk=vk, oa=oa, nkt=nkt):
                        pt = ptmap[kti]
                        mm(PS[oa][:], vt[:, kti, :], PT[pt][:], kti == 0, kti == nkt - 1,
                           [vk, "PT%d" % pt], ["ps%d" % oa])

                    LA = 2
                    for kti in range(min(LA, nkt)):
                        sgrp(kti)
                    if pending:
                        pending.pop()()
                    for kti in range(nkt):
                        if kti + LA < nkt:
                            sgrp(kti + LA)
                        pvg(kti)
                    pending.append(lambda oa=oa, pr=pr, r0=r0, qc=qc: finalize(oa, pr, r0, qc))

            def finalize(oa, pr, r0, qc):
                if True:
                    sr = srow_r.next()
                    r = rs_r.next()
                    P.add("act", lambda g, sr=sr, oa=oa: g.activation(out=SROW[sr][64:65, :], in_=PS[oa][64:65, :],
                                                                      func=AF.Copy),
                          reads=["ps%d" % oa], writes=["SROW%d" % sr])
                    P.add("dve", lambda g, sr=sr: g.reciprocal(out=SROW[sr][64:65, :], in_=SROW[sr][64:65, :]),
                          reads=["SROW%d" % sr], writes=["SROW%d" % sr])
                    P.add("pe", lambda g, sr=sr: g.matmul(PS[6][:64, :], ONESF[64:65, :], SROW[sr][64:65, :],
                                                          start=True, stop=True),
                          reads=["SROW%d" % sr], writes=["ps6"])
                    P.add("act", lambda g, r=r: g.activation(out=RS[r][:64, :], in_=PS[6][:64, :], func=AF.Copy),
                          reads=["ps6"], writes=["RS%d" % r])
                    o = ots_r.next()
                    P.add("dve", lambda g, r=r, oa=oa, o=o: g.tensor_tensor(
                        out=OTS[o][:64, :], in0=PS[oa][:64, :], in1=RS[r][:64, :], op=ALU.mult),
                          reads=["ps%d" % oa, "RS%d" % r], writes=["OTS%d" % o])
                    dma(OT_d[br, pr][r0:r0 + 64, qc * 512:(qc + 1) * 512], OTS[o][:64, :],
                        ["OTS%d" % o], [], eng="act")

            pending = []
            main_loop()
            if pending:
                pending.pop()()
            P.flush()

    with ExitStack() as st:
        NSET = 4
        KT = [sb(st, "KTc%d" % i, [128, S], BF16) for i in range(1)] * 2
        VT = [sb(st, "VTc%d" % i, [128, 32, 128], BF16) for i in range(2)]
        QT = [sb(st, "QTc%d" % i, [128, 2048], BF16) for i in range(1)] * 2
        NSG = 3
        SG = [sb(st, "SG%d" % i, [128, S], F32) for i in range(NSG)]
        INC = [sb(st, "INC%d" % i, [128, S + 1], F32) for i in range(NSET)]
        AA = [sb(st, "AA%d" % i, [128, S], BF16) for i in range(NSET)]
        ATS = [sb(st, "ATS%d" % i, [128, 512], BF16) for i in range(3)]
        OTS = [sb(st, "OTSc%d" % i, [128, 128], BF16) for i in range(2)]
        ots_r = Rot([0, 1])
        zr, at_r, ats_r, oc_r = Rot([0, 1, 2]), Rot([3, 4]), Rot([0, 1, 2]), Rot([5, 6])
        items = [(pr, hh, i) for pr in range(4) for hh in range(2) for i in range(16)]

        def stage_a(n):
            pr, hh, i = items[n]
            kt, qt = KT[pr % 2], QT[pr % 2]
            kk, vk, qk = "KTc0", "VTc%d" % (pr % 2), "QTc0"
            if hh == 0 and i == 0:
                dma(kt[:], KT_d[8 + pr], [], [kk])
                dma(VT[pr % 2][:], VV_d[2].rearrange("(t p) c -> p t c", p=128)[:, :, pr * 128:(pr + 1) * 128], [], [vk])
                dma(qt[:], QT_d[8 + pr], [], [qk])
            r0 = 64 * hh
            nk = 128 * (2 * i + 2)
            sg, inc, aa = SG[n % NSG], INC[n % NSET], AA[n % NSET]
            sgk, inck, aak = "SG%d" % (n % NSG), "INC%d" % (n % NSET), "AA%d" % (n % NSET)
            for kc in range((nk + 511) // 512):
                k0 = kc * 512
                kn = min(512, nk - k0)
                b = zr.next()
                mm(PS[b][:, :kn], qt[r0:r0 + 64, i * 128:(i + 1) * 128], kt[r0:r0 + 64, k0:k0 + kn],
                   True, True, [kk, qk], ["ps%d" % b])
                P.add("act", lambda g, b=b, kn=kn, k0=k0, sg=sg: g.activation(
                    out=sg[:, k0:k0 + kn], in_=PS[b][:, :kn], func=AF.Sigmoid, scale=-0.125),
                      reads=["ps%d" % b], writes=[sgk])
            P.add("dve", lambda g, sg=sg, i=i: g.tensor_tensor(out=sg[:, 256 * i:256 * i + 256],
                                                                in0=sg[:, 256 * i:256 * i + 256], in1=M1[:],
                                                                op=ALU.max), reads=[sgk], writes=[sgk])
            P.add("pool", lambda g, inc=inc, nk=nk: g.memset(inc[:, nk:nk + 1], 1.0), writes=[inck])
            P.add("dve", lambda g, inc=inc, sg=sg, nk=nk: g.tensor_tensor_scan(
                out=inc[:, 0:nk][:, ::-1], data0=sg[:, 0:nk][:, ::-1],
                data1=sg[:, 0:nk][:, ::-1], initial=1.0, op0=ALU.mult, op1=ALU.bypass),
                  reads=[sgk, inck], writes=[inck])
            P.add("pool", lambda g, aa=aa, inc=inc, nk=nk: g.tensor_tensor(
                out=aa[:, :nk], in0=inc[:, 1:nk + 1], in1=inc[:, 0:nk], op=ALU.subtract),
                  reads=[inck], writes=[aak])

        def stage_b(n):
            pr, hh, i = items[n]
            vt = VT[pr % 2]
            vk = "VTc%d" % (pr % 2)
            r0 = 64 * hh
            aa = AA[n % NSET]
            aak = "AA%d" % (n % NSET)
            oc = oc_r.next()
            nkt = 2 * i + 2
            ng = (nkt + 3) // 4
            st_ = {}

            def tr(g4):
                n4 = min(4, nkt - 4 * g4)
                a = at_r.next()
                apv = PS[a][:].bitcast(BF16)
                for u in range(n4):
                    kti = 4 * g4 + u
                    P.add("pe", lambda g, apv=apv, u=u, kti=kti: g.transpose(
                        apv[:, u * 128:(u + 1) * 128], aa[:, kti * 128:(kti + 1) * 128], ident_bf[:]),
                          reads=[aak], writes=["ps%d" % a])
                s_ = ats_r.next()
                if True:
                    P.add("act", lambda g, s_=s_, apv=apv, n4=n4: g.activation(
                        out=ATS[s_][:, :n4 * 128], in_=apv[:, :n4 * 128], func=AF.Copy),
                          reads=["ps%d" % a], writes=["ATS%d" % s_])
                else:
                    P.add("dve", lambda g, s_=s_, apv=apv, n4=n4: g.tensor_copy(
                        out=ATS[s_][:, :n4 * 128], in_=apv[:, :n4 * 128]),
                          reads=["ps%d" % a], writes=["ATS%d" % s_])
                st_[g4] = (s_, n4)

            def pv(g4):
                s_, n4 = st_[g4]
                for u in range(n4):
                    kti = 4 * g4 + u
                    mm(PS[oc][:, :128], vt[:, kti, :], ATS[s_][:, u * 128:(u + 1) * 128], kti == 0,
                       kti == nkt - 1, [vk, "ATS%d" % s_], ["ps%d" % oc])

            tr(0)
            for g4 in range(ng):
                if g4 + 1 < ng:
                    tr(g4 + 1)
                pv(g4)
            o = ots_r.next()
            P.add("act", lambda g, oc=oc, r0=r0, o=o: g.activation(
                out=OTS[o][r0:r0 + 64, :], in_=PS[oc][r0:r0 + 64, :128], func=AF.Copy),
                  reads=["ps%d" % oc], writes=["OTSc%d" % o])
            dma(OT_d[2, pr][r0:r0 + 64, i * 128:(i + 1) * 128], OTS[o][r0:r0 + 64, :], ["OTSc%d" % o], [],
                eng="act")

        NI = len(items)
        DEPTH_A = NSET - 1
        for n in range(DEPTH_A):
            stage_a(n)
        for n in range(NI):
            stage_b(n)
            if n + DEPTH_A < NI:
                stage_a(n + DEPTH_A)
        P.flush()

    with ExitStack() as st:
        STG = [sb(st, "tstg%d" % i, [128, 8, 512], F32) for i in range(2)]
        WB = [sb(st, "twb%d" % i, [128, 8, 512], BF16) for i in range(3)]
        OTc = [sb(st, "OTc%d" % i, [128, 4, 512], BF16) for i in range(3)]
        GT = [sb(st, "GT%d" % i, [128, 512], F32) for i in range(3)]
        MF = sb(st, "MF", [128, 512], F32)
        MT = sb(st, "MT", [128, 8, 512], BF16)
        XO = sb(st, "XO", [128, 4, D], F32)
        X1 = sb(st, "X1", [128, 4, D], F32)
        X1T = sb(st, "X1T", [128, 8, 512], BF16)
        HR = [sb(st, "HR%d" % i, [128, 512], F32) for i in range(2)]
        HT = sb(st, "HT", [128, 32, 512], BF16)
        XN = sb(st, "XN", [128, D], F32)
        ST6 = sb(st, "ST6", [128, 2, 6], F32)
        MV = sb(st, "MV", [128, 4], F32)
        LNP = sb(st, "LNP", [128, 4, D], F32)
        stg_r, wb_r, gt_r, hr_r = Rot([0, 1]), Rot([0, 1, 2]), Rot([0, 1, 2]), Rot([0, 1])
        for n, src in enumerate((ln1g_d, ln1b_d, ln2g_d, ln2b_d)):
            dma(LNP[:, n, :], src[layer].partition_broadcast(128), [], ["LNP"])

        cast_flip = [0]

        def load_wt(src3, kc, n):
            i = stg_r.next()
            j = wb_r.next()
            dma(STG[i][:, :kc, :n], src3, [], ["tstg%d" % i])
            cast_flip[0] ^= 1
            if cast_flip[0]:
                P.add("dve", lambda g: g.tensor_copy(out=WB[j][:, :kc, :n], in_=STG[i][:, :kc, :n]),
                      reads=["tstg%d" % i], writes=["twb%d" % j])
            else:
                P.add("act", lambda g: g.activation(out=WB[j][:, :kc, :n], in_=STG[i][:, :kc, :n], func=AF.Copy),
                      reads=["tstg%d" % i], writes=["twb%d" % j])
            return j

        def layer_norm(src, dst, gi, key_src, key_dst):
            for hf in range(2):
                P.add("dve", lambda g, hf=hf: g.bn_stats(out=ST6[:, hf, :], in_=src[:, hf * 512:(hf + 1) * 512]),
                      reads=[key_src], writes=["ST6"])
            P.add("dve", lambda g: g.bn_aggr(out=MV[:, 0:2], in_=ST6[:].rearrange("p a b -> p (a b)")),
                  reads=["ST6"], writes=["MV"])
            P.add("act", lambda g: g.activation(out=MV[:, 2:3], in_=MV[:, 1:2], func=AF.Sqrt, bias=EPSB[:, 0:1],
                                                scale=1.0), reads=["MV", "EPSB"], writes=["MV"])
            P.add("dve", lambda g: g.reciprocal(out=MV[:, 3:4], in_=MV[:, 2:3]), reads=["MV"], writes=["MV"])
            P.add("dve", lambda g: g.tensor_scalar(out=XN[:], in0=src, scalar1=MV[:, 0:1], scalar2=MV[:, 3:4],
                                                   op0=ALU.subtract, op1=ALU.mult),
                  reads=[key_src, "MV"], writes=["XN"])
            P.add("dve", lambda g: g.tensor_tensor(out=XN[:], in0=XN[:], in1=LNP[:, gi, :], op=ALU.mult),
                  reads=["XN", "LNP"], writes=["XN"])
            P.add("dve", lambda g: g.tensor_tensor(out=dst, in0=XN[:], in1=LNP[:, gi + 1, :], op=ALU.add),
                  reads=["XN", "LNP"], writes=[key_dst])

        EPSB = sb(st, "EPSB", [128, 1], F32)
        P.add("pool", lambda g: g.memset(EPSB[:], EPS), writes=["EPSB"])
        wbr = wbr_d[layer]
        wout = wout_d[layer].rearrange("(c p) n -> p c n", p=128)
        wff1 = wff1_d[layer].rearrange("(c p) n -> p c n", p=128)
        wff2 = wff2_d[layer].rearrange("(c p) n -> p c n", p=128)
        for tcx in range(4):
            t0 = tcx * 512
            if layer == 0:
                dma(XO[:], xo_d[t0:t0 + 512, :].rearrange("(t p) d -> p t d", p=128), [], ["XO"])
            else:
                dma(XO[:], X1own_t[tcx].ap().rearrange("(t p) d -> p t d", p=128), ["X1own%d" % tcx], ["XO"])
            for bi in range(3):
                dma(OTc[bi][:], OT_d[bi].rearrange("w p t -> p w t")[:, :, t0:t0 + 512], [], [("OTc", bi)])
            for dh in range(2):
                wj = [load_wt(wbr[bi].rearrange("(c p) n -> p c n", p=128)[:, :, dh * 512:(dh + 1) * 512], 4, 512)
                      for bi in range(3)]
                for dq in range(4):
                    dc = dh * 4 + dq
                    for bi in range(3):
                        gt = gt_r.next()
                        dma(GT[gt][:], G_d[bi * 8 + dc][:, t0:t0 + 512], [], ["GT%d" % gt])
                        b = psr.next()
                        for wc in range(4):
                            mm(PS[b][:], WB[wj[bi]][:, wc, dq * 128:(dq + 1) * 128], OTc[bi][:, wc, :],
                               wc == 0, wc == 3, ["twb%d" % wj[bi], ("OTc", bi)], ["ps%d" % b])
                        if bi == 0:
                            P.add("dve", lambda g, b=b, gt=gt: g.tensor_tensor(out=MF[:], in0=PS[b][:], in1=GT[gt][:],
                                                                               op=ALU.mult),
                                  reads=["ps%d" % b, "GT%d" % gt], writes=["MF"])
                        else:
                            P.add("dve", lambda g, b=b, gt=gt: g.tensor_tensor(out=GT[gt][:], in0=PS[b][:],
                                                                               in1=GT[gt][:], op=ALU.mult),
                                  reads=["ps%d" % b, "GT%d" % gt], writes=["GT%d" % gt])
                            if bi == 1:
                                P.add("dve", lambda g, gt=gt: g.tensor_tensor(out=MF[:], in0=MF[:], in1=GT[gt][:],
                                                                               op=ALU.add),
                                      reads=["MF", "GT%d" % gt], writes=["MF"])
                            else:
                                P.add("dve", lambda g, gt=gt, dc=dc: g.tensor_tensor(out=MT[:, dc, :], in0=MF[:],
                                                                                      in1=GT[gt][:], op=ALU.add),
                                      reads=["MF", "GT%d" % gt], writes=["MT"])
            wo = [load_wt(wout[:, :, hf * 512:(hf + 1) * 512], 8, 512) for hf in range(2)]
            for t in range(4):
                for hf in range(2):
                    b = psr.next()
                    for dc in range(8):
                        mm(PS[b][:], MT[:, dc, t * 128:(t + 1) * 128], WB[wo[hf]][:, dc, :], dc == 0, dc == 7,
                           ["MT", "twb%d" % wo[hf]], ["ps%d" % b])
                    P.add("dve", lambda g, b=b, t=t, hf=hf: g.scalar_tensor_tensor(
                        out=X1[:, t, hf * 512:(hf + 1) * 512], in0=XO[:, t, hf * 512:(hf + 1) * 512], scalar=ALPHA,
                        in1=PS[b][:], op0=ALU.mult, op1=ALU.add), reads=["ps%d" % b, "XO"], writes=[("X1", t)])
            for t in range(4):
                layer_norm(X1[:, t, :], X1[:, t, :], 0, ("X1", t), ("X1", t))
            for t in range(4):
                for dc in range(8):
                    if dc % 4 == 0:
                        b = psr.next()
                    P.add("pe", lambda g, b=b, t=t, dc=dc: g.transpose(PS[b][:, (dc % 4) * 128:(dc % 4 + 1) * 128],
                                                                       X1[:, t, dc * 128:(dc + 1) * 128], ident_f[:]),
                          reads=[("X1", t)], writes=["ps%d" % b])
                    if dc % 4 == 3:
                        d0 = dc - 3
                        P.add("act", lambda g, b=b, t=t, d0=d0: g.activation(
                            out=X1T[:, d0:d0 + 4, t * 128:(t + 1) * 128],
                            in_=PS[b][:].rearrange("p (c q) -> p c q", q=128), func=AF.Copy),
                              reads=["ps%d" % b], writes=["X1T"])
            for ft in range(8):
                j = load_wt(wff1[:, :, ft * 512:(ft + 1) * 512], 8, 512)
                for fq in range(4):
                    fc = ft * 4 + fq
                    b = psr.next()
                    for dc in range(8):
                        mm(PS[b][:], WB[j][:, dc, fq * 128:(fq + 1) * 128], X1T[:, dc, :], dc == 0, dc == 7,
                           ["twb%d" % j, "X1T"], ["ps%d" % b])
                    hr = hr_r.next()
                    P.add("act", lambda g, b=b, hr=hr: g.activation(out=HR[hr][:], in_=PS[b][:], func=AF.Relu),
                          reads=["ps%d" % b], writes=["HR%d" % hr])
                    P.add("dve", lambda g, hr=hr, fc=fc: g.tensor_tensor(out=HT[:, fc, :], in0=HR[hr][:],
                                                                          in1=HR[hr][:], op=ALU.mult),
                          reads=["HR%d" % hr], writes=["HT"])
            ybanks = [psr.next() for _ in range(8)]
            for kg in range(4):
                for hf in range(2):
                    j = load_wt(wff2[:, kg * 8:(kg + 1) * 8, hf * 512:(hf + 1) * 512], 8, 512)
                    for t in range(4):
                        b = ybanks[t * 2 + hf]
                        for k8 in range(8):
                            fc = kg * 8 + k8
                            mm(PS[b][:], HT[:, fc, t * 128:(t + 1) * 128], WB[j][:, k8, :], fc == 0, fc == 31,
                               ["HT", "twb%d" % j], ["ps%d" % b])
            for t in range(4):
                for hf in range(2):
                    b = ybanks[t * 2 + hf]
                    P.add("dve", lambda g, b=b, t=t, hf=hf: g.scalar_tensor_tensor(
                        out=XO[:, t, hf * 512:(hf + 1) * 512], in0=X1[:, t, hf * 512:(hf + 1) * 512], scalar=ALPHA,
                        in1=PS[b][:], op0=ALU.mult, op1=ALU.add), reads=["ps%d" % b, ("X1", t)], writes=["XO"])
                layer_norm(XO[:, t, :], XO[:, t, :], 2, "XO", "XO")
            if is_last:
                dma(out_d[t0:t0 + 512, :].rearrange("(t p) d -> p t d", p=128), XO[:], ["XO"], [], eng="act")
            else:
                dma(X1own_t[tcx].ap().rearrange("(t p) d -> p t d", p=128), XO[:], ["XO"], ["X1own%d" % tcx],
                    eng="act")
                P.add("pool", lambda g, tcx=tcx: g.collective_compute(
                    "AllGather", ALU.bypass, replica_groups=[[0, 1], [2, 3], [4, 5], [6, 7]],
                    ins=[X1own_t[tcx].ap().opt()], outs=[X1all_t[tcx].ap().opt()]),
                      reads=["X1own%d" % tcx], writes=["X1all%d" % tcx], cc=True)
        P.flush()


_NC_CACHE = {}


def _get_nc(n_layers):
    if n_layers not in _NC_CACHE:
        _NC_CACHE[n_layers] = build_program(n_layers)
    return _NC_CACHE[n_layers]


def _own_tiles(x_b, p):
    return np.ascontiguousarray(x_b.reshape(16, 2, 128, D)[:, p].reshape(2048, D))


def _run(xs, weights, n_layers):
    in_maps = []
    for c in range(8):
        b, p = c // 2, c % 2
        pfv = np.zeros((128, 4), np.float32)
        pfv[:, 0] = p
        pfv[:, 1] = 1 - p
        pfv[:, 2] = 128 * p
        m = {"xa": np.ascontiguousarray(xs[b]), "xo": _own_tiles(xs[b], p), "pf": pfv}
        for k, v in weights.items():
            a = v[:n_layers]
            if k == "b_forget":
                a = a.reshape(n_layers, 8, 1)
            m[k] = np.ascontiguousarray(a)
        in_maps.append(m)
    nc = _get_nc(n_layers)
    res = run_bass_kernel_spmd(nc, in_maps, core_ids=list(range(8)))
    out = np.empty((NB, S, D), np.float32)
    for c in range(8):
        b, p = c // 2, c % 2
        out[b].reshape(16, 2, 128, D)[:, p] = res.results[c]["out"].reshape(16, 128, D)
    return out


def kernel(x, w_in, b_forget, w_branch, w_out, ln1_g, ln1_b, w_ff1, w_ff2, ln2_g, ln2_b):
    weights = dict(w_in=np.asarray(w_in, np.float32), b_forget=np.asarray(b_forget, np.float32),
                   w_branch=np.asarray(w_branch, np.float32), w_out=np.asarray(w_out, np.float32),
                   ln1_g=np.asarray(ln1_g, np.float32), ln1_b=np.asarray(ln1_b, np.float32),
                   w_ff1=np.asarray(w_ff1, np.float32), w_ff2=np.asarray(w_ff2, np.float32),
                   ln2_g=np.asarray(ln2_g, np.float32), ln2_b=np.asarray(ln2_b, np.float32))
    return _run(np.asarray(x, np.float32), weights, DEPTH)
```
